# Optimizing a Trainium2 kernel written in Bass

```python
import jax, jax.numpy as jnp
from jax import lax
import numpy as np

D_MODEL = 2048
BATCH = 4
SEQ = 2048
DEPTH = 1
DEC_BATCH = 32
DEC_SEQ = 4
PAST_LEN = 8192
PAGE_SIZE = 128

SGU_WIDTH = D_MODEL
SGU_GROUPS = 8
SGU_GROUP_DIM = SGU_WIDTH // SGU_GROUPS
CHUNK = 128
HEAD_DIM = 128
ATT_HEADS = D_MODEL // HEAD_DIM
ATT_WIDTH = ATT_HEADS * HEAD_DIM
DIL_WINDOWS = (128, 512, 2048)
DIL_RATES = (1, 4, 16)
N_DIL = 3
ATT_BLOCK = 128
ATT_COLS = 3 * N_DIL * ATT_WIDTH
IN_COLS = 2 * SGU_WIDTH + ATT_COLS + 2 * D_MODEL
PEER_HEADS = 8
PEER_TOPK = 16
N_KEYS = 128
N_EXPERTS = N_KEYS * N_KEYS
PEER_QDIM = 256
PEER_HALF = PEER_QDIM // 2
PEER_BLOCK = 128
NORM_EPS = 1e-6
MASK_VALUE = -1e30

kernel_name = "hybrid_sgu_dilated_attn_peer_step"


def rmsnorm(x, g):
    xf = x.astype(jnp.float32)
    y = xf * lax.rsqrt(jnp.mean(xf * xf, axis=-1, keepdims=True) + NORM_EPS)
    return (y * g.astype(jnp.float32)).astype(x.dtype)


def layernorm_gain(x, g):
    xf = x.astype(jnp.float32)
    xc = xf - jnp.mean(xf, axis=-1, keepdims=True)
    y = xc * lax.rsqrt(jnp.mean(xc * xc, axis=-1, keepdims=True) + NORM_EPS)
    return (y * g.astype(jnp.float32)).astype(x.dtype)


def alibi_slopes():
    n = N_DIL * ATT_HEADS
    e = jnp.arange(1, n + 1, dtype=jnp.float32)
    return jnp.exp2(-8.0 * e / n).reshape(N_DIL, ATT_HEADS)


def mixer_projections(x, norm_g, w_in):
    lead = x.shape[:-1]
    p = rmsnorm(x, norm_g) @ w_in
    u = jax.nn.gelu(p[..., :SGU_WIDTH])
    v = jax.nn.gelu(p[..., SGU_WIDTH:2 * SGU_WIDTH])
    att = p[..., 2 * SGU_WIDTH:2 * SGU_WIDTH + ATT_COLS].reshape(*lead, N_DIL, 3, ATT_HEADS, HEAD_DIM)
    gates = jax.nn.sigmoid(p[..., 2 * SGU_WIDTH + ATT_COLS:]).reshape(*lead, 2, D_MODEL)
    return u, v, att, gates


def sgu_mix(u, vn, sgu_w, sgu_b):
    n, length, _ = u.shape
    nc = length // CHUNK
    causal = jnp.tril(jnp.ones((CHUNK, CHUNK), dtype=sgu_w.dtype))
    vc = vn.reshape(n, nc, CHUNK, SGU_GROUPS, SGU_GROUP_DIM)
    mix = jnp.einsum("gts,ncsgd->nctgd", sgu_w * causal, vc) + sgu_b.T[None, None, :, :, None]
    return u * mix.reshape(n, length, SGU_WIDTH)


def dilated_band_attention(q, k, v, slopes, dil, window):
    b, s, h, dh = q.shape
    steps = window // dil
    length = s // dil

    def to_streams(t):
        return t.reshape(b, length, dil, h, dh).transpose(0, 2, 1, 3, 4).reshape(b * dil, length, h, dh)

    qs, ks, vs = to_streams(q), to_streams(k), to_streams(v)
    n = b * dil
    nb = -(-length // ATT_BLOCK)
    lp = nb * ATT_BLOCK
    qb = jnp.pad(qs, ((0, 0), (0, lp - length), (0, 0), (0, 0))).reshape(n, nb, ATT_BLOCK, h, dh)

    def band(t):
        tp = jnp.pad(t, ((0, 0), (ATT_BLOCK, lp - length), (0, 0), (0, 0)))
        prev = tp[:, :lp].reshape(n, nb, ATT_BLOCK, h, dh)
        cur = tp[:, ATT_BLOCK:].reshape(n, nb, ATT_BLOCK, h, dh)
        return jnp.concatenate([prev, cur], axis=2)

    kb, vb = band(ks), band(vs)
    sc = jnp.einsum("nbqhd,nbkhd->nbhqk", qb, kb, preferred_element_type=jnp.float32) * (HEAD_DIM ** -0.5)
    qi = jnp.arange(ATT_BLOCK)[:, None]
    ki = jnp.arange(2 * ATT_BLOCK)[None, :]
    delta = qi + ATT_BLOCK - ki
    k_abs = jnp.arange(nb)[:, None, None] * ATT_BLOCK + ki[None] - ATT_BLOCK
    valid = (delta >= 0) & (delta <= steps) & (k_abs >= 0)
    bias = -slopes[:, None, None] * (dil * delta).astype(jnp.float32)[None]
    sc = jnp.where(valid[None, :, None], sc + bias[None, None], MASK_VALUE)
    mx = jnp.max(sc, axis=-1, keepdims=True)
    p = jnp.exp(sc - mx)
    l = jnp.sum(p, axis=-1)
    o = jnp.einsum("nbhqk,nbkhd->nbqhd", p, vb.astype(jnp.float32)) / l.transpose(0, 1, 3, 2)[..., None]
    lse = (mx[..., 0] + jnp.log(l)).transpose(0, 1, 3, 2)
    o = o.reshape(n, lp, h, dh)[:, :length]
    lse = lse.reshape(n, lp, h)[:, :length]
    o = o.reshape(b, dil, length, h, dh).transpose(0, 2, 1, 3, 4).reshape(b, s, h, dh)
    lse = lse.reshape(b, dil, length, h).transpose(0, 2, 1, 3).reshape(b, s, h)
    return o, lse


def dilated_cached_attention(q, k_new, v_new, kv_cache, slopes, dil, window):
    lw = kv_cache.shape[1]
    ds = q.shape[1]
    k_all = jnp.concatenate([kv_cache[:, :, 0], k_new], axis=1)
    v_all = jnp.concatenate([kv_cache[:, :, 1], v_new], axis=1)
    nj = window // dil + 1
    j = jnp.arange(nj)
    idx = lw + jnp.arange(ds)[:, None] - dil * j[None, :]
    valid = idx >= 0
    idx = jnp.maximum(idx, 0)
    kg = k_all[:, idx]
    vg = v_all[:, idx]
    sc = jnp.einsum("bihd,bijhd->bhij", q, kg, preferred_element_type=jnp.float32) * (HEAD_DIM ** -0.5)
    bias = -slopes[:, None, None] * (dil * j).astype(jnp.float32)[None, None, :]
    sc = jnp.where(valid[None, None], sc + bias[None], MASK_VALUE)
    mx = jnp.max(sc, axis=-1, keepdims=True)
    p = jnp.exp(sc - mx)
    l = jnp.sum(p, axis=-1)
    o = jnp.einsum("bhij,bijhd->bihd", p, vg.astype(jnp.float32)) / l.transpose(0, 2, 1)[..., None]
    lse = (mx[..., 0] + jnp.log(l)).transpose(0, 2, 1)
    return o, lse


def combine_groups(outs, lses):
    w = jax.nn.softmax(jnp.stack(lses, axis=0), axis=0)
    o = jnp.sum(w[..., None] * jnp.stack(outs, axis=0), axis=0)
    return o.reshape(*o.shape[:-2], ATT_WIDTH)


def peer_ffn(x, w_q, sub_keys, u_tab, v_tab):
    lead = x.shape[:-1]
    xf = x.reshape(-1, D_MODEL)
    t = xf.shape[0]
    q = (xf @ w_q).reshape(t, PEER_HEADS, 2, PEER_HALF)
    s = jnp.einsum("thcd,ckd->thck", q, sub_keys, preferred_element_type=jnp.float32)
    s1, i1 = lax.top_k(s[:, :, 0], PEER_TOPK)
    s2, i2 = lax.top_k(s[:, :, 1], PEER_TOPK)
    cand = (s1[..., :, None] + s2[..., None, :]).reshape(t, PEER_HEADS, PEER_TOPK * PEER_TOPK)
    cidx = (i1[..., :, None] * N_KEYS + i2[..., None, :]).reshape(t, PEER_HEADS, PEER_TOPK * PEER_TOPK)
    top_s, pos = lax.top_k(cand, PEER_TOPK)
    eidx = jnp.take_along_axis(cidx, pos, axis=-1)
    gate = jax.nn.softmax(top_s, axis=-1)
    tp = -(-t // PEER_BLOCK) * PEER_BLOCK
    nblk = tp // PEER_BLOCK
    kk = PEER_HEADS * PEER_TOPK
    xb = jnp.pad(xf, ((0, tp - t), (0, 0))).reshape(nblk, PEER_BLOCK, D_MODEL)
    eb = jnp.pad(eidx.reshape(t, kk), ((0, tp - t), (0, 0))).reshape(nblk, PEER_BLOCK, kk)
    gb = jnp.pad(gate.reshape(t, kk), ((0, tp - t), (0, 0))).reshape(nblk, PEER_BLOCK, kk)

    def block_fn(args):
        xk, ek, gk = args
        hk = jnp.einsum("td,tkd->tk", xk, u_tab[ek], preferred_element_type=jnp.float32)
        act = jax.nn.gelu(hk) * gk
        return jnp.einsum("tk,tkd->td", act, v_tab[ek].astype(jnp.float32))

    out = lax.map(block_fn, (xb, eb, gb)).reshape(tp, D_MODEL)[:t]
    return out.reshape(*lead, D_MODEL).astype(x.dtype)


def block_output(x, a_out, b_out, gates, w_out, norm_ffn_g, peer_w_q, peer_sub_keys, peer_u, peer_v):
    merged = gates[..., 0, :] * a_out + gates[..., 1, :] * b_out.astype(x.dtype)
    h = x + merged @ w_out
    return h + peer_ffn(rmsnorm(h, norm_ffn_g), peer_w_q, peer_sub_keys, peer_u, peer_v)


def setup_inputs(seed: int = 0) -> dict:
    key = jax.random.key(seed)
    ks = jax.random.split(key, 17)
    f32 = jnp.float32

    def nrm(k, shape, scale):
        return jax.random.normal(k, shape, f32) * scale

    def gain(k, shape):
        return 1.0 + 0.05 * jax.random.normal(k, shape, f32)

    def kv_cache(k, window):
        return jax.random.normal(k, (DEPTH, DEC_BATCH, min(window, PAST_LEN), 2, ATT_HEADS, HEAD_DIM), f32)

    return {
        "x_prompt": nrm(ks[0], (BATCH, SEQ, D_MODEL), 1.0),
        "x_sample": nrm(ks[1], (DEC_BATCH, DEC_SEQ, D_MODEL), 1.0),
        "cache_kv_w128": kv_cache(ks[2], DIL_WINDOWS[0]),
        "cache_kv_w512": kv_cache(ks[3], DIL_WINDOWS[1]),
        "cache_kv_w2048": kv_cache(ks[4], DIL_WINDOWS[2]),
        "norm_mix_g": gain(ks[5], (DEPTH, D_MODEL)),
        "w_in": nrm(ks[6], (DEPTH, D_MODEL, IN_COLS), D_MODEL ** -0.5),
        "sgu_norm_g": gain(ks[7], (DEPTH, SGU_WIDTH)),
        "sgu_w": nrm(ks[8], (DEPTH, SGU_GROUPS, CHUNK, CHUNK), CHUNK ** -0.5),
        "sgu_b": 1.0 + nrm(ks[9], (DEPTH, SGU_GROUPS, CHUNK), 0.1),
        "w_out": nrm(ks[10], (DEPTH, D_MODEL, D_MODEL), D_MODEL ** -0.5),
        "norm_ffn_g": gain(ks[11], (DEPTH, D_MODEL)),
        "peer_w_q": nrm(ks[12], (DEPTH, D_MODEL, PEER_HEADS * PEER_QDIM), D_MODEL ** -0.5),
        "peer_sub_keys": nrm(ks[13], (DEPTH, 2, N_KEYS, PEER_HALF), PEER_HALF ** -0.5),
        "peer_u": nrm(ks[14], (DEPTH, N_EXPERTS, D_MODEL), D_MODEL ** -0.5),
        "peer_v": nrm(ks[15], (DEPTH, N_EXPERTS, D_MODEL), PEER_HEADS ** -0.5),
        "norm_final_g": gain(ks[16], (D_MODEL,)),
    }


def reference(x_prompt, x_sample, cache_kv_w128, cache_kv_w512, cache_kv_w2048, norm_mix_g, w_in, sgu_norm_g, sgu_w, sgu_b, w_out, norm_ffn_g, peer_w_q, peer_sub_keys, peer_u, peer_v, norm_final_g):
    slopes = alibi_slopes()
    caches = (cache_kv_w128, cache_kv_w512, cache_kv_w2048)
    yp, ys = x_prompt, x_sample
    kv_prompt = [[] for _ in range(N_DIL)]
    kv_sample = [[] for _ in range(N_DIL)]
    sgu_rows = []
    for layer in range(DEPTH):
        u, v, att, gates = mixer_projections(yp, norm_mix_g[layer], w_in[layer])
        vn = layernorm_gain(v, sgu_norm_g[layer])
        a_out = sgu_mix(u, vn, sgu_w[layer], sgu_b[layer])
        outs, lses = [], []
        for g in range(N_DIL):
            q, k, vv = att[..., g, 0, :, :], att[..., g, 1, :, :], att[..., g, 2, :, :]
            o, lse = dilated_band_attention(q, k, vv, slopes[g], DIL_RATES[g], DIL_WINDOWS[g])
            outs.append(o)
            lses.append(lse)
            lw = min(DIL_WINDOWS[g], yp.shape[1])
            kv_prompt[g].append(jnp.stack([k[:, -lw:], vv[:, -lw:]], axis=2))
        b_out = combine_groups(outs, lses)
        yp = block_output(yp, a_out, b_out, gates, w_out[layer], norm_ffn_g[layer], peer_w_q[layer], peer_sub_keys[layer], peer_u[layer], peer_v[layer])

        u, v, att, gates = mixer_projections(ys, norm_mix_g[layer], w_in[layer])
        vn = layernorm_gain(v, sgu_norm_g[layer])
        ds = ys.shape[1]
        lpad = -(-ds // CHUNK) * CHUNK - ds
        a_out = sgu_mix(jnp.pad(u, ((0, 0), (0, lpad), (0, 0))), jnp.pad(vn, ((0, 0), (0, lpad), (0, 0))), sgu_w[layer], sgu_b[layer])[:, :ds]
        sgu_rows.append(vn)
        outs, lses = [], []
        for g in range(N_DIL):
            q, k, vv = att[..., g, 0, :, :], att[..., g, 1, :, :], att[..., g, 2, :, :]
            o, lse = dilated_cached_attention(q, k, vv, caches[g][layer], slopes[g], DIL_RATES[g], DIL_WINDOWS[g])
            outs.append(o)
            lses.append(lse)
            kv_sample[g].append(jnp.stack([k, vv], axis=2))
        b_out = combine_groups(outs, lses)
        ys = block_output(ys, a_out, b_out, gates, w_out[layer], norm_ffn_g[layer], peer_w_q[layer], peer_sub_keys[layer], peer_u[layer], peer_v[layer])

    y_prompt = rmsnorm(yp, norm_final_g)
    y_sample = rmsnorm(ys, norm_final_g)
    kv_w128_prompt = jnp.stack(kv_prompt[0], axis=0)
    kv_w512_prompt = jnp.stack(kv_prompt[1], axis=0)
    kv_w2048_prompt = jnp.stack(kv_prompt[2], axis=0)
    kv_w128_sample = jnp.stack(kv_sample[0], axis=0)
    kv_w512_sample = jnp.stack(kv_sample[1], axis=0)
    kv_w2048_sample = jnp.stack(kv_sample[2], axis=0)
    sgu_v_sample = jnp.stack(sgu_rows, axis=0)
    return (y_prompt, y_sample, kv_w128_prompt, kv_w512_prompt, kv_w2048_prompt, kv_w128_sample, kv_w512_sample, kv_w2048_sample, sgu_v_sample)
```

```python
import contextlib
import math
import numpy as np
import concourse.bass as bass
import concourse.mybir as mybir
from concourse.bass_utils import run_bass_kernel_spmd

F32 = mybir.dt.float32
BF16 = mybir.dt.bfloat16
AF = mybir.ActivationFunctionType
ALU = mybir.AluOpType
AX = mybir.AxisListType

ENGS = ("pe", "act", "dve", "pool", "sp")
PSUM_RES = ("PJ", "SBK", "OB", "PT", "SC", "O2", "OBS")
NDSEM = 12

D = 2048
NH = 16
DH = 128
IN_COLS = 26624
SOFF = 2048
TOKS = 2048 + 16
BIG = 1.0e30
SCALE = DH ** -0.5
SQ = math.sqrt(DH)
EPS = 1e-6
DIL = (1, 4, 16)
NEXP = 16384


def alibi():
    e = np.arange(1, 49, dtype=np.float64)
    return np.exp2(-8.0 * e / 48).reshape(3, 16)


SLOPES = alibi()
A_BLOCKS = list(range(17))
B1_BUNDLES = [(hq, g) for hq in range(4) for g in range(3)]
B1_CUT = 99
import os
KNOB = os.environ.get('KNOB', '')
DBG_F32 = False


class Op:
    __slots__ = ("eng", "fn", "waits", "ticket", "needed", "is_dma", "idx")


class Prog:
    def __init__(self, nc):
        self.nc = nc
        self.es = contextlib.ExitStack()
        self.pes = contextlib.ExitStack()
        self.ops = {e: [] for e in ENGS}
        self.res = {}
        self.allops = []
        self.sem = {e: self.es.enter_context(nc.semaphore("s_" + e)) for e in ENGS}
        self.dsem = {}
        self.dcount = {}
        for q in ("sp", "act", "pool"):
            self.dsem[q] = [self.es.enter_context(nc.semaphore("d_%s%d" % (q, i))) for i in range(NDSEM)]
            self.dcount[q] = 0
        self.waited = {e: {} for e in ENGS}
        self.n_sb = 0
        self.cnt = {e: 0 for e in ENGS}
        self.dlast = {q: [None] * NDSEM for q in self.dsem}
        self.emitted = 0
        self.bar_start = 0
        self.ninstr = 0

    def _alloc(self, stack, shape, dt, psum=False):
        self.n_sb += 1
        name = "t%d" % self.n_sb
        if psum:
            return stack.enter_context(self.nc.psum_tensor(name, list(shape), dt))
        return stack.enter_context(self.nc.sbuf_tensor(name, list(shape), dt))

    def sb(self, shape, dt):
        return self._alloc(self.es, shape, dt)

    def psb(self, shape, dt):
        return self._alloc(self.pes, shape, dt)

    def ssb(self, stack, shape, dt):
        return self._alloc(stack, shape, dt)

    def ps(self, shape, dt):
        return self._alloc(self.es, shape, dt, psum=True)

    def _deps(self, reads, writes, eng=None):
        deps = []
        for r in reads:
            st = self.res.get(r)
            if st is not None and st[0] is not None:
                deps.append(st[0])
            if st is not None and isinstance(r, tuple) and r[0] in PSUM_RES:
                deps.extend(x for x in st[1] if x.eng != eng)
        for w in writes:
            st = self.res.get(w)
            if st is not None:
                if st[0] is not None:
                    deps.append(st[0])
                deps.extend(st[1])
        return deps

    def _commit(self, op, reads, writes):
        for r in reads:
            st = self.res.setdefault(r, [None, []])
            st[1].append(op)
        for w in writes:
            self.res[w] = [op, []]

    def op(self, eng, fn, reads=(), writes=()):
        o = Op()
        o.eng = eng
        o.fn = fn
        o.is_dma = False
        o.needed = False
        deps = self._deps(reads, writes, eng)
        o.waits = [d for d in deps if not (eng == "pe" and d.eng == "pe" and not d.is_dma)]
        for d in o.waits:
            d.needed = True
        self._commit(o, reads, writes)
        self.ops[eng].append(o)
        self.allops.append(o)
        return o

    def dma(self, q, fn, reads=(), writes=()):
        o = Op()
        o.eng = q
        o.fn = fn
        o.is_dma = True
        o.needed = True
        o.idx = self.dcount[q]
        self.dcount[q] += 1
        o.waits = list(self._deps(reads, writes))
        for d in o.waits:
            d.needed = True
        self._commit(o, reads, writes)
        self.ops[q].append(o)
        self.allops.append(o)
        return o

    def I(self, eng, meth, *args, R=(), W=(), **kw):
        return self.op(eng, (meth, args, kw), R, W)

    def D(self, q, out, in_, R=(), W=(), **kw):
        return self.dma(q, ("dma_start", (), dict(out=out, in_=in_, **kw)), R, W)

    def barrier(self):
        lastc = {}
        dm = {q: [] for q in self.dsem}
        for o in self.allops[self.bar_start:]:
            if o.fn is None:
                continue
            if o.is_dma:
                dm[o.eng].append(o)
            else:
                lastc[o.eng] = o
        self.bar_start = len(self.allops)
        deps = list(lastc.values())
        for q in dm:
            deps.extend(dm[q][-NDSEM:])
        for e in ENGS:
            o = Op()
            o.eng = e
            o.fn = None
            o.is_dma = False
            o.needed = False
            o.waits = [d for d in deps if not (e == "pe" and d.eng == "pe" and not d.is_dma)]
            for d in o.waits:
                d.needed = True
            self.ops[e].append(o)
            self.allops.append(o)
        self.res = {}

    def end_phase(self):
        self.barrier()
        self.emit()
        self.pes.close()
        self.pes = contextlib.ExitStack()

    def emit(self):
        nc = self.nc
        cnt = self.cnt
        dlast = self.dlast
        new_ops = self.allops[self.emitted:]
        self.emitted = len(self.allops)
        for o in new_ops:
            if o.is_dma:
                q = o.eng
                k = o.idx % NDSEM
                o.ticket = (("d", q, k), 16 * (o.idx // NDSEM + 1))
                prev = dlast[q][k]
                if prev is not None:
                    o.waits.append(prev)
                dlast[q][k] = o
            elif o.needed and o.fn is not None:
                cnt[o.eng] += 1
                o.ticket = (("c", o.eng), cnt[o.eng])
            else:
                o.ticket = None

        def semof(key):
            if key[0] == "c":
                return self.sem[key[1]]
            return self.dsem[key[1]][key[2]]

        def run(ename, eng):
            waited = self.waited[ename]
            ops = self.ops[ename]
            self.ops[ename] = []
            for o in ops:
                need = {}
                for d in o.waits:
                    key, val = d.ticket
                    if waited.get(key, 0) >= val:
                        continue
                    if need.get(key, 0) < val:
                        need[key] = val
                for key, val in need.items():
                    eng.wait_ge(semof(key), val)
                    waited[key] = val
                    self.ninstr += 1
                if o.fn is None:
                    continue
                if callable(o.fn):
                    ins = o.fn(eng)
                else:
                    meth, args, kw = o.fn
                    ins = getattr(eng, meth)(*args, **kw)
                self.ninstr += 1
                if o.ticket is not None:
                    key, val = o.ticket
                    ins.then_inc(semof(key), 16 if o.is_dma else 1)

        with nc.Block() as block:
            @block.tensor
            def _(e):
                run("pe", e)

            @block.scalar
            def _(e):
                run("act", e)

            @block.vector
            def _(e):
                run("dve", e)

            @block.gpsimd
            def _(e):
                run("pool", e)

            @block.sync
            def _(e):
                run("sp", e)


def const_tables(th):
    c = {}
    c["ident"] = np.eye(128, dtype=np.float32)
    j = np.arange(128)[:, None]
    i = np.arange(128)[None, :]
    bprev = np.where(i <= j, -(i - j + 128.0), -BIG)
    bcur = np.where(i >= j, -(i - j + 0.0), -BIG)
    c["btab"] = np.concatenate([bprev, bcur], axis=1).astype(np.float32)
    c["cmask"] = (j <= i).astype(np.float32)
    hbv = 0.0 if th == 1 else -BIG
    hb = np.zeros((128, 4), np.float32)
    hb[:, 0] = hbv
    hb[:64, 1] = hbv
    c["hb"] = hb
    st = np.full((128, 12, 16, 4), -BIG, np.float64)
    row = np.arange(128)
    for h in range(16):
        for q in range(4):
            d = 128 + q - row
            st[:, 0, h, q] = np.where(row >= q, -SLOPES[0, h] * d, -BIG)
            for r in range(4):
                if q == r:
                    st[:, 1 + r, h, q] = -SLOPES[1, h] * 4.0 * (128 - row)
                    st[:, 5 + r, h, q] = -SLOPES[2, h] * 16.0 * (128 - row)
            for kp in range(4):
                if kp <= q:
                    st[kp, 9, h, q] = -SLOPES[0, h] * (q - kp)
                if kp == q:
                    st[kp, 10, h, q] = 0.0
                    st[kp, 11, h, q] = 0.0
    c["stab"] = st.reshape(128, 12, 64).astype(np.float32)
    dm = np.zeros((16, 16, 16), np.float32)
    for p in range(16):
        dm[p, :, p] = 1.0
    c["dmask"] = dm.reshape(16, 256)
    cms = np.zeros((16, 16), np.float32)
    for b in range(4):
        for s in range(4):
            for t in range(4):
                if s <= t:
                    cms[4 * b + s, 4 * b + t] = 1.0
    c["cms"] = cms
    return c


def build(stop_after=None, debug=False):
    nc = bass.Bass("TRN2", target_bir_lowering=False)

    def di(n, s, dt=F32):
        return nc.dram_tensor(n, list(s), dt, kind="ExternalInput").ap()

    def do(n, s, dt=F32):
        return nc.dram_tensor(n, list(s), dt, kind="ExternalOutput").ap()

    def dscr(n, s, dt=F32):
        return nc.dram_tensor(n, list(s), dt, kind="Internal").ap()

    xp = di("xp", [2048, D])
    xs = di("xs", [16, D])
    if stop_after == "A":
        _di = di
        di = lambda n, s, dt=F32: (_di(n, s, dt) if n in ("g_mix", "ident") else None)
        dscr = lambda n, s, dt=F32: None
    w_in = di("w_in", [D, IN_COLS])
    w_out = di("w_out", [D, D])
    w_q = di("w_q", [D, D])
    subk = di("subk", [2, 128, 128])
    pu = di("pu", [NEXP, D])
    pv = di("pv", [NEXP, D])
    g_mix = di("g_mix", [1, D])
    g_sgu = di("g_sgu", [1, D])
    g_ffn = di("g_ffn", [1, D])
    g_fin = di("g_fin", [1, D])
    sgu_w = di("sgu_w", [8, 128, 128])
    sgu_b = di("sgu_b", [8, 128])
    ck = [di("ck128", [4, 128, 2, D]), di("ck512", [4, 512, 2, D]), di("ck2048", [4, 2048, 2, D])]
    ident_d = di("ident", [128, 128])
    btab_d = di("btab", [128, 256])
    cmask_d = di("cmask", [128, 128])
    hb_d = di("hb", [128, 4])
    stab_d = di("stab", [128, 12, 64])
    dmask_d = di("dmask", [16, 256])
    cms_d = di("cms", [16, 16])

    yp = do("yp", [1024, D])
    ys = do("ys", [16, D])
    kvp = [do("kvp128", [128, 2, D]), do("kvp512", [512, 2, D]), do("kvp2048", [1024, 2, D])]
    kvs = [do("kvs128", [16, 2, D]), do("kvs512", [16, 2, D]), do("kvs2048", [16, 2, D])]
    sguv = do("sguv", [16, D])

    OG = dscr("OG", [3, 1024, 16, 129])
    sqd = dscr("sqd", [3, 3, 16, D])
    Gd = dscr("Gd", [9, 128, NEXP], BF16)
    mgd = dscr("mgd", [9, 128, D], BF16)
    dbg = {}

    P = Prog(nc)
    PJ = [P.ps([128, 512], F32) for _ in range(2)]
    SBK = [P.ps([128, 512], F32) for _ in range(2)]
    OB = [P.ps([128, 512], F32) for _ in range(2)]
    PTs = [P.ps([128, 8, 128], BF16) for _ in range(2)]

    identf = P.sb([128, 128], F32)
    identb = P.sb([128, 128], BF16)
    onesb = P.sb([128, 16], BF16)
    zcol = P.sb([128, 1], F32)

    P.D("sp", out=identf[:], in_=ident_d, W=["identf"])
    P.I("dve", "tensor_copy", out=identb[:], in_=identf[:], R=["identf"], W=["identb"])
    P.I("dve", "memset", onesb[:], 1.0, W=["onesb"])
    P.I("dve", "memset", zcol[:], 0.0, W=["zcol"])

    bouts = P.sb([16, D], F32)
    es_ab = contextlib.ExitStack()
    xnT = P.ssb(es_ab, [128, 16, TOKS], BF16)
    wbs = [P.ssb(es_ab, [128, 16, 512], BF16) for _ in range(2)]
    wstate = {"n": 0}
    tcount = {"n": 0}

    def load_w(src, col0, extra=None):
        bufs = wbs + ([extra] if extra is not None else [])
        k = wstate["n"] % len(bufs)
        wstate["n"] += 1
        wb = bufs[k]
        P.D("pool", out=wb[:], in_=src[:, col0:col0 + 512].rearrange("(c p) n -> p c n", p=128),
              W=[("wb", id(wb))])
        return wb

    def rmsnorm_T(xt, npart, gB, xnb, dstT, col0, tagp):
        k = tcount["n"]
        tcount["n"] += 1
        ss = P.psb([128, 1], F32)
        rs = P.psb([128, 1], F32)
        sq = xnb["sq"]
        P.I("act", "activation", out=sq[:npart], in_=xt[:npart], func=AF.Square, accum_out=ss[:npart],
             R=[tagp + "xt"], W=["sqj", ("ss", k)])
        P.I("act", "activation", out=rs[:npart], in_=ss[:npart], func=AF.Sqrt, scale=1.0 / D, bias=EPS,
             R=[("ss", k)], W=[("rs", k)])
        P.I("dve", "reciprocal", out=rs[:npart], in_=rs[:npart], R=[("rs", k)], W=[("rs", k)])
        xb = xnb["b"][k % 2]
        P.I("dve", "scalar_tensor_tensor", out=xb[:npart], in0=xt[:npart], scalar=rs[:npart, 0:1], in1=gB[:npart],
                                                     op0=ALU.mult, op1=ALU.mult,
             R=[tagp + "xt", ("rs", k), "gB"], W=[("xnb", k % 2)])
        for c4 in range(4):
            half = (k * 4 + c4) % 2
            for j in range(4):
                c = c4 * 4 + j
                P.I("pe", "transpose", out=PTs[half][:, j, :npart], in_=xb[:npart, c * 128:(c + 1) * 128],
                                                                       identity=identb[:npart, :npart],
                     R=[("xnb", k % 2), "identb"], W=[("PT", half)])
            eng = "act" if c4 % 2 == 0 else "dve"
            if eng == "act":
                P.I("act", "copy", out=dstT[:, c4 * 4:(c4 + 1) * 4, col0:col0 + npart], in_=PTs[half][:, 0:4, :npart],
                     R=[("PT", half)], W=[("dstT", id(dstT), col0)])
            else:
                P.I("dve", "tensor_copy", out=dstT[:, c4 * 4:(c4 + 1) * 4, col0:col0 + npart], in_=PTs[half][:, 0:4, :npart],
                     R=[("PT", half)], W=[("dstT", id(dstT), col0)])

    xts = [P.psb([128, D], F32) for _ in range(2)]
    xnbA = {"sq": P.psb([128, D], BF16), "b": [P.psb([128, D], BF16) for _ in range(2)]}
    gBmix = P.psb([128, D], F32)
    P.D("sp", out=gBmix[:], in_=g_mix.partition_broadcast(128), W=["gB"])
    for blk in A_BLOCKS:
        xt = xts[blk % 2]
        npart = 128 if blk < 16 else 16
        src = xp[blk * 128:(blk + 1) * 128, :] if blk < 16 else xs
        tag = "A%d" % (blk % 2)
        P.D("sp", out=xt[:npart], in_=src, W=[tag + "xt"])
        rmsnorm_T(xt, npart, gBmix, xnbA, xnT, blk * 128, tag)
    if debug:
        if DBG_F32:
            xnT32 = P.psb([128, 16, 256], F32)
            P.I("dve", "tensor_copy", out=xnT32[:], in_=xnT[:, :, 0:256], R=[("dstT", id(xnT), c * 128) for c in range(17)], W=["xnT32"])
            dbg["xnT"] = do("dbg_xnT32", [128, 16, 256], F32)
            P.D("sp", out=dbg["xnT"], in_=xnT32[:], R=["xnT32"])
        else:
            dbg["xnT"] = do("dbg_xnT", [128, 16, TOKS], BF16)
            P.D("sp", out=dbg["xnT"], in_=xnT[:], R=[("dstT", id(xnT), c * 128) for c in range(17)])
    P.end_phase()
    if stop_after == "A":
        return nc, P, dbg

    wb3 = P.psb([128, 16, 512], BF16)
    qT = P.psb([128, 4, 2048], BF16)
    kT = P.psb([128, 4, 2048], BF16)
    vsb = P.psb([128, 16, 512], BF16)
    tqs = [P.psb([128, 512], BF16) for _ in range(2)]
    st32 = [P.psb([128, 512], F32) for _ in range(3)]
    Sbuf = [P.psb([128, 256], F32) for _ in range(2)]
    Ptb_ = [P.psb([128, 256], BF16) for _ in range(2)]
    ostg = [P.psb([128, 4, 129], F32) for _ in range(2)]
    btab = P.psb([128, 256], F32)
    hb = P.psb([128, 4], F32)
    P.D("sp", out=btab[:], in_=btab_d, W=["btab"])
    P.D("sp", out=hb[:], in_=hb_d, W=["hb"])

    pjn = {"n": 0}

    def proj_block(wb, start, step, npart):
        k = pjn["n"] % 2
        pjn["n"] += 1
        pj = PJ[k]
        for dc in range(16):
            if npart == 128:
                lt = xnT[:, dc, start:start + 128 * step:step]
            else:
                lt = xnT[:, dc, start:start + npart]
            P.I("pe", "matmul", pj[:npart, :], lhsT=lt, rhs=wb[:, dc, :], start=(dc == 0), stop=(dc == 15),
                 R=[("wb", id(wb)), "xnT"], W=[("PJ", k)])
        return pj, k

    KV_BLOCKS = [
        [(128 * lb, 1) for lb in range(7, 16)],
        [(rho + 4 * 128 * sb, 4) for rho in range(4) for sb in range(1, 4)],
        [(r, 16) for r in range(16)],
    ]
    Q_BLOCKS = [
        [(128 * lb, 1) for lb in range(8, 16)],
        [(rho + 4 * 128 * sb, 4) for rho in range(4) for sb in range(2, 4)],
        [(r, 16) for r in range(16)],
    ]

    def kv_out_rows(g, bi):
        if g == 0:
            if bi == 8:
                return slice(0, 128), kvp[0][0:128]
            return None
        if g == 1:
            rho, sbi = bi // 3, bi % 3
            if sbi == 2:
                return slice(0, 128), kvp[1][rho:rho + 509:4]
            return None
        r = bi
        return slice(64, 128), kvp[2][r:r + 1009:16]

    tn = {"n": 0, "s": 0}

    def to_T(pj, pk, dst, col0):
        k = tn["n"] % 2
        tn["n"] += 1
        tq = tqs[k]
        P.I("act", "copy", out=tq[:], in_=pj[:], R=[("PJ", pk)], W=[("tq", k)])
        for j in range(4):
            P.I("pe", "transpose", out=PTs[k][:, j, :], in_=tq[:, j * 128:(j + 1) * 128], identity=identb[:],
                 R=[("tq", k), "identb"], W=[("PT", k)])
        P.I("dve", "tensor_copy", out=dst[:, :, col0:col0 + 128], in_=PTs[k][:, 0:4, :],
             R=[("PT", k)], W=[("T", id(dst))])

    def stage32(pj, pk, npart):
        k = tn["s"] % 3
        tn["s"] += 1
        st = st32[k]
        if True:
            P.I("act", "copy", out=st[:npart], in_=pj[:npart], R=[("PJ", pk)], W=[("st32", k)])
        else:
            P.I("dve", "tensor_copy", out=st[:npart], in_=pj[:npart], R=[("PJ", pk)], W=[("st32", k)])
        return st, k

    def attention(g, hq):
        units = []
        if g == 0:
            for qb in range(8):
                units.append((qb * 128, 128, [(qb, 0, (0 if qb == 0 else 2)), (qb + 1, 128, 2)], OG[0, qb * 128:(qb + 1) * 128]))
        elif g == 1:
            for rho in range(4):
                for sbi in range(2):
                    qi = rho * 2 + sbi
                    r0 = rho + 512 * sbi
                    units.append((qi * 128, 128, [(rho * 3 + sbi, 0, (0 if sbi == 0 else 2)), (rho * 3 + sbi + 1, 128, 2)],
                                  OG[1, r0:r0 + 509:4]))
        else:
            for r in range(16):
                units.append((r * 128 + 64, 64, [(r, 128 + 64, 1)], OG[2, r:r + 1009:16]))
        for ui, (qc0, nq, tiles, orow) in enumerate(units):
            osl = ui % 2
            for h in range(4):
                head = hq * 4 + h
                cgh = float(SLOPES[g, head] * DIL[g] * SQ)
                sl = (ui * 4 + h) % 2
                Sps = SBK[sl]
                nt = len(tiles)
                for j, (kidx, tcol, hbsel) in enumerate(tiles):
                    P.I("pe", "matmul", Sps[:, j * 128:j * 128 + nq], lhsT=kT[:, h, kidx * 128:(kidx + 1) * 128],
                                                                                rhs=qT[:, h, qc0:qc0 + nq], start=True, stop=True,
                         R=[("T", id(kT)), ("T", id(qT))], W=[("SBK", sl)])
                Sb = Sbuf[sl]
                if nt == 2:
                    P.I("dve", "scalar_tensor_tensor", out=Sb[:, 0:256], in0=btab[:, 0:256], scalar=cgh, in1=Sps[:, 0:256],
                                                                                         op0=ALU.mult, op1=ALU.add,
                         R=["btab", ("SBK", sl)], W=[("Sbuf", sl)])
                else:
                    tcol = tiles[0][1]
                    P.I("dve", "scalar_tensor_tensor", out=Sb[:, 0:nq], in0=btab[:, tcol:tcol + nq], scalar=cgh,
                                                                                                    in1=Sps[:, 0:nq], op0=ALU.mult, op1=ALU.add,
                         R=["btab", ("SBK", sl)], W=[("Sbuf", sl)])
                Pt = Ptb_[sl]
                for j, (kidx, tcol, hbsel) in enumerate(tiles):
                    P.I("act", "activation", out=Pt[:, j * 128:j * 128 + nq], in_=Sb[:, j * 128:j * 128 + nq], func=AF.Exp,
                                                                                      bias=hb[:, hbsel:hbsel + 1], scale=SCALE,
                         R=[("Sbuf", sl), "hb"], W=[("Pt", sl, j)])
                for j, (kidx, tcol, hbsel) in enumerate(tiles):
                    P.I("pe", "matmul", OB[osl][:nq, h * 128:(h + 1) * 128], lhsT=Pt[:, j * 128:j * 128 + nq],
                                                                             rhs=vsb[:, kidx, h * 128:(h + 1) * 128], start=(j == 0), stop=(j == nt - 1),
                         R=[("Pt", sl, j), "vsb"], W=[("OB", osl)])
                    P.I("pe", "matmul", PJ[osl][:nq, h:h + 1], lhsT=Pt[:, j * 128:j * 128 + nq],
                                                                   rhs=onesb[:, 0:1], start=(j == 0), stop=(j == nt - 1),
                         R=[("Pt", sl, j), "onesb"], W=[("PJ", osl)])
            og = ostg[osl]
            P.I("act", "copy", out=og[:nq, :, 0:128], in_=OB[osl][:nq, :].rearrange("p (h d) -> p h d", h=4),
                 R=[("OB", osl)], W=[("ostg", osl)])
            P.I("dve", "tensor_copy", out=og[:nq, :, 128], in_=PJ[osl][:nq, 0:4],
                 R=[("PJ", osl)], W=[("ostg", osl)])
            P.D("sp", out=orow[:, hq * 4:(hq + 1) * 4, :], in_=og[:nq],
                  R=[("ostg", osl)], W=["OG"])

    for (hq, g) in B1_BUNDLES:
        for _once in (0,):
            base = 4096 + g * 6144 + hq * 512
            wb = load_w(w_in, base, wb3)
            for bi, (st, sp_) in enumerate(Q_BLOCKS[g]):
                pj, pk = proj_block(wb, st, sp_, 128)
                to_T(pj, pk, qT, bi * 128)
            if B1_CUT < 2:
                continue
            pj, pk = proj_block(wb, SOFF, 1, 16)
            s32, sk = stage32(pj, pk, 16)
            P.D("sp", out=sqd[g, 0, :, hq * 512:(hq + 1) * 512], in_=s32[:16],
                  R=[("st32", sk)], W=["sqd"])
            if B1_CUT < 3:
                continue
            wb = load_w(w_in, base + 2048, wb3)
            for bi, (st, sp_) in enumerate(KV_BLOCKS[g]):
                pj, pk = proj_block(wb, st, sp_, 128)
                kvo = kv_out_rows(g, bi) if 'nokvout' not in KNOB else None
                if kvo is not None:
                    psl, rows = kvo
                    s32, sk = stage32(pj, pk, 128)
                    P.D("sp", out=rows[:, 0, hq * 512:(hq + 1) * 512], in_=s32[psl],
                          R=[("st32", sk)])
                to_T(pj, pk, kT, bi * 128)
            if 'nosample' in KNOB:
                continue
            pj, pk = proj_block(wb, SOFF, 1, 16)
            s32, sk = stage32(pj, pk, 16)
            if 'nosqd' not in KNOB:
                P.D("sp", out=sqd[g, 1, :, hq * 512:(hq + 1) * 512], in_=s32[:16],
                      R=[("st32", sk)], W=["sqd"])
            if 'nokvs' not in KNOB:
                P.D("sp", out=kvs[g][:, 0, hq * 512:(hq + 1) * 512], in_=s32[:16],
                      R=[("st32", sk)])
            if B1_CUT < 4:
                continue
            wb = load_w(w_in, base + 4096, wb3)
            for bi, (st, sp_) in enumerate(KV_BLOCKS[g]):
                pj, pk = proj_block(wb, st, sp_, 128)
                kvo = kv_out_rows(g, bi)
                if kvo is not None:
                    psl, rows = kvo
                    s32, sk = stage32(pj, pk, 128)
                    P.D("sp", out=rows[:, 1, hq * 512:(hq + 1) * 512], in_=s32[psl],
                          R=[("st32", sk)])
                P.I("act", "copy", out=vsb[:, bi, :], in_=pj[:], R=[("PJ", pk)], W=["vsb"])
            pj, pk = proj_block(wb, SOFF, 1, 16)
            s32, sk = stage32(pj, pk, 16)
            P.D("sp", out=sqd[g, 2, :, hq * 512:(hq + 1) * 512], in_=s32[:16],
                  R=[("st32", sk)], W=["sqd"])
            P.D("sp", out=kvs[g][:, 1, hq * 512:(hq + 1) * 512], in_=s32[:16],
                  R=[("st32", sk)])
            if B1_CUT < 5:
                continue
            attention(g, hq)
    if debug and B1_CUT >= 6:
        dbg["OG"] = do("dbg_OG", [3, 1024, 16, 129])
        for g in range(3):
            P.D("sp", out=dbg["OG"][g], in_=OG[g], R=["OG"])
    P.end_phase()
    if stop_after == "B1":
        return nc, P, dbg

    stab = P.psb([128, 12, 64], F32)
    dmask = P.psb([16, 256], F32)
    P.D("sp", out=stab[:], in_=stab_d, W=["stab"])
    P.D("sp", out=dmask[:], in_=dmask_d, W=["dmask"])
    qload = P.psb([16, D], F32)
    qbf = P.psb([16, D], BF16)
    qTs = P.psb([128, 3, 16, 16], BF16)
    kTn = P.psb([128, 3, 16, 16], BF16)
    for g in range(3):
        for kind, dst in ((0, qTs), (1, kTn)):
            P.D("sp", out=qload[:], in_=sqd[g, kind], R=["sqd"], W=["qload"])
            P.I("dve", "tensor_copy", out=qbf[:], in_=qload[:], R=["qload"], W=["qbf"])
            for h4 in range(4):
                for j in range(4):
                    h = h4 * 4 + j
                    P.I("pe", "transpose", out=PTs[0][:, j, 0:16], in_=qbf[0:16, h * 128:(h + 1) * 128], identity=identb[0:16, 0:16],
                         R=["qbf", "identb"], W=[("PT", 0)])
                P.I("act", "copy", out=dst[:, g, h4 * 4:(h4 + 1) * 4, :], in_=PTs[0][:, 0:4, 0:16],
                     R=[("PT", 0)], W=[("qk", id(dst))])
    Oacc = P.psb([16, D], F32)
    Lacc = P.psb([16, 256], F32)
    P.I("dve", "memset", Oacc[:], 0.0, W=["Oacc"])
    P.I("dve", "memset", Lacc[:], 0.0, W=["Lacc"])
    PtS = [P.psb([128, 16, 16], BF16) for _ in range(4)]
    for b in range(4):
        P.I("pool", "memset", PtS[b][:], 0.0, W=[("PtS", b)])
    kvc = [P.psb([128, 2, D], BF16) for _ in range(2)]
    kTc = [P.psb([128, 16, 128], BF16) for _ in range(2)]
    SbS = [P.psb([128, 64], F32) for _ in range(2)]
    tl = 0
    for b in range(4):
        for g in range(3):
            tiles = []
            if g == 0:
                tiles.append((128, ck[0][b], 0))
            elif g == 1:
                v4 = ck[1][b].rearrange("(m r) k c -> r m k c", r=4)
                for r in range(4):
                    tiles.append((128, v4[r], 1 + r))
            else:
                v16 = ck[2][b].rearrange("(m r) k c -> r m k c", r=16)
                for r in range(4):
                    tiles.append((128, v16[r], 5 + r))
            tiles.append((4, None, 9 + g))
            for (nk, src, tbl) in tiles:
                sl = tl % 2
                tl += 1
                kv_ = kvc[sl]
                kt_ = kTc[sl]
                if src is not None:
                    P.D("pool", out=kv_[:], in_=src, W=[("kvc", sl)], max_dma_last_dim=4096)
                else:
                    P.D("pool", out=kv_[0:4, 0, :], in_=sqd[g, 1, 4 * b:4 * b + 4, :], R=["sqd"], W=[("kvc", sl)], max_dma_last_dim=4096)
                    P.D("pool", out=kv_[0:4, 1, :], in_=sqd[g, 2, 4 * b:4 * b + 4, :], R=["sqd"], W=[("kvc", sl)], max_dma_last_dim=4096)
                for h4 in range(4):
                    half = h4 % 2
                    for j in range(4):
                        h = h4 * 4 + j
                        P.I("pe", "transpose", out=PTs[half][:, j, 0:nk], in_=kv_[0:nk, 0, h * 128:(h + 1) * 128],
                                                                                             identity=identb[0:nk, 0:nk],
                             R=[("kvc", sl), "identb"], W=[("PT", half)])
                    P.I("act", "copy", out=kt_[:, h4 * 4:(h4 + 1) * 4, 0:nk], in_=PTs[half][:, 0:4, 0:nk],
                         R=[("PT", half)], W=[("kTc", sl)])
                Sps = PJ[sl]
                for h in range(16):
                    P.I("pe", "matmul", Sps[0:nk, h * 4:(h + 1) * 4], lhsT=kt_[:, h, 0:nk],
                                                                                          rhs=qTs[:, g, h, 4 * b:4 * b + 4], start=True, stop=True,
                         R=[("kTc", sl), ("qk", id(qTs))], W=[("PJ", sl)])
                Sb = SbS[sl]
                P.I("dve", "scalar_tensor_tensor", out=Sb[0:nk, :], in0=Sps[0:nk, 0:64], scalar=SCALE, in1=stab[0:nk, tbl, :],
                                                                                             op0=ALU.mult, op1=ALU.add,
                     R=[("PJ", sl), "stab"], W=[("SbS", sl)])
                pts = PtS[b]
                P.I("act", "activation", out=pts[0:nk, :, 4 * b:4 * b + 4], in_=Sb[0:nk, :].rearrange("p (h q) -> p h q", h=16),
                                                                               func=AF.Exp,
                     R=[("SbS", sl)], W=[("PtS", b)])
                obanks = [OB[0], OB[1], SBK[0], SBK[1]]
                for h in range(16):
                    ob = obanks[h // 4]
                    P.I("pe", "matmul", ob[0:16, (h % 4) * 128:(h % 4 + 1) * 128], lhsT=pts[0:nk, h, :],
                                                                                       rhs=kv_[0:nk, 1, h * 128:(h + 1) * 128], start=True, stop=True,
                         R=[("PtS", b), ("kvc", sl)], W=[("OBS", h // 4)])
                P.I("pe", "matmul", PJ[sl][0:16, 256:512], lhsT=onesb[0:nk, 0:16], rhs=pts[0:nk].rearrange("p h q -> p (h q)"), start=True, stop=True,
                     R=[("PtS", b), "onesb"], W=[("PJ", sl)])
                for k4 in range(4):
                    ob = obanks[k4]
                    P.I("dve", "tensor_tensor", out=Oacc[:, k4 * 512:(k4 + 1) * 512], in0=ob[0:16, :], in1=Oacc[:, k4 * 512:(k4 + 1) * 512], op=ALU.add,
                         R=[("OBS", k4), "Oacc"], W=["Oacc", ("OBS", k4)])
                P.I("dve", "tensor_tensor", out=Lacc[:], in0=PJ[sl][0:16, 256:512], in1=Lacc[:], op=ALU.add, R=[("PJ", sl), "Lacc"], W=["Lacc"])
    ltmp = P.psb([16, 256], F32)
    lsum = P.psb([16, 16], F32)
    P.I("dve", "tensor_tensor", out=ltmp[:], in0=Lacc[:], in1=dmask[:], op=ALU.mult, R=["Lacc", "dmask"], W=["ltmp"])
    P.I("dve", "tensor_reduce", out=lsum[:], in_=ltmp[:].rearrange("p (h q) -> p h q", h=16), axis=AX.X, op=ALU.add, R=["ltmp"], W=["lsum"])
    P.I("dve", "reciprocal", out=lsum[:], in_=lsum[:], R=["lsum"], W=["lsum"])
    P.I("dve", "tensor_tensor", out=bouts[:].rearrange("p (h d) -> p h d", h=16), in0=Oacc[:].rearrange("p (h d) -> p h d", h=16),
                                          in1=lsum[:, :, None].to_broadcast([16, 16, 128]), op=ALU.mult,
         R=["Oacc", "lsum"], W=["bouts"])
    if debug:
        dbg["bouts"] = do("dbg_bouts", [16, D])
        P.D("sp", out=dbg["bouts"], in_=bouts[:], R=["bouts"])
    P.end_phase()
    if stop_after == "B1s":
        return nc, P, dbg

    vm = P.psb([128, 8, D], BF16)
    vms = P.psb([16, D], BF16)
    vsf = P.psb([16, D], F32)
    vsb16 = P.psb([16, D], BF16)
    gBsgu = P.psb([128, D], F32)
    cmask = P.psb([128, 128], F32)
    cms = P.psb([16, 16], F32)
    wst = P.psb([128, 128], F32)
    WcT = P.psb([128, 8, 128], BF16)
    WsT = P.psb([16, 8, 16], BF16)
    Wtmp = P.psb([16, 8, 16], F32)
    bT = P.psb([128, 8], F32)
    bsT = P.psb([16, 8], F32)
    sbl = P.psb([8, 128], F32)
    stats = P.psb([128, 9, 4, 6], F32)
    mv = P.psb([128, 9, 2], F32)
    utmp = [P.psb([128, 512], F32) for _ in range(2)]
    t2048 = P.psb([128, D], F32)
    ogt = [P.psb([128, 3, 4, 129], F32) for _ in range(2)]
    num = P.psb([128, 4, 129], F32)
    rl = P.psb([128, 4], F32)
    bt = P.psb([128, 512], F32)
    P.D("sp", out=gBsgu[:], in_=g_sgu.partition_broadcast(128), W=["gBsgu"])
    P.D("sp", out=cmask[:], in_=cmask_d, W=["cmask"])
    P.D("sp", out=cms[:], in_=cms_d, W=["cms"])
    P.D("sp", out=sbl[:], in_=sgu_b, W=["sbl"])
    for g in range(8):
        P.D("sp", out=wst[:], in_=sgu_w[g], W=["wst"])
        P.I("pe", "transpose", out=SBK[0][:, 0:128], in_=wst[:], identity=identf[:], R=["wst", "identf"], W=[("SBK", 0)])
        P.I("dve", "tensor_tensor", out=WcT[:, g, :], in0=SBK[0][:, 0:128], in1=cmask[:], op=ALU.mult,
             R=[("SBK", 0), "cmask"], W=["WcT"])
    P.I("pe", "transpose", out=SBK[1][:, 0:8], in_=sbl[0:8, :], identity=identf[0:8, 0:8], R=["sbl", "identf"], W=[("SBK", 1)])
    P.I("dve", "tensor_copy", out=bT[:], in_=SBK[1][:, 0:8], R=[("SBK", 1)], W=["bT"])
    P.I("dve", "memset", WsT[:], 0.0, W=["WsT"])
    for b in range(4):
        P.D("sp", out=WsT[4 * b:4 * b + 4, :, 4 * b:4 * b + 4], in_=WcT[0:4, :, 0:4], R=["WcT", "WsT"], W=["WsT"])
        P.D("sp", out=bsT[4 * b:4 * b + 4, :], in_=bT[0:4, :], R=["bT"], W=["bsT"])

    TB = list(range(8)) + ["s"]

    def tok_block(tb):
        if tb == "s":
            return SOFF, 16
        return 1024 + tb * 128, 128

    for ct in range(4):
        wb = load_w(w_in, 2048 + ct * 512)
        for ti, tb in enumerate(TB):
            st, npart = tok_block(tb)
            pj, pk = proj_block(wb, st, 1, npart)
            if tb == "s":
                P.I("act", "activation", out=vsf[:, ct * 512:(ct + 1) * 512], in_=pj[:16], func=AF.Gelu_apprx_tanh,
                     R=[("PJ", pk)], W=[("vsf", ct)])
                P.I("dve", "bn_stats", out=stats[:16, 8, ct, :], in_=vsf[:, ct * 512:(ct + 1) * 512], R=[("vsf", ct)], W=[("stats", 8)])
            else:
                P.I("act", "activation", out=vm[:, tb, ct * 512:(ct + 1) * 512], in_=pj[:], func=AF.Gelu_apprx_tanh,
                     R=[("PJ", pk)], W=[("vm", tb, ct)])
                P.I("dve", "bn_stats", out=stats[:, tb, ct, :], in_=vm[:, tb, ct * 512:(ct + 1) * 512], R=[("vm", tb, ct)], W=[("stats", tb)])
    for ti, tb in enumerate(TB):
        npart = 16 if tb == "s" else 128
        P.I("dve", "bn_aggr", out=mv[:npart, ti, :], in_=stats[:npart, ti].rearrange("p a b -> p (a b)"),
             R=[("stats", ti)], W=[("mv", ti)])
        P.I("act", "activation", out=mv[:npart, ti, 1:2], in_=mv[:npart, ti, 1:2], func=AF.Sqrt, bias=EPS, scale=1.0,
             R=[("mv", ti)], W=[("mv", ti)])
        P.I("dve", "reciprocal", out=mv[:npart, ti, 1:2], in_=mv[:npart, ti, 1:2], R=[("mv", ti)], W=[("mv", ti)])
        src = vsf[:] if tb == "s" else vm[:, tb, :]
        rtag = [("vsf", c) for c in range(4)] if tb == "s" else [("vm", tb, c) for c in range(4)]
        P.I("dve", "scalar_tensor_tensor", out=t2048[:npart], in0=src, scalar=mv[:npart, ti, 0:1], in1=gBsgu[:npart],
                                                                                 op0=ALU.subtract, op1=ALU.mult,
             R=rtag + [("mv", ti), "gBsgu"], W=["t2048"])
        if tb == "s":
            P.I("act", "activation", out=vsf[:], in_=t2048[:16], func=AF.Identity, scale=mv[:16, ti, 1:2], R=["t2048", ("mv", ti)], W=rtag)
            P.I("dve", "tensor_copy", out=vsb16[:], in_=vsf[:], R=rtag, W=["vsb16"])
            P.D("sp", out=sguv, in_=vsf[:], R=rtag)
        else:
            P.I("act", "activation", out=vm[:, tb, :], in_=t2048[:], func=AF.Identity, scale=mv[:, ti, 1:2], R=["t2048", ("mv", ti)], W=rtag)
    for ct in range(4):
        wb = load_w(w_in, ct * 512)
        for ti, tb in enumerate(TB):
            st, npart = tok_block(tb)
            pj, pk = proj_block(wb, st, 1, npart)
            ut = utmp[ti % 2]
            P.I("act", "activation", out=ut[:npart], in_=pj[:npart], func=AF.Gelu_apprx_tanh,
                 R=[("PJ", pk)], W=[("utmp", ti % 2)])
            sl = ti % 2
            for gg in range(2):
                g = ct * 2 + gg
                if tb == "s":
                    P.I("pe", "matmul", SBK[sl][0:16, gg * 256:(gg + 1) * 256], lhsT=WsT[:, g, :], rhs=vsb16[:, g * 256:(g + 1) * 256], start=True, stop=True,
                         R=["WsT", "vsb16"], W=[("SBK", sl)])
                else:
                    P.I("pe", "matmul", SBK[sl][:, gg * 256:(gg + 1) * 256], lhsT=WcT[:, g, :], rhs=vm[:, tb, g * 256:(g + 1) * 256], start=True, stop=True,
                         R=["WcT", ("vm", tb, ct)], W=[("SBK", sl)])
            for gg in range(2):
                g = ct * 2 + gg
                if tb == "s":
                    P.I("dve", "scalar_tensor_tensor", out=vms[:, g * 256:(g + 1) * 256], in0=SBK[sl][0:16, gg * 256:(gg + 1) * 256], scalar=bsT[:, g:g + 1],
                                                                                          in1=ut[:16, gg * 256:(gg + 1) * 256], op0=ALU.add, op1=ALU.mult,
                         R=[("SBK", sl), "bsT", ("utmp", ti % 2)], W=[("vms", ct)])
                else:
                    P.I("dve", "scalar_tensor_tensor", out=vm[:, tb, g * 256:(g + 1) * 256], in0=SBK[sl][:, gg * 256:(gg + 1) * 256], scalar=bT[:, g:g + 1],
                                                                                                 in1=ut[:, gg * 256:(gg + 1) * 256], op0=ALU.add, op1=ALU.mult,
                         R=[("SBK", sl), "bT", ("utmp", ti % 2)], W=[("vm", tb, ct)])
    if debug:
        dbg["aout"] = do("dbg_aout", [128, 8, D], BF16)
        P.D("sp", out=dbg["aout"], in_=vm[:], R=[("vm", tb, c) for tb in range(8) for c in range(4)])
    gbase = 4096 + 3 * 6144
    for ct in range(4):
        wb = load_w(w_in, gbase + ct * 512)
        for ti, tb in enumerate(TB):
            st, npart = tok_block(tb)
            pj, pk = proj_block(wb, st, 1, npart)
            ut = utmp[ti % 2]
            P.I("act", "activation", out=ut[:npart], in_=pj[:npart], func=AF.Sigmoid, R=[("PJ", pk)], W=[("utmp", ti % 2)])
            if tb == "s":
                P.I("dve", "tensor_tensor", out=vms[:, ct * 512:(ct + 1) * 512], in0=ut[:16], in1=vms[:, ct * 512:(ct + 1) * 512], op=ALU.mult,
                     R=[("utmp", ti % 2), ("vms", ct)], W=[("vms", ct)])
            else:
                P.I("dve", "tensor_tensor", out=vm[:, tb, ct * 512:(ct + 1) * 512], in0=ut[:], in1=vm[:, tb, ct * 512:(ct + 1) * 512], op=ALU.mult,
                     R=[("utmp", ti % 2), ("vm", tb, ct)], W=[("vm", tb, ct)])
    for ct in range(4):
        wb = load_w(w_in, gbase + 2048 + ct * 512)
        for ti, tb in enumerate(TB):
            st, npart = tok_block(tb)
            pj, pk = proj_block(wb, st, 1, npart)
            ut = utmp[ti % 2]
            P.I("act", "activation", out=ut[:npart], in_=pj[:npart], func=AF.Sigmoid, R=[("PJ", pk)], W=[("utmp", ti % 2)])
            if tb == "s":
                P.I("dve", "tensor_tensor", out=bt[:16], in0=ut[:16], in1=bouts[:, ct * 512:(ct + 1) * 512], op=ALU.mult,
                     R=[("utmp", ti % 2)], W=["bt"])
                P.I("dve", "tensor_tensor", out=vms[:, ct * 512:(ct + 1) * 512], in0=bt[:16], in1=vms[:, ct * 512:(ct + 1) * 512], op=ALU.add,
                     R=["bt", ("vms", ct)], W=[("vms", ct)])
            else:
                og = ogt[ti % 2]
                P.D("sp", out=og[:], in_=OG[:, tb * 128:(tb + 1) * 128, ct * 4:(ct + 1) * 4, :].rearrange("g p h d -> p g h d"),
                      W=[("ogt", ti % 2)])
                P.I("dve", "tensor_tensor", out=num[:], in0=og[:, 0], in1=og[:, 1], op=ALU.add, R=[("ogt", ti % 2)], W=["num"])
                P.I("dve", "tensor_tensor", out=num[:], in0=num[:], in1=og[:, 2], op=ALU.add, R=[("ogt", ti % 2), "num"], W=["num"])
                P.I("dve", "reciprocal", out=rl[:], in_=num[:, :, 128], R=["num"], W=["rl"])
                P.I("dve", "tensor_tensor", out=bt[:].rearrange("p (h d) -> p h d", h=4), in0=num[:, :, 0:128], in1=rl[:, :, None].to_broadcast([128, 4, 128]), op=ALU.mult,
                     R=["num", "rl"], W=["bt"])
                P.I("dve", "tensor_tensor", out=bt[:], in0=bt[:], in1=ut[:], op=ALU.mult, R=["bt", ("utmp", ti % 2)], W=["bt"])
                P.I("dve", "tensor_tensor", out=vm[:, tb, ct * 512:(ct + 1) * 512], in0=bt[:], in1=vm[:, tb, ct * 512:(ct + 1) * 512], op=ALU.add,
                     R=["bt", ("vm", tb, ct)], W=[("vm", tb, ct)])
    if debug:
        dbg["merged"] = do("dbg_merged", [128, 8, D], BF16)
        P.D("sp", out=dbg["merged"], in_=vm[:], R=[("vm", tb, c) for tb in range(8) for c in range(4)])
        dbg["merged_s"] = do("dbg_merged_s", [16, D], BF16)
        P.D("sp", out=dbg["merged_s"], in_=vms[:], R=[("vms", c) for c in range(4)])
    for tb in range(8):
        P.D("sp", out=mgd[tb], in_=vm[:, tb, :], R=[("vm", tb, c) for c in range(4)], W=["mgd"])
    P.D("sp", out=mgd[8, 0:16], in_=vms[:], R=[("vms", c) for c in range(4)], W=["mgd"])
    P.end_phase()
    es_ab.close()
    if stop_after == "B2":
        return nc, P, dbg

    es_d = contextlib.ExitStack()
    acc = P.ssb(es_d, [128, 9, D], F32)
    es_e = contextlib.ExitStack()
    xn2T = P.ssb(es_e, [128, 16, 1040], BF16)
    es_c2 = contextlib.ExitStack()
    mT = P.ssb(es_c2, [128, 16, 1040], BF16)
    wbs2 = [P.ssb(es_c2, [128, 16, 512], BF16) for _ in range(2)]
    mrow = [P.psb([128, D], BF16) for _ in range(2)]
    tc1 = {"n": 0}

    def transpose_rows(src_rows, npart, dstT, col0, rtag="srcrows"):
        for c4 in range(4):
            half = tc1["n"] % 2
            tc1["n"] += 1
            for j in range(4):
                c = c4 * 4 + j
                P.I("pe", "transpose", out=PTs[half][:, j, :npart], in_=src_rows[:, c * 128:(c + 1) * 128], identity=identb[:npart, :npart],
                    R=[rtag, "identb"], W=[("PT", half)])
            if c4 % 2 == 0:
                P.I("act", "copy", out=dstT[:, c4 * 4:(c4 + 1) * 4, col0:col0 + npart], in_=PTs[half][:, 0:4, :npart], R=[("PT", half)], W=["dstT"])
            else:
                P.I("dve", "tensor_copy", out=dstT[:, c4 * 4:(c4 + 1) * 4, col0:col0 + npart], in_=PTs[half][:, 0:4, :npart], R=[("PT", half)], W=["dstT"])

    for tb in range(9):
        npart = 128 if tb < 8 else 16
        mr = mrow[tb % 2]
        P.D("sp", out=mr[:npart], in_=mgd[tb, 0:npart], W=[("mrow", tb % 2)])
        transpose_rows(mr[:npart, :], npart, mT, tb * 128, rtag=("mrow", tb % 2))
    P.end_phase()

    xres = [P.psb([128, 512], F32) for _ in range(2)]
    ssC = P.psb([128, 9], F32)
    rsC = P.psb([128, 9], F32)
    gBffn = P.psb([128, D], F32)
    xnbC = {"sq": P.psb([128, D], BF16), "b": [P.psb([128, D], BF16) for _ in range(2)]}
    P.D("sp", out=gBffn[:], in_=g_ffn.partition_broadcast(128), W=["gB"])
    wn = {"n": 0}
    xr = 0
    for ct in range(4):
        wb = wbs2[wn["n"] % 2]
        wn["n"] += 1
        P.D("pool", out=wb[:], in_=w_out[:, ct * 512:(ct + 1) * 512].rearrange("(c p) n -> p c n", p=128), W=[("wb", id(wb))])
        for tb in range(9):
            npart = 128 if tb < 8 else 16
            k = pjn["n"] % 2
            pjn["n"] += 1
            pj = PJ[k]
            for dc in range(16):
                P.I("pe", "matmul", pj[:npart, :], lhsT=mT[:, dc, tb * 128:tb * 128 + npart], rhs=wb[:, dc, :],
                                                                                       start=(dc == 0), stop=(dc == 15),
                     R=[("wb", id(wb))], W=[("PJ", k)])
            xrt = xres[xr % 2]
            src = xp[1024 + tb * 128:1024 + (tb + 1) * 128, ct * 512:(ct + 1) * 512] if tb < 8 else xs[:, ct * 512:(ct + 1) * 512]
            P.D("sp", out=xrt[:npart], in_=src, W=[("xres", xr % 2)])
            P.I("dve", "tensor_tensor", out=acc[:npart, tb, ct * 512:(ct + 1) * 512], in0=pj[:npart], in1=xrt[:npart], op=ALU.add,
                 R=[("PJ", k), ("xres", xr % 2)], W=[("acc", tb, ct)])
            xr += 1
    if debug:
        dbg["h"] = do("dbg_h", [128, 9, D])
        P.D("sp", out=dbg["h"][:, 0:8], in_=acc[:, 0:8], R=[("acc", tb, c) for tb in range(8) for c in range(4)])
        P.D("sp", out=dbg["h"][0:16, 8], in_=acc[0:16, 8], R=[("acc", 8, c) for c in range(4)])
    tcount["n"] = 0
    for tb in range(9):
        npart = 128 if tb < 8 else 16
        k = tb
        ss = ssC[:, tb:tb + 1]
        rs = rsC[:, tb:tb + 1]
        sq = xnbC["sq"]
        rt = [("acc", tb, c) for c in range(4)]
        P.I("act", "activation", out=sq[:npart], in_=acc[:npart, tb, :], func=AF.Square, accum_out=ss[:npart], R=rt, W=["sqj", ("ss", k)])
        P.I("act", "activation", out=rs[:npart], in_=ss[:npart], func=AF.Sqrt, scale=1.0 / D, bias=EPS, R=[("ss", k)], W=[("rs", k)])
        P.I("dve", "reciprocal", out=rs[:npart], in_=rs[:npart], R=[("rs", k)], W=[("rs", k)])
        xb = xnbC["b"][k % 2]
        P.I("dve", "scalar_tensor_tensor", out=xb[:npart], in0=acc[:npart, tb, :], scalar=rs[:npart, 0:1], in1=gBffn[:npart], op0=ALU.mult, op1=ALU.mult,
             R=rt + [("rs", k), "gB"], W=["srcrows"])
        transpose_rows(xb[:npart, :], npart, xn2T, tb * 128)
    P.end_phase()
    es_c2.close()
    if stop_after == "C":
        return nc, P, dbg

    es_q = contextlib.ExitStack()
    qTp = P.ssb(es_q, [128, 16, 1040], BF16)
    kTs = P.ssb(es_q, [128, 2, 128], BF16)
    wq = [P.psb([128, 16, 512], BF16) for _ in range(2)]
    skl = P.psb([128, 128], F32)
    for c in range(2):
        P.D("sp", out=skl[:], in_=subk[c], W=["skl"])
        P.I("pe", "transpose", out=SBK[0][:, 0:128], in_=skl[:], identity=identf[:], R=["skl", "identf"], W=[("SBK", 0)])
        P.I("dve", "tensor_copy", out=kTs[:, c, :], in_=SBK[0][:, 0:128], R=[("SBK", 0)], W=["kTs"])
    TT = [(0, 512), (512, 512), (1024, 16)]
    for ct in range(4):
        wb = wq[ct % 2]
        P.D("pool", out=wb[:], in_=w_q[:, ct * 512:(ct + 1) * 512].rearrange("(c p) n -> p c n", p=128), W=[("wb", id(wb))])
        for j in range(4):
            hc = ct * 4 + j
            for (t0, tn_) in TT:
                k = pjn["n"] % 2
                pjn["n"] += 1
                pj = PJ[k]
                for dc in range(16):
                    P.I("pe", "matmul", pj[:, 0:tn_], lhsT=wb[:, dc, j * 128:(j + 1) * 128], rhs=xn2T[:, dc, t0:t0 + tn_],
                                                                                          start=(dc == 0), stop=(dc == 15),
                         R=[("wb", id(wb)), "dstT"], W=[("PJ", k)])
                P.I("act", "copy", out=qTp[:, hc, t0:t0 + tn_], in_=pj[:, 0:tn_], R=[("PJ", k)], W=["qTp"])
    P.end_phase()

    s_sb = P.psb([128, 16, 128], F32)
    t16 = P.psb([128, 2, 16], F32)
    tmp128 = P.psb([128, 128], F32)
    cand = P.psb([128, 256], F32)
    cand2 = P.psb([128, 256], F32)
    tc16 = P.psb([128, 16], F32)
    etmp = P.psb([128, 16], F32)
    thr = P.psb([128, 8], F32)
    c0 = P.psb([128, 8], F32)
    zs = P.psb([128, 8], F32)
    NQ = 16
    RQ = 128 // NQ
    At = [P.psb([128, RQ, 128], F32) for _ in range(2)]
    Et = [P.psb([128, RQ, 128], F32) for _ in range(2)]
    Gh = [P.psb([128, RQ, 128], F32) for _ in range(2)]
    Gacc = P.psb([128, RQ, 128], F32)
    Gq = [P.psb([128, RQ * 128], BF16) for _ in range(2)]
    gq = 0
    for tb in range(9):
        npart = 128 if tb < 8 else 16
        for hc4 in range(4):
            for j in range(4):
                hc = hc4 * 4 + j
                bank = [SBK[0], SBK[1], OB[0], OB[1]][hc4]
                P.I("pe", "matmul", bank[:npart, j * 128:(j + 1) * 128], lhsT=qTp[:, hc, tb * 128:tb * 128 + npart], rhs=kTs[:, hc % 2, :],
                                                                                         start=True, stop=True,
                     R=["qTp", "kTs"], W=[("SC", hc4)])
            bank = [SBK[0], SBK[1], OB[0], OB[1]][hc4]
            P.I("act", "copy", out=s_sb[:npart, hc4 * 4:(hc4 + 1) * 4, :], in_=bank[:npart, :].rearrange("p (a k) -> p a k", a=4),
                 R=[("SC", hc4)], W=[("s_sb", hc4)])
        for h in range(8):
            srd = [("s_sb", h // 2)]
            for c in range(2):
                sv = s_sb[:npart, 2 * h + c, :]
                P.I("dve", "max", out=t16[:npart, c, 0:8], in_=sv, R=srd, W=["t16"])
                P.I("dve", "match_replace", out=tmp128[:npart], in_to_replace=t16[:npart, c, 0:8], in_values=sv, imm_value=-BIG,
                     R=srd + ["t16"], W=["tmp128"])
                P.I("dve", "max", out=t16[:npart, c, 8:16], in_=tmp128[:npart], R=["tmp128"], W=["t16"])
            P.I("dve", "tensor_tensor", out=cand[:npart].rearrange("p (a b) -> p a b", a=16), in0=t16[:npart, 0, :, None].to_broadcast([npart, 16, 16]),
                                                              in1=t16[:npart, 1, None, :].to_broadcast([npart, 16, 16]), op=ALU.add,
                 R=["t16"], W=["cand"])
            P.I("dve", "max", out=tc16[:npart, 0:8], in_=cand[:npart], R=["cand"], W=["tc16"])
            P.I("dve", "match_replace", out=cand2[:npart], in_to_replace=tc16[:npart, 0:8], in_values=cand[:npart], imm_value=-BIG,
                 R=["cand", "tc16"], W=["cand2"])
            P.I("dve", "max", out=tc16[:npart, 8:16], in_=cand2[:npart], R=["cand2"], W=["tc16"])
            P.I("dve", "tensor_copy", out=thr[:npart, h:h + 1], in_=tc16[:npart, 15:16], R=["tc16"], W=[("thr", h)])
            P.I("dve", "tensor_scalar", out=c0[:npart, h:h + 1], in0=tc16[:npart, 0:1], scalar1=-1.0, scalar2=None, op0=ALU.mult,
                 R=["tc16"], W=[("c0", h)])
            P.I("act", "activation", out=etmp[:npart], in_=tc16[:npart], func=AF.Exp, bias=c0[:npart, h:h + 1], scale=1.0, accum_out=zs[:npart, h:h + 1],
                 R=["tc16", ("c0", h)], W=["etmp", ("zs", h)])
            P.I("act", "activation", out=zs[:npart, h:h + 1], in_=zs[:npart, h:h + 1], func=AF.Ln, R=[("zs", h)], W=[("zs", h)])
            P.I("dve", "tensor_tensor", out=c0[:npart, h:h + 1], in0=c0[:npart, h:h + 1], in1=zs[:npart, h:h + 1], op=ALU.subtract,
                 R=[("c0", h), ("zs", h)], W=[("c0", h)])
        for qi in range(NQ):
            for h in range(8):
                sl = h % 2
                A = At[sl]
                E = Et[sl]
                G_ = Gh[sl]
                s1 = s_sb[:npart, 2 * h, qi * RQ:(qi + 1) * RQ]
                s2 = s_sb[:npart, 2 * h + 1, :]
                P.I("pool", "tensor_tensor", out=A[:npart], in0=s1[:, :, None].to_broadcast([npart, RQ, 128]),
                                                                                      in1=s2[:, None, :].to_broadcast([npart, RQ, 128]), op=ALU.add,
                     R=[("s_sb", h // 2)], W=[("At", sl)])
                P.I("act", "activation", out=E[:npart], in_=A[:npart], func=AF.Exp, bias=c0[:npart, h:h + 1], scale=1.0,
                     R=[("At", sl), ("c0", h)], W=[("Et", sl)])
                if h == 0:
                    P.I("dve", "scalar_tensor_tensor", out=Gacc[:npart], in0=A[:npart], scalar=thr[:npart, h:h + 1], in1=E[:npart], op0=ALU.is_ge, op1=ALU.mult,
                         R=[("At", sl), ("Et", sl), ("thr", h)], W=["Gacc"])
                else:
                    P.I("dve", "scalar_tensor_tensor", out=G_[:npart], in0=A[:npart], scalar=thr[:npart, h:h + 1], in1=E[:npart], op0=ALU.is_ge, op1=ALU.mult,
                         R=[("At", sl), ("Et", sl), ("thr", h)], W=[("Gh", sl)])
                    if h < 7:
                        P.I("dve", "tensor_tensor", out=Gacc[:npart], in0=Gacc[:npart], in1=G_[:npart], op=ALU.add,
                             R=[("Gh", sl), "Gacc"], W=["Gacc"])
                    else:
                        gb = Gq[gq % 2]
                        P.I("dve", "tensor_tensor", out=gb[:npart].rearrange("p (a b) -> p a b", a=RQ), in0=Gacc[:npart], in1=G_[:npart], op=ALU.add,
                             R=[("Gh", sl), "Gacc"], W=[("Gq", gq % 2)])
                        P.D("sp", out=Gd[tb, 0:npart, qi * RQ * 128:(qi + 1) * RQ * 128], in_=gb[:npart],
                              R=[("Gq", gq % 2)], W=["Gd"])
                        gq += 1
    P.end_phase()
    es_q.close()
    if stop_after == "D1":
        return nc, P, dbg

    EG = 256
    NG = NEXP // EG
    NCH = EG // 128
    ub = [P.psb([128, NCH, D], BF16) for _ in range(2)]
    vb = [P.psb([128, NCH, D], BF16) for _ in range(2)]
    uT = [P.psb([128, 16, EG], BF16) for _ in range(2)]
    Gt = [P.psb([128, 9, EG], BF16) for _ in range(2)]
    gt32 = [P.psb([128, EG], F32) for _ in range(2)]
    actb = [P.psb([128, EG], BF16) for _ in range(2)]
    actT = [P.psb([128, NCH, 128], BF16) for _ in range(2)]
    o2b = [SBK[0], SBK[1], OB[0], OB[1]]
    it = 0
    for eg in range(NG):
        sl = eg % 2
        for c in range(NCH):
            P.D("pool", out=ub[sl][:, c, :], in_=pu[eg * EG + c * 128:eg * EG + (c + 1) * 128, :], W=[("ub", sl)], max_dma_last_dim=4096)
        for c in range(NCH):
            P.D("pool", out=vb[sl][:, c, :], in_=pv[eg * EG + c * 128:eg * EG + (c + 1) * 128, :], W=[("vb", sl)], max_dma_last_dim=4096)
        P.D("sp", out=Gt[sl][:, 0:8, :], in_=Gd[0:8, :, eg * EG:(eg + 1) * EG].rearrange("t p e -> p t e"), R=["Gd"], W=[("Gt", sl)])
        P.D("sp", out=Gt[sl][0:16, 8, :], in_=Gd[8, 0:16, eg * EG:(eg + 1) * EG], R=["Gd"], W=[("Gt", sl)])
        for c in range(NCH):
            for d4 in range(4):
                half = (c * 4 + d4) % 2
                for j in range(4):
                    dc = d4 * 4 + j
                    P.I("pe", "transpose", out=PTs[half][:, j, :], in_=ub[sl][:, c, dc * 128:(dc + 1) * 128], identity=identb[:],
                         R=[("ub", sl), "identb"], W=[("PT", half)])
                if d4 % 2 == 0:
                    P.I("act", "copy", out=uT[sl][:, d4 * 4:(d4 + 1) * 4, c * 128:(c + 1) * 128], in_=PTs[half][:, 0:4, :],
                         R=[("PT", half)], W=[("uT", sl)])
                else:
                    P.I("dve", "tensor_copy", out=uT[sl][:, d4 * 4:(d4 + 1) * 4, c * 128:(c + 1) * 128], in_=PTs[half][:, 0:4, :],
                         R=[("PT", half)], W=[("uT", sl)])
        for tb in range(9):
            npart = 128 if tb < 8 else 16
            k = pjn["n"] % 2
            pjn["n"] += 1
            pj = PJ[k]
            s2 = it % 2
            it += 1
            for dc in range(16):
                P.I("pe", "matmul", pj[:npart, 0:EG], lhsT=xn2T[:, dc, tb * 128:tb * 128 + npart], rhs=uT[sl][:, dc, :],
                                                                                       start=(dc == 0), stop=(dc == 15),
                     R=[("uT", sl)], W=[("PJ", k)])
            P.I("act", "activation", out=gt32[s2][:npart], in_=pj[:npart, 0:EG], func=AF.Gelu_apprx_tanh, R=[("PJ", k)], W=[("gt32", s2)])
            P.I("dve", "tensor_tensor", out=actb[s2][:npart], in0=gt32[s2][:npart], in1=Gt[sl][:npart, tb, :], op=ALU.mult,
                 R=[("gt32", s2), ("Gt", sl)], W=[("actb", s2)])
            for c in range(NCH):
                P.I("pe", "transpose", out=PTs[s2][:, c, :npart], in_=actb[s2][:npart, c * 128:(c + 1) * 128], identity=identb[:npart, :npart],
                     R=[("actb", s2), "identb"], W=[("PT", s2)])
            P.I("act", "copy", out=actT[s2][:, :, :npart], in_=PTs[s2][:, 0:NCH, :npart], R=[("PT", s2)], W=[("actT", s2)])
            for dt_ in range(4):
                for c in range(NCH):
                    P.I("pe", "matmul", o2b[dt_][:npart, :], lhsT=actT[s2][:, c, :npart], rhs=vb[sl][:, c, dt_ * 512:(dt_ + 1) * 512],
                                                                                         start=(c == 0), stop=(c == NCH - 1),
                         R=[("actT", s2), ("vb", sl)], W=[("O2", dt_)])
            for dt_ in range(4):
                P.I("dve", "tensor_tensor", out=acc[:npart, tb, dt_ * 512:(dt_ + 1) * 512], in0=o2b[dt_][:npart, :], in1=acc[:npart, tb, dt_ * 512:(dt_ + 1) * 512], op=ALU.add,
                     R=[("O2", dt_)], W=[("O2", dt_), ("accD", tb, dt_)])
    P.end_phase()
    es_e.close()

    gBfin = P.psb([128, D], F32)
    sqE = P.psb([128, D], F32)
    outE = [P.psb([128, D], F32) for _ in range(2)]
    P.D("sp", out=gBfin[:], in_=g_fin.partition_broadcast(128), W=["gBfin"])
    for tb in range(9):
        npart = 128 if tb < 8 else 16
        ss = P.psb([128, 1], F32)
        rs = P.psb([128, 1], F32)
        ot = outE[tb % 2]
        P.I("act", "activation", out=sqE[:npart], in_=acc[:npart, tb, :], func=AF.Square, accum_out=ss[:npart], W=["sqE", ("ssE", tb)])
        P.I("act", "activation", out=rs[:npart], in_=ss[:npart], func=AF.Sqrt, scale=1.0 / D, bias=EPS, R=[("ssE", tb)], W=[("rsE", tb)])
        P.I("dve", "reciprocal", out=rs[:npart], in_=rs[:npart], R=[("rsE", tb)], W=[("rsE", tb)])
        P.I("dve", "scalar_tensor_tensor", out=ot[:npart], in0=acc[:npart, tb, :], scalar=rs[:npart, 0:1], in1=gBfin[:npart], op0=ALU.mult, op1=ALU.mult,
             R=[("rsE", tb), "gBfin"], W=[("outE", tb % 2)])
        dst = yp[tb * 128:(tb + 1) * 128, :] if tb < 8 else ys
        P.D("sp", out=dst, in_=ot[:npart], R=[("outE", tb % 2)])
    P.end_phase()
    es_d.close()
    return nc, P, dbg


def make_in_maps(inputs):
    f = lambda a: np.ascontiguousarray(np.asarray(a, dtype=np.float32))
    xpr = f(inputs["x_prompt"])
    xsm = f(inputs["x_sample"])
    shared = dict(
        w_in=f(inputs["w_in"][0]), w_out=f(inputs["w_out"][0]), w_q=f(inputs["peer_w_q"][0]), subk=f(inputs["peer_sub_keys"][0]),
        pu=f(inputs["peer_u"][0]), pv=f(inputs["peer_v"][0]),
        g_mix=f(inputs["norm_mix_g"][0]).reshape(1, D), g_sgu=f(inputs["sgu_norm_g"][0]).reshape(1, D),
        g_ffn=f(inputs["norm_ffn_g"][0]).reshape(1, D), g_fin=f(inputs["norm_final_g"]).reshape(1, D),
        sgu_w=f(inputs["sgu_w"][0]), sgu_b=f(inputs["sgu_b"][0]),
    )
    c128 = f(inputs["cache_kv_w128"][0]).reshape(32, 128, 2, D)
    c512 = f(inputs["cache_kv_w512"][0]).reshape(32, 512, 2, D)
    c2048 = f(inputs["cache_kv_w2048"][0]).reshape(32, 2048, 2, D)
    maps = []
    for c in range(8):
        b, th = c // 2, c % 2
        xpc = np.zeros((2048, D), np.float32)
        if th == 1:
            xpc[:] = xpr[b]
        else:
            xpc[1024:] = xpr[b, :1024]
        m = dict(shared)
        m.update(const_tables(th))
        m["xp"] = xpc
        m["xs"] = np.ascontiguousarray(xsm[4 * c:4 * c + 4].reshape(16, D))
        m["ck128"] = np.ascontiguousarray(c128[4 * c:4 * c + 4])
        m["ck512"] = np.ascontiguousarray(c512[4 * c:4 * c + 4])
        m["ck2048"] = np.ascontiguousarray(c2048[4 * c:4 * c + 4])
        maps.append(m)
    return maps


def assemble(results):
    y_prompt = np.zeros((4, 2048, D), np.float32)
    y_sample = np.zeros((32, 4, D), np.float32)
    kvp = [np.zeros((1, 4, w, 2, NH, DH), np.float32) for w in (128, 512, 2048)]
    kvs = [np.zeros((1, 32, 4, 2, NH, DH), np.float32) for _ in range(3)]
    sguv = np.zeros((1, 32, 4, D), np.float32)
    for c in range(8):
        r = results[c]
        b, th = c // 2, c % 2
        y_prompt[b, th * 1024:(th + 1) * 1024] = r["yp"]
        y_sample[4 * c:4 * c + 4] = r["ys"].reshape(4, 4, D)
        if th == 1:
            kvp[0][0, b] = r["kvp128"].reshape(128, 2, NH, DH)
            kvp[1][0, b] = r["kvp512"].reshape(512, 2, NH, DH)
        kvp[2][0, b, th * 1024:(th + 1) * 1024] = r["kvp2048"].reshape(1024, 2, NH, DH)
        for g, nm in enumerate(("kvs128", "kvs512", "kvs2048")):
            kvs[g][0, 4 * c:4 * c + 4] = r[nm].reshape(4, 4, 2, NH, DH)
        sguv[0, 4 * c:4 * c + 4] = r["sguv"].reshape(4, 4, D)
    return (y_prompt, y_sample, kvp[0], kvp[1], kvp[2], kvs[0], kvs[1], kvs[2], sguv)


_CACHE = {}


def kernel(**inputs):
    if "nc" not in _CACHE:
        _CACHE["nc"] = build()[0]
    nc = _CACHE["nc"]
    maps = make_in_maps(inputs)
    res = run_bass_kernel_spmd(nc, maps, core_ids=list(range(8)))
    return assemble(res.results)
```

```python
import contextlib
import math
import numpy as np
import concourse.bass as bass
import concourse.mybir as mybir
from concourse.bass_utils import run_bass_kernel_spmd

F32 = mybir.dt.float32
BF16 = mybir.dt.bfloat16
AF = mybir.ActivationFunctionType
ALU = mybir.AluOpType
AX = mybir.AxisListType

ENGS = ("pe", "act", "dve", "pool", "sp")
PSUM_RES = ("PJ", "SBK", "OB", "PT", "SC", "O2", "OBS")
NDSEM = 12

D = 2048
NH = 16
DH = 128
IN_COLS = 26624
SOFF = 2048
TOKS = 2048 + 16
BIG = 1.0e30
SCALE = DH ** -0.5
SQ = math.sqrt(DH)
EPS = 1e-6
DIL = (1, 4, 16)
NEXP = 16384


def alibi():
    e = np.arange(1, 49, dtype=np.float64)
    return np.exp2(-8.0 * e / 48).reshape(3, 16)


SLOPES = alibi()
A_BLOCKS = list(range(17))
B1_BUNDLES = [(hq, g) for hq in range(4) for g in range(3)]
B1_CUT = 99
import os
KNOB = os.environ.get('KNOB', '')
DBG_F32 = False


class Op:
    __slots__ = ("eng", "fn", "waits", "ticket", "needed", "is_dma", "idx")


class Prog:
    def __init__(self, nc):
        self.nc = nc
        self.es = contextlib.ExitStack()
        self.pes = contextlib.ExitStack()
        self.ops = {e: [] for e in ENGS}
        self.res = {}
        self.allops = []
        self.sem = {e: self.es.enter_context(nc.semaphore("s_" + e)) for e in ENGS}
        self.dsem = {}
        self.dcount = {}
        for q in ("sp", "act", "pool"):
            self.dsem[q] = [self.es.enter_context(nc.semaphore("d_%s%d" % (q, i))) for i in range(NDSEM)]
            self.dcount[q] = 0
        self.waited = {e: {} for e in ENGS}
        self.n_sb = 0
        self.cnt = {e: 0 for e in ENGS}
        self.dlast = {q: [None] * NDSEM for q in self.dsem}
        self.emitted = 0
        self.bar_start = 0
        self.ninstr = 0

    def _alloc(self, stack, shape, dt, psum=False):
        self.n_sb += 1
        name = "t%d" % self.n_sb
        if psum:
            return stack.enter_context(self.nc.psum_tensor(name, list(shape), dt))
        return stack.enter_context(self.nc.sbuf_tensor(name, list(shape), dt))

    def sb(self, shape, dt):
        return self._alloc(self.es, shape, dt)

    def psb(self, shape, dt):
        return self._alloc(self.pes, shape, dt)

    def ssb(self, stack, shape, dt):
        return self._alloc(stack, shape, dt)

    def ps(self, shape, dt):
        return self._alloc(self.es, shape, dt, psum=True)

    def _deps(self, reads, writes, eng=None):
        deps = []
        for r in reads:
            st = self.res.get(r)
            if st is not None and st[0] is not None:
                deps.append(st[0])
            if st is not None and isinstance(r, tuple) and r[0] in PSUM_RES:
                deps.extend(x for x in st[1] if x.eng != eng)
        for w in writes:
            st = self.res.get(w)
            if st is not None:
                if st[0] is not None:
                    deps.append(st[0])
                deps.extend(st[1])
        return deps

    def _commit(self, op, reads, writes):
        for r in reads:
            st = self.res.setdefault(r, [None, []])
            st[1].append(op)
        for w in writes:
            self.res[w] = [op, []]

    def op(self, eng, fn, reads=(), writes=()):
        o = Op()
        o.eng = eng
        o.fn = fn
        o.is_dma = False
        o.needed = False
        deps = self._deps(reads, writes, eng)
        o.waits = [d for d in deps if not (eng == "pe" and d.eng == "pe" and not d.is_dma)]
        for d in o.waits:
            d.needed = True
        self._commit(o, reads, writes)
        self.ops[eng].append(o)
        self.allops.append(o)
        return o

    def dma(self, q, fn, reads=(), writes=()):
        o = Op()
        o.eng = q
        o.fn = fn
        o.is_dma = True
        o.needed = True
        o.idx = self.dcount[q]
        self.dcount[q] += 1
        o.waits = list(self._deps(reads, writes))
        for d in o.waits:
            d.needed = True
        self._commit(o, reads, writes)
        self.ops[q].append(o)
        self.allops.append(o)
        return o

    def I(self, eng, meth, *args, R=(), W=(), **kw):
        return self.op(eng, (meth, args, kw), R, W)

    def D(self, q, out, in_, R=(), W=(), **kw):
        return self.dma(q, ("dma_start", (), dict(out=out, in_=in_, **kw)), R, W)

    def barrier(self):
        lastc = {}
        dm = {q: [] for q in self.dsem}
        for o in self.allops[self.bar_start:]:
            if o.fn is None:
                continue
            if o.is_dma:
                dm[o.eng].append(o)
            else:
                lastc[o.eng] = o
        self.bar_start = len(self.allops)
        deps = list(lastc.values())
        for q in dm:
            deps.extend(dm[q][-NDSEM:])
        for e in ENGS:
            o = Op()
            o.eng = e
            o.fn = None
            o.is_dma = False
            o.needed = False
            o.waits = [d for d in deps if not (e == "pe" and d.eng == "pe" and not d.is_dma)]
            for d in o.waits:
                d.needed = True
            self.ops[e].append(o)
            self.allops.append(o)
        self.res = {}

    def end_phase(self):
        self.barrier()
        self.emit()
        self.pes.close()
        self.pes = contextlib.ExitStack()

    def emit(self):
        nc = self.nc
        cnt = self.cnt
        dlast = self.dlast
        new_ops = self.allops[self.emitted:]
        self.emitted = len(self.allops)
        for o in new_ops:
            if o.is_dma:
                q = o.eng
                k = o.idx % NDSEM
                o.ticket = (("d", q, k), 16 * (o.idx // NDSEM + 1))
                prev = dlast[q][k]
                if prev is not None:
                    o.waits.append(prev)
                dlast[q][k] = o
            elif o.needed and o.fn is not None:
                cnt[o.eng] += 1
                o.ticket = (("c", o.eng), cnt[o.eng])
            else:
                o.ticket = None

        def semof(key):
            if key[0] == "c":
                return self.sem[key[1]]
            return self.dsem[key[1]][key[2]]

        def run(ename, eng):
            waited = self.waited[ename]
            ops = self.ops[ename]
            self.ops[ename] = []
            for o in ops:
                need = {}
                for d in o.waits:
                    key, val = d.ticket
                    if waited.get(key, 0) >= val:
                        continue
                    if need.get(key, 0) < val:
                        need[key] = val
                for key, val in need.items():
                    eng.wait_ge(semof(key), val)
                    waited[key] = val
                    self.ninstr += 1
                if o.fn is None:
                    continue
                if callable(o.fn):
                    ins = o.fn(eng)
                else:
                    meth, args, kw = o.fn
                    ins = getattr(eng, meth)(*args, **kw)
                self.ninstr += 1
                if o.ticket is not None:
                    key, val = o.ticket
                    ins.then_inc(semof(key), 16 if o.is_dma else 1)

        with nc.Block() as block:
            @block.tensor
            def _(e):
                run("pe", e)

            @block.scalar
            def _(e):
                run("act", e)

            @block.vector
            def _(e):
                run("dve", e)

            @block.gpsimd
            def _(e):
                run("pool", e)

            @block.sync
            def _(e):
                run("sp", e)


def const_tables(th):
    c = {}
    c["ident"] = np.eye(128, dtype=np.float32)
    j = np.arange(128)[:, None]
    i = np.arange(128)[None, :]
    bprev = np.where(i <= j, -(i - j + 128.0), -BIG)
    bcur = np.where(i >= j, -(i - j + 0.0), -BIG)
    c["btab"] = np.concatenate([bprev, bcur], axis=1).astype(np.float32)
    c["cmask"] = (j <= i).astype(np.float32)
    hbv = 0.0 if th == 1 else -BIG
    hb = np.zeros((128, 4), np.float32)
    hb[:, 0] = hbv
    hb[:64, 1] = hbv
    c["hb"] = hb
    st = np.full((128, 12, 16, 4), -BIG, np.float64)
    row = np.arange(128)
    for h in range(16):
        for q in range(4):
            d = 128 + q - row
            st[:, 0, h, q] = np.where(row >= q, -SLOPES[0, h] * d, -BIG)
            for r in range(4):
                if q == r:
                    st[:, 1 + r, h, q] = -SLOPES[1, h] * 4.0 * (128 - row)
                    st[:, 5 + r, h, q] = -SLOPES[2, h] * 16.0 * (128 - row)
            for kp in range(4):
                if kp <= q:
                    st[kp, 9, h, q] = -SLOPES[0, h] * (q - kp)
                if kp == q:
                    st[kp, 10, h, q] = 0.0
                    st[kp, 11, h, q] = 0.0
    c["stab"] = st.reshape(128, 12, 64).astype(np.float32)
    dm = np.zeros((16, 16, 16), np.float32)
    for p in range(16):
        dm[p, :, p] = 1.0
    c["dmask"] = dm.reshape(16, 256)
    cms = np.zeros((16, 16), np.float32)
    for b in range(4):
        for s in range(4):
            for t in range(4):
                if s <= t:
                    cms[4 * b + s, 4 * b + t] = 1.0
    c["cms"] = cms
    return c


def build(stop_after=None, debug=False):
    nc = bass.Bass("TRN2", target_bir_lowering=False)

    def di(n, s, dt=F32):
        return nc.dram_tensor(n, list(s), dt, kind="ExternalInput").ap()

    def do(n, s, dt=F32):
        return nc.dram_tensor(n, list(s), dt, kind="ExternalOutput").ap()

    def dscr(n, s, dt=F32):
        return nc.dram_tensor(n, list(s), dt, kind="Internal").ap()

    xp = di("xp", [2048, D])
    xs = di("xs", [16, D])
    if stop_after == "A":
        _di = di
        di = lambda n, s, dt=F32: (_di(n, s, dt) if n in ("g_mix", "ident") else None)
        dscr = lambda n, s, dt=F32: None
    w_in = di("w_in", [D, IN_COLS])
    w_out = di("w_out", [D, D])
    w_q = di("w_q", [D, D])
    subk = di("subk", [2, 128, 128])
    pu = di("pu", [NEXP, D])
    pv = di("pv", [NEXP, D])
    g_mix = di("g_mix", [1, D])
    g_sgu = di("g_sgu", [1, D])
    g_ffn = di("g_ffn", [1, D])
    g_fin = di("g_fin", [1, D])
    sgu_w = di("sgu_w", [8, 128, 128])
    sgu_b = di("sgu_b", [8, 128])
    ck = [di("ck128", [4, 128, 2, D]), di("ck512", [4, 512, 2, D]), di("ck2048", [4, 2048, 2, D])]
    ident_d = di("ident", [128, 128])
    btab_d = di("btab", [128, 256])
    cmask_d = di("cmask", [128, 128])
    hb_d = di("hb", [128, 4])
    stab_d = di("stab", [128, 12, 64])
    dmask_d = di("dmask", [16, 256])
    cms_d = di("cms", [16, 16])

    yp = do("yp", [1024, D])
    ys = do("ys", [16, D])
    kvp = [do("kvp128", [128, 2, D]), do("kvp512", [512, 2, D]), do("kvp2048", [1024, 2, D])]
    kvs = [do("kvs128", [16, 2, D]), do("kvs512", [16, 2, D]), do("kvs2048", [16, 2, D])]
    sguv = do("sguv", [16, D])

    OG = dscr("OG", [3, 1024, 16, 129])
    sqd = dscr("sqd", [3, 3, 16, D])
    Gd = dscr("Gd", [9, 128, NEXP], BF16)
    mgd = dscr("mgd", [9, 128, D], BF16)
    dbg = {}

    P = Prog(nc)
    PJ = [P.ps([128, 512], F32) for _ in range(2)]
    SBK = [P.ps([128, 512], F32) for _ in range(2)]
    OB = [P.ps([128, 512], F32) for _ in range(2)]
    PTs = [P.ps([128, 8, 128], BF16) for _ in range(2)]

    identf = P.sb([128, 128], F32)
    identb = P.sb([128, 128], BF16)
    onesb = P.sb([128, 16], BF16)
    zcol = P.sb([128, 1], F32)

    P.D("sp", out=identf[:], in_=ident_d, W=["identf"])
    P.I("dve", "tensor_copy", out=identb[:], in_=identf[:], R=["identf"], W=["identb"])
    P.I("dve", "memset", onesb[:], 1.0, W=["onesb"])
    P.I("dve", "memset", zcol[:], 0.0, W=["zcol"])

    bouts = P.sb([16, D], F32)
    es_ab = contextlib.ExitStack()
    xnT = P.ssb(es_ab, [128, 16, TOKS], BF16)
    wbs = [P.ssb(es_ab, [128, 16, 512], BF16) for _ in range(2)]
    wstate = {"n": 0}
    tcount = {"n": 0}

    def load_w(src, col0, extra=None):
        bufs = wbs + ([extra] if extra is not None else [])
        k = wstate["n"] % len(bufs)
        wstate["n"] += 1
        wb = bufs[k]
        P.D("pool", out=wb[:], in_=src[:, col0:col0 + 512].rearrange("(c p) n -> p c n", p=128),
              W=[("wb", id(wb))])
        return wb

    def rmsnorm_T(xt, npart, gB, xnb, dstT, col0, tagp):
        k = tcount["n"]
        tcount["n"] += 1
        ss = P.psb([128, 1], F32)
        rs = P.psb([128, 1], F32)
        sq = xnb["sq"]
        P.I("act", "activation", out=sq[:npart], in_=xt[:npart], func=AF.Square, accum_out=ss[:npart],
             R=[tagp + "xt"], W=["sqj", ("ss", k)])
        P.I("act", "activation", out=rs[:npart], in_=ss[:npart], func=AF.Sqrt, scale=1.0 / D, bias=EPS,
             R=[("ss", k)], W=[("rs", k)])
        P.I("dve", "reciprocal", out=rs[:npart], in_=rs[:npart], R=[("rs", k)], W=[("rs", k)])
        xb = xnb["b"][k % 2]
        P.I("dve", "scalar_tensor_tensor", out=xb[:npart], in0=xt[:npart], scalar=rs[:npart, 0:1], in1=gB[:npart],
                                                     op0=ALU.mult, op1=ALU.mult,
             R=[tagp + "xt", ("rs", k), "gB"], W=[("xnb", k % 2)])
        for c4 in range(4):
            half = (k * 4 + c4) % 2
            for j in range(4):
                c = c4 * 4 + j
                P.I("pe", "transpose", out=PTs[half][:, j, :npart], in_=xb[:npart, c * 128:(c + 1) * 128],
                                                                       identity=identb[:npart, :npart],
                     R=[("xnb", k % 2), "identb"], W=[("PT", half)])
            eng = "act" if c4 % 2 == 0 else "dve"
            if eng == "act":
                P.I("act", "copy", out=dstT[:, c4 * 4:(c4 + 1) * 4, col0:col0 + npart], in_=PTs[half][:, 0:4, :npart],
                     R=[("PT", half)], W=[("dstT", id(dstT), col0)])
            else:
                P.I("dve", "tensor_copy", out=dstT[:, c4 * 4:(c4 + 1) * 4, col0:col0 + npart], in_=PTs[half][:, 0:4, :npart],
                     R=[("PT", half)], W=[("dstT", id(dstT), col0)])

    xts = [P.psb([128, D], F32) for _ in range(2)]
    xnbA = {"sq": P.psb([128, D], BF16), "b": [P.psb([128, D], BF16) for _ in range(2)]}
    gBmix = P.psb([128, D], F32)
    P.D("sp", out=gBmix[:], in_=g_mix.partition_broadcast(128), W=["gB"])
    for blk in A_BLOCKS:
        xt = xts[blk % 2]
        npart = 128 if blk < 16 else 16
        src = xp[blk * 128:(blk + 1) * 128, :] if blk < 16 else xs
        tag = "A%d" % (blk % 2)
        P.D("sp", out=xt[:npart], in_=src, W=[tag + "xt"])
        rmsnorm_T(xt, npart, gBmix, xnbA, xnT, blk * 128, tag)
    if debug:
        if DBG_F32:
            xnT32 = P.psb([128, 16, 256], F32)
            P.I("dve", "tensor_copy", out=xnT32[:], in_=xnT[:, :, 0:256], R=[("dstT", id(xnT), c * 128) for c in range(17)], W=["xnT32"])
            dbg["xnT"] = do("dbg_xnT32", [128, 16, 256], F32)
            P.D("sp", out=dbg["xnT"], in_=xnT32[:], R=["xnT32"])
        else:
            dbg["xnT"] = do("dbg_xnT", [128, 16, TOKS], BF16)
            P.D("sp", out=dbg["xnT"], in_=xnT[:], R=[("dstT", id(xnT), c * 128) for c in range(17)])
    P.end_phase()
    if stop_after == "A":
        return nc, P, dbg

    wb3 = P.psb([128, 16, 512], BF16)
    qT = P.psb([128, 4, 2048], BF16)
    kT = P.psb([128, 4, 2048], BF16)
    vsb = P.psb([128, 16, 512], BF16)
    tqs = [P.psb([128, 512], BF16) for _ in range(2)]
    st32 = [P.psb([128, 512], F32) for _ in range(3)]
    Sbuf = [P.psb([128, 256], F32) for _ in range(2)]
    Ptb_ = [P.psb([128, 256], BF16) for _ in range(2)]
    ostg = [P.psb([128, 4, 129], F32) for _ in range(2)]
    btab = P.psb([128, 256], F32)
    hb = P.psb([128, 4], F32)
    P.D("sp", out=btab[:], in_=btab_d, W=["btab"])
    P.D("sp", out=hb[:], in_=hb_d, W=["hb"])

    pjn = {"n": 0}

    def proj_block(wb, start, step, npart):
        k = pjn["n"] % 2
        pjn["n"] += 1
        pj = PJ[k]
        for dc in range(16):
            if npart == 128:
                lt = xnT[:, dc, start:start + 128 * step:step]
            else:
                lt = xnT[:, dc, start:start + npart]
            P.I("pe", "matmul", pj[:npart, :], lhsT=lt, rhs=wb[:, dc, :], start=(dc == 0), stop=(dc == 15),
                 R=[("wb", id(wb)), "xnT"], W=[("PJ", k)])
        return pj, k

    KV_BLOCKS = [
        [(128 * lb, 1) for lb in range(7, 16)],
        [(rho + 4 * 128 * sb, 4) for rho in range(4) for sb in range(1, 4)],
        [(r, 16) for r in range(16)],
    ]
    Q_BLOCKS = [
        [(128 * lb, 1) for lb in range(8, 16)],
        [(rho + 4 * 128 * sb, 4) for rho in range(4) for sb in range(2, 4)],
        [(r, 16) for r in range(16)],
    ]

    def kv_out_rows(g, bi):
        if g == 0:
            if bi == 8:
                return slice(0, 128), kvp[0][0:128]
            return None
        if g == 1:
            rho, sbi = bi // 3, bi % 3
            if sbi == 2:
                return slice(0, 128), kvp[1][rho:rho + 509:4]
            return None
        r = bi
        return slice(64, 128), kvp[2][r:r + 1009:16]

    tn = {"n": 0, "s": 0}

    def to_T(pj, pk, dst, col0):
        k = tn["n"] % 2
        tn["n"] += 1
        tq = tqs[k]
        P.I("act", "copy", out=tq[:], in_=pj[:], R=[("PJ", pk)], W=[("tq", k)])
        for j in range(4):
            P.I("pe", "transpose", out=PTs[k][:, j, :], in_=tq[:, j * 128:(j + 1) * 128], identity=identb[:],
                 R=[("tq", k), "identb"], W=[("PT", k)])
        P.I("dve", "tensor_copy", out=dst[:, :, col0:col0 + 128], in_=PTs[k][:, 0:4, :],
             R=[("PT", k)], W=[("T", id(dst))])

    def stage32(pj, pk, npart):
        k = tn["s"] % 3
        tn["s"] += 1
        st = st32[k]
        if True:
            P.I("act", "copy", out=st[:npart], in_=pj[:npart], R=[("PJ", pk)], W=[("st32", k)])
        else:
            P.I("dve", "tensor_copy", out=st[:npart], in_=pj[:npart], R=[("PJ", pk)], W=[("st32", k)])
        return st, k

    def attention(g, hq):
        units = []
        if g == 0:
            for qb in range(8):
                units.append((qb * 128, 128, [(qb, 0, (0 if qb == 0 else 2)), (qb + 1, 128, 2)], OG[0, qb * 128:(qb + 1) * 128]))
        elif g == 1:
            for rho in range(4):
                for sbi in range(2):
                    qi = rho * 2 + sbi
                    r0 = rho + 512 * sbi
                    units.append((qi * 128, 128, [(rho * 3 + sbi, 0, (0 if sbi == 0 else 2)), (rho * 3 + sbi + 1, 128, 2)],
                                  OG[1, r0:r0 + 509:4]))
        else:
            for r in range(16):
                units.append((r * 128 + 64, 64, [(r, 128 + 64, 1)], OG[2, r:r + 1009:16]))
        for ui, (qc0, nq, tiles, orow) in enumerate(units):
            osl = ui % 2
            for h in range(4):
                head = hq * 4 + h
                cgh = float(SLOPES[g, head] * DIL[g] * SQ)
                sl = (ui * 4 + h) % 2
                Sps = SBK[sl]
                nt = len(tiles)
                for j, (kidx, tcol, hbsel) in enumerate(tiles):
                    P.I("pe", "matmul", Sps[:, j * 128:j * 128 + nq], lhsT=kT[:, h, kidx * 128:(kidx + 1) * 128],
                                                                                rhs=qT[:, h, qc0:qc0 + nq], start=True, stop=True,
                         R=[("T", id(kT)), ("T", id(qT))], W=[("SBK", sl)])
                Sb = Sbuf[sl]
                if nt == 2:
                    P.I("dve", "scalar_tensor_tensor", out=Sb[:, 0:256], in0=btab[:, 0:256], scalar=cgh, in1=Sps[:, 0:256],
                                                                                         op0=ALU.mult, op1=ALU.add,
                         R=["btab", ("SBK", sl)], W=[("Sbuf", sl)])
                else:
                    tcol = tiles[0][1]
                    P.I("dve", "scalar_tensor_tensor", out=Sb[:, 0:nq], in0=btab[:, tcol:tcol + nq], scalar=cgh,
                                                                                                    in1=Sps[:, 0:nq], op0=ALU.mult, op1=ALU.add,
                         R=["btab", ("SBK", sl)], W=[("Sbuf", sl)])
                Pt = Ptb_[sl]
                for j, (kidx, tcol, hbsel) in enumerate(tiles):
                    P.I("act", "activation", out=Pt[:, j * 128:j * 128 + nq], in_=Sb[:, j * 128:j * 128 + nq], func=AF.Exp,
                                                                                      bias=hb[:, hbsel:hbsel + 1], scale=SCALE,
                         R=[("Sbuf", sl), "hb"], W=[("Pt", sl, j)])
                for j, (kidx, tcol, hbsel) in enumerate(tiles):
                    P.I("pe", "matmul", OB[osl][:nq, h * 128:(h + 1) * 128], lhsT=Pt[:, j * 128:j * 128 + nq],
                                                                             rhs=vsb[:, kidx, h * 128:(h + 1) * 128], start=(j == 0), stop=(j == nt - 1),
                         R=[("Pt", sl, j), "vsb"], W=[("OB", osl)])
                    P.I("pe", "matmul", PJ[osl][:nq, h:h + 1], lhsT=Pt[:, j * 128:j * 128 + nq],
                                                                   rhs=onesb[:, 0:1], start=(j == 0), stop=(j == nt - 1),
                         R=[("Pt", sl, j), "onesb"], W=[("PJ", osl)])
            og = ostg[osl]
            P.I("act", "copy", out=og[:nq, :, 0:128], in_=OB[osl][:nq, :].rearrange("p (h d) -> p h d", h=4),
                 R=[("OB", osl)], W=[("ostg", osl)])
            P.I("dve", "tensor_copy", out=og[:nq, :, 128], in_=PJ[osl][:nq, 0:4],
                 R=[("PJ", osl)], W=[("ostg", osl)])
            P.D("sp", out=orow[:, hq * 4:(hq + 1) * 4, :], in_=og[:nq],
                  R=[("ostg", osl)], W=["OG"])

    for (hq, g) in B1_BUNDLES:
        for _once in (0,):
            base = 4096 + g * 6144 + hq * 512
            wb = load_w(w_in, base, wb3)
            for bi, (st, sp_) in enumerate(Q_BLOCKS[g]):
                pj, pk = proj_block(wb, st, sp_, 128)
                to_T(pj, pk, qT, bi * 128)
            if B1_CUT < 2:
                continue
            pj, pk = proj_block(wb, SOFF, 1, 16)
            s32, sk = stage32(pj, pk, 16)
            P.D("sp", out=sqd[g, 0, :, hq * 512:(hq + 1) * 512], in_=s32[:16],
                  R=[("st32", sk)], W=["sqd"])
            if B1_CUT < 3:
                continue
            wb = load_w(w_in, base + 2048, wb3)
            for bi, (st, sp_) in enumerate(KV_BLOCKS[g]):
                pj, pk = proj_block(wb, st, sp_, 128)
                kvo = kv_out_rows(g, bi) if 'nokvout' not in KNOB else None
                if kvo is not None:
                    psl, rows = kvo
                    s32, sk = stage32(pj, pk, 128)
                    P.D("sp", out=rows[:, 0, hq * 512:(hq + 1) * 512], in_=s32[psl],
                          R=[("st32", sk)])
                to_T(pj, pk, kT, bi * 128)
            if 'nosample' in KNOB:
                continue
            pj, pk = proj_block(wb, SOFF, 1, 16)
            s32, sk = stage32(pj, pk, 16)
            if 'nosqd' not in KNOB:
                P.D("sp", out=sqd[g, 1, :, hq * 512:(hq + 1) * 512], in_=s32[:16],
                      R=[("st32", sk)], W=["sqd"])
            if 'nokvs' not in KNOB:
                P.D("sp", out=kvs[g][:, 0, hq * 512:(hq + 1) * 512], in_=s32[:16],
                      R=[("st32", sk)])
            if B1_CUT < 4:
                continue
            wb = load_w(w_in, base + 4096, wb3)
            for bi, (st, sp_) in enumerate(KV_BLOCKS[g]):
                pj, pk = proj_block(wb, st, sp_, 128)
                kvo = kv_out_rows(g, bi)
                if kvo is not None:
                    psl, rows = kvo
                    s32, sk = stage32(pj, pk, 128)
                    P.D("sp", out=rows[:, 1, hq * 512:(hq + 1) * 512], in_=s32[psl],
                          R=[("st32", sk)])
                P.I("act", "copy", out=vsb[:, bi, :], in_=pj[:], R=[("PJ", pk)], W=["vsb"])
            pj, pk = proj_block(wb, SOFF, 1, 16)
            s32, sk = stage32(pj, pk, 16)
            P.D("sp", out=sqd[g, 2, :, hq * 512:(hq + 1) * 512], in_=s32[:16],
                  R=[("st32", sk)], W=["sqd"])
            P.D("sp", out=kvs[g][:, 1, hq * 512:(hq + 1) * 512], in_=s32[:16],
                  R=[("st32", sk)])
            if B1_CUT < 5:
                continue
            attention(g, hq)
    if debug and B1_CUT >= 6:
        dbg["OG"] = do("dbg_OG", [3, 1024, 16, 129])
        for g in range(3):
            P.D("sp", out=dbg["OG"][g], in_=OG[g], R=["OG"])
    P.end_phase()
    if stop_after == "B1":
        return nc, P, dbg

    stab = P.psb([128, 12, 64], F32)
    dmask = P.psb([16, 256], F32)
    P.D("sp", out=stab[:], in_=stab_d, W=["stab"])
    P.D("sp", out=dmask[:], in_=dmask_d, W=["dmask"])
    qload = P.psb([16, D], F32)
    qbf = P.psb([16, D], BF16)
    qTs = P.psb([128, 3, 16, 16], BF16)
    kTn = P.psb([128, 3, 16, 16], BF16)
    for g in range(3):
        for kind, dst in ((0, qTs), (1, kTn)):
            P.D("sp", out=qload[:], in_=sqd[g, kind], R=["sqd"], W=["qload"])
            P.I("dve", "tensor_copy", out=qbf[:], in_=qload[:], R=["qload"], W=["qbf"])
            for h4 in range(4):
                for j in range(4):
                    h = h4 * 4 + j
                    P.I("pe", "transpose", out=PTs[0][:, j, 0:16], in_=qbf[0:16, h * 128:(h + 1) * 128], identity=identb[0:16, 0:16],
                         R=["qbf", "identb"], W=[("PT", 0)])
                P.I("act", "copy", out=dst[:, g, h4 * 4:(h4 + 1) * 4, :], in_=PTs[0][:, 0:4, 0:16],
                     R=[("PT", 0)], W=[("qk", id(dst))])
    Oacc = P.psb([16, D], F32)
    Lacc = P.psb([16, 256], F32)
    P.I("dve", "memset", Oacc[:], 0.0, W=["Oacc"])
    P.I("dve", "memset", Lacc[:], 0.0, W=["Lacc"])
    PtS = [P.psb([128, 16, 16], BF16) for _ in range(4)]
    for b in range(4):
        P.I("pool", "memset", PtS[b][:], 0.0, W=[("PtS", b)])
    kvc = [P.psb([128, 2, D], BF16) for _ in range(2)]
    kTc = [P.psb([128, 16, 128], BF16) for _ in range(2)]
    SbS = [P.psb([128, 64], F32) for _ in range(2)]
    tl = 0
    for b in range(4):
        for g in range(3):
            tiles = []
            if g == 0:
                tiles.append((128, ck[0][b], 0))
            elif g == 1:
                v4 = ck[1][b].rearrange("(m r) k c -> r m k c", r=4)
                for r in range(4):
                    tiles.append((128, v4[r], 1 + r))
            else:
                v16 = ck[2][b].rearrange("(m r) k c -> r m k c", r=16)
                for r in range(4):
                    tiles.append((128, v16[r], 5 + r))
            tiles.append((4, None, 9 + g))
            for (nk, src, tbl) in tiles:
                sl = tl % 2
                tl += 1
                kv_ = kvc[sl]
                kt_ = kTc[sl]
                if src is not None:
                    P.D("pool", out=kv_[:], in_=src, W=[("kvc", sl)], max_dma_last_dim=4096)
                else:
                    P.D("pool", out=kv_[0:4, 0, :], in_=sqd[g, 1, 4 * b:4 * b + 4, :], R=["sqd"], W=[("kvc", sl)], max_dma_last_dim=4096)
                    P.D("pool", out=kv_[0:4, 1, :], in_=sqd[g, 2, 4 * b:4 * b + 4, :], R=["sqd"], W=[("kvc", sl)], max_dma_last_dim=4096)
                for h4 in range(4):
                    half = h4 % 2
                    for j in range(4):
                        h = h4 * 4 + j
                        P.I("pe", "transpose", out=PTs[half][:, j, 0:nk], in_=kv_[0:nk, 0, h * 128:(h + 1) * 128],
                                                                                             identity=identb[0:nk, 0:nk],
                             R=[("kvc", sl), "identb"], W=[("PT", half)])
                    P.I("act", "copy", out=kt_[:, h4 * 4:(h4 + 1) * 4, 0:nk], in_=PTs[half][:, 0:4, 0:nk],
                         R=[("PT", half)], W=[("kTc", sl)])
                Sps = PJ[sl]
                for h in range(16):
                    P.I("pe", "matmul", Sps[0:nk, h * 4:(h + 1) * 4], lhsT=kt_[:, h, 0:nk],
                                                                                          rhs=qTs[:, g, h, 4 * b:4 * b + 4], start=True, stop=True,
                         R=[("kTc", sl), ("qk", id(qTs))], W=[("PJ", sl)])
                Sb = SbS[sl]
                P.I("dve", "scalar_tensor_tensor", out=Sb[0:nk, :], in0=Sps[0:nk, 0:64], scalar=SCALE, in1=stab[0:nk, tbl, :],
                                                                                             op0=ALU.mult, op1=ALU.add,
                     R=[("PJ", sl), "stab"], W=[("SbS", sl)])
                pts = PtS[b]
                P.I("act", "activation", out=pts[0:nk, :, 4 * b:4 * b + 4], in_=Sb[0:nk, :].rearrange("p (h q) -> p h q", h=16),
                                                                               func=AF.Exp,
                     R=[("SbS", sl)], W=[("PtS", b)])
                obanks = [OB[0], OB[1], SBK[0], SBK[1]]
                for h in range(16):
                    ob = obanks[h // 4]
                    P.I("pe", "matmul", ob[0:16, (h % 4) * 128:(h % 4 + 1) * 128], lhsT=pts[0:nk, h, :],
                                                                                       rhs=kv_[0:nk, 1, h * 128:(h + 1) * 128], start=True, stop=True,
                         R=[("PtS", b), ("kvc", sl)], W=[("OBS", h // 4)])
                P.I("pe", "matmul", PJ[sl][0:16, 256:512], lhsT=onesb[0:nk, 0:16], rhs=pts[0:nk].rearrange("p h q -> p (h q)"), start=True, stop=True,
                     R=[("PtS", b), "onesb"], W=[("PJ", sl)])
                for k4 in range(4):
                    ob = obanks[k4]
                    P.I("dve", "tensor_tensor", out=Oacc[:, k4 * 512:(k4 + 1) * 512], in0=ob[0:16, :], in1=Oacc[:, k4 * 512:(k4 + 1) * 512], op=ALU.add,
                         R=[("OBS", k4), "Oacc"], W=["Oacc", ("OBS", k4)])
                P.I("dve", "tensor_tensor", out=Lacc[:], in0=PJ[sl][0:16, 256:512], in1=Lacc[:], op=ALU.add, R=[("PJ", sl), "Lacc"], W=["Lacc"])
    ltmp = P.psb([16, 256], F32)
    lsum = P.psb([16, 16], F32)
    P.I("dve", "tensor_tensor", out=ltmp[:], in0=Lacc[:], in1=dmask[:], op=ALU.mult, R=["Lacc", "dmask"], W=["ltmp"])
    P.I("dve", "tensor_reduce", out=lsum[:], in_=ltmp[:].rearrange("p (h q) -> p h q", h=16), axis=AX.X, op=ALU.add, R=["ltmp"], W=["lsum"])
    P.I("dve", "reciprocal", out=lsum[:], in_=lsum[:], R=["lsum"], W=["lsum"])
    P.I("dve", "tensor_tensor", out=bouts[:].rearrange("p (h d) -> p h d", h=16), in0=Oacc[:].rearrange("p (h d) -> p h d", h=16),
                                          in1=lsum[:, :, None].to_broadcast([16, 16, 128]), op=ALU.mult,
         R=["Oacc", "lsum"], W=["bouts"])
    if debug:
        dbg["bouts"] = do("dbg_bouts", [16, D])
        P.D("sp", out=dbg["bouts"], in_=bouts[:], R=["bouts"])
    P.end_phase()
    if stop_after == "B1s":
        return nc, P, dbg

    vm = P.psb([128, 8, D], BF16)
    vms = P.psb([16, D], BF16)
    vsf = P.psb([16, D], F32)
    vsb16 = P.psb([16, D], BF16)
    gBsgu = P.psb([128, D], F32)
    cmask = P.psb([128, 128], F32)
    cms = P.psb([16, 16], F32)
    wst = P.psb([128, 128], F32)
    WcT = P.psb([128, 8, 128], BF16)
    WsT = P.psb([16, 8, 16], BF16)
    Wtmp = P.psb([16, 8, 16], F32)
    bT = P.psb([128, 8], F32)
    bsT = P.psb([16, 8], F32)
    sbl = P.psb([8, 128], F32)
    stats = P.psb([128, 9, 4, 6], F32)
    mv = P.psb([128, 9, 2], F32)
    utmp = [P.psb([128, 512], F32) for _ in range(2)]
    t2048 = P.psb([128, D], F32)
    ogt = [P.psb([128, 3, 4, 129], F32) for _ in range(2)]
    num = P.psb([128, 4, 129], F32)
    rl = P.psb([128, 4], F32)
    bt = P.psb([128, 512], F32)
    P.D("sp", out=gBsgu[:], in_=g_sgu.partition_broadcast(128), W=["gBsgu"])
    P.D("sp", out=cmask[:], in_=cmask_d, W=["cmask"])
    P.D("sp", out=cms[:], in_=cms_d, W=["cms"])
    P.D("sp", out=sbl[:], in_=sgu_b, W=["sbl"])
    for g in range(8):
        P.D("sp", out=wst[:], in_=sgu_w[g], W=["wst"])
        P.I("pe", "transpose", out=SBK[0][:, 0:128], in_=wst[:], identity=identf[:], R=["wst", "identf"], W=[("SBK", 0)])
        P.I("dve", "tensor_tensor", out=WcT[:, g, :], in0=SBK[0][:, 0:128], in1=cmask[:], op=ALU.mult,
             R=[("SBK", 0), "cmask"], W=["WcT"])
    P.I("pe", "transpose", out=SBK[1][:, 0:8], in_=sbl[0:8, :], identity=identf[0:8, 0:8], R=["sbl", "identf"], W=[("SBK", 1)])
    P.I("dve", "tensor_copy", out=bT[:], in_=SBK[1][:, 0:8], R=[("SBK", 1)], W=["bT"])
    P.I("dve", "memset", WsT[:], 0.0, W=["WsT"])
    for b in range(4):
        P.D("sp", out=WsT[4 * b:4 * b + 4, :, 4 * b:4 * b + 4], in_=WcT[0:4, :, 0:4], R=["WcT", "WsT"], W=["WsT"])
        P.D("sp", out=bsT[4 * b:4 * b + 4, :], in_=bT[0:4, :], R=["bT"], W=["bsT"])

    TB = list(range(8)) + ["s"]

    def tok_block(tb):
        if tb == "s":
            return SOFF, 16
        return 1024 + tb * 128, 128

    for ct in range(4):
        wb = load_w(w_in, 2048 + ct * 512)
        for ti, tb in enumerate(TB):
            st, npart = tok_block(tb)
            pj, pk = proj_block(wb, st, 1, npart)
            if tb == "s":
                P.I("act", "activation", out=vsf[:, ct * 512:(ct + 1) * 512], in_=pj[:16], func=AF.Gelu_apprx_tanh,
                     R=[("PJ", pk)], W=[("vsf", ct)])
                P.I("dve", "bn_stats", out=stats[:16, 8, ct, :], in_=vsf[:, ct * 512:(ct + 1) * 512], R=[("vsf", ct)], W=[("stats", 8)])
            else:
                P.I("act", "activation", out=vm[:, tb, ct * 512:(ct + 1) * 512], in_=pj[:], func=AF.Gelu_apprx_tanh,
                     R=[("PJ", pk)], W=[("vm", tb, ct)])
                P.I("dve", "bn_stats", out=stats[:, tb, ct, :], in_=vm[:, tb, ct * 512:(ct + 1) * 512], R=[("vm", tb, ct)], W=[("stats", tb)])
    for ti, tb in enumerate(TB):
        npart = 16 if tb == "s" else 128
        P.I("dve", "bn_aggr", out=mv[:npart, ti, :], in_=stats[:npart, ti].rearrange("p a b -> p (a b)"),
             R=[("stats", ti)], W=[("mv", ti)])
        P.I("act", "activation", out=mv[:npart, ti, 1:2], in_=mv[:npart, ti, 1:2], func=AF.Sqrt, bias=EPS, scale=1.0,
             R=[("mv", ti)], W=[("mv", ti)])
        P.I("dve", "reciprocal", out=mv[:npart, ti, 1:2], in_=mv[:npart, ti, 1:2], R=[("mv", ti)], W=[("mv", ti)])
        src = vsf[:] if tb == "s" else vm[:, tb, :]
        rtag = [("vsf", c) for c in range(4)] if tb == "s" else [("vm", tb, c) for c in range(4)]
        P.I("dve", "scalar_tensor_tensor", out=t2048[:npart], in0=src, scalar=mv[:npart, ti, 0:1], in1=gBsgu[:npart],
                                                                                 op0=ALU.subtract, op1=ALU.mult,
             R=rtag + [("mv", ti), "gBsgu"], W=["t2048"])
        if tb == "s":
            P.I("act", "activation", out=vsf[:], in_=t2048[:16], func=AF.Identity, scale=mv[:16, ti, 1:2], R=["t2048", ("mv", ti)], W=rtag)
            P.I("dve", "tensor_copy", out=vsb16[:], in_=vsf[:], R=rtag, W=["vsb16"])
            P.D("sp", out=sguv, in_=vsf[:], R=rtag)
        else:
            P.I("act", "activation", out=vm[:, tb, :], in_=t2048[:], func=AF.Identity, scale=mv[:, ti, 1:2], R=["t2048", ("mv", ti)], W=rtag)
    for ct in range(4):
        wb = load_w(w_in, ct * 512)
        for ti, tb in enumerate(TB):
            st, npart = tok_block(tb)
            pj, pk = proj_block(wb, st, 1, npart)
            ut = utmp[ti % 2]
            P.I("act", "activation", out=ut[:npart], in_=pj[:npart], func=AF.Gelu_apprx_tanh,
                 R=[("PJ", pk)], W=[("utmp", ti % 2)])
            sl = ti % 2
            for gg in range(2):
                g = ct * 2 + gg
                if tb == "s":
                    P.I("pe", "matmul", SBK[sl][0:16, gg * 256:(gg + 1) * 256], lhsT=WsT[:, g, :], rhs=vsb16[:, g * 256:(g + 1) * 256], start=True, stop=True,
                         R=["WsT", "vsb16"], W=[("SBK", sl)])
                else:
                    P.I("pe", "matmul", SBK[sl][:, gg * 256:(gg + 1) * 256], lhsT=WcT[:, g, :], rhs=vm[:, tb, g * 256:(g + 1) * 256], start=True, stop=True,
                         R=["WcT", ("vm", tb, ct)], W=[("SBK", sl)])
            for gg in range(2):
                g = ct * 2 + gg
                if tb == "s":
                    P.I("dve", "scalar_tensor_tensor", out=vms[:, g * 256:(g + 1) * 256], in0=SBK[sl][0:16, gg * 256:(gg + 1) * 256], scalar=bsT[:, g:g + 1],
                                                                                          in1=ut[:16, gg * 256:(gg + 1) * 256], op0=ALU.add, op1=ALU.mult,
                         R=[("SBK", sl), "bsT", ("utmp", ti % 2)], W=[("vms", ct)])
                else:
                    P.I("dve", "scalar_tensor_tensor", out=vm[:, tb, g * 256:(g + 1) * 256], in0=SBK[sl][:, gg * 256:(gg + 1) * 256], scalar=bT[:, g:g + 1],
                                                                                                 in1=ut[:, gg * 256:(gg + 1) * 256], op0=ALU.add, op1=ALU.mult,
                         R=[("SBK", sl), "bT", ("utmp", ti % 2)], W=[("vm", tb, ct)])
    if debug:
        dbg["aout"] = do("dbg_aout", [128, 8, D], BF16)
        P.D("sp", out=dbg["aout"], in_=vm[:], R=[("vm", tb, c) for tb in range(8) for c in range(4)])
    gbase = 4096 + 3 * 6144
    for ct in range(4):
        wb = load_w(w_in, gbase + ct * 512)
        for ti, tb in enumerate(TB):
            st, npart = tok_block(tb)
            pj, pk = proj_block(wb, st, 1, npart)
            ut = utmp[ti % 2]
            P.I("act", "activation", out=ut[:npart], in_=pj[:npart], func=AF.Sigmoid, R=[("PJ", pk)], W=[("utmp", ti % 2)])
            if tb == "s":
                P.I("dve", "tensor_tensor", out=vms[:, ct * 512:(ct + 1) * 512], in0=ut[:16], in1=vms[:, ct * 512:(ct + 1) * 512], op=ALU.mult,
                     R=[("utmp", ti % 2), ("vms", ct)], W=[("vms", ct)])
            else:
                P.I("dve", "tensor_tensor", out=vm[:, tb, ct * 512:(ct + 1) * 512], in0=ut[:], in1=vm[:, tb, ct * 512:(ct + 1) * 512], op=ALU.mult,
                     R=[("utmp", ti % 2), ("vm", tb, ct)], W=[("vm", tb, ct)])
    for ct in range(4):
        wb = load_w(w_in, gbase + 2048 + ct * 512)
        for ti, tb in enumerate(TB):
            st, npart = tok_block(tb)
            pj, pk = proj_block(wb, st, 1, npart)
            ut = utmp[ti % 2]
            P.I("act", "activation", out=ut[:npart], in_=pj[:npart], func=AF.Sigmoid, R=[("PJ", pk)], W=[("utmp", ti % 2)])
            if tb == "s":
                P.I("dve", "tensor_tensor", out=bt[:16], in0=ut[:16], in1=bouts[:, ct * 512:(ct + 1) * 512], op=ALU.mult,
                     R=[("utmp", ti % 2)], W=["bt"])
                P.I("dve", "tensor_tensor", out=vms[:, ct * 512:(ct + 1) * 512], in0=bt[:16], in1=vms[:, ct * 512:(ct + 1) * 512], op=ALU.add,
                     R=["bt", ("vms", ct)], W=[("vms", ct)])
            else:
                og = ogt[ti % 2]
                P.D("sp", out=og[:], in_=OG[:, tb * 128:(tb + 1) * 128, ct * 4:(ct + 1) * 4, :].rearrange("g p h d -> p g h d"),
                      W=[("ogt", ti % 2)])
                P.I("dve", "tensor_tensor", out=num[:], in0=og[:, 0], in1=og[:, 1], op=ALU.add, R=[("ogt", ti % 2)], W=["num"])
                P.I("dve", "tensor_tensor", out=num[:], in0=num[:], in1=og[:, 2], op=ALU.add, R=[("ogt", ti % 2), "num"], W=["num"])
                P.I("dve", "reciprocal", out=rl[:], in_=num[:, :, 128], R=["num"], W=["rl"])
                P.I("dve", "tensor_tensor", out=bt[:].rearrange("p (h d) -> p h d", h=4), in0=num[:, :, 0:128], in1=rl[:, :, None].to_broadcast([128, 4, 128]), op=ALU.mult,
                     R=["num", "rl"], W=["bt"])
                P.I("dve", "tensor_tensor", out=bt[:], in0=bt[:], in1=ut[:], op=ALU.mult, R=["bt", ("utmp", ti % 2)], W=["bt"])
                P.I("dve", "tensor_tensor", out=vm[:, tb, ct * 512:(ct + 1) * 512], in0=bt[:], in1=vm[:, tb, ct * 512:(ct + 1) * 512], op=ALU.add,
                     R=["bt", ("vm", tb, ct)], W=[("vm", tb, ct)])
    if debug:
        dbg["merged"] = do("dbg_merged", [128, 8, D], BF16)
        P.D("sp", out=dbg["merged"], in_=vm[:], R=[("vm", tb, c) for tb in range(8) for c in range(4)])
        dbg["merged_s"] = do("dbg_merged_s", [16, D], BF16)
        P.D("sp", out=dbg["merged_s"], in_=vms[:], R=[("vms", c) for c in range(4)])
    for tb in range(8):
        P.D("sp", out=mgd[tb], in_=vm[:, tb, :], R=[("vm", tb, c) for c in range(4)], W=["mgd"])
    P.D("sp", out=mgd[8, 0:16], in_=vms[:], R=[("vms", c) for c in range(4)], W=["mgd"])
    P.end_phase()
    es_ab.close()
    if stop_after == "B2":
        return nc, P, dbg

    es_d = contextlib.ExitStack()
    acc = P.ssb(es_d, [128, 9, D], F32)
    es_e = contextlib.ExitStack()
    xn2T = P.ssb(es_e, [128, 16, 1040], BF16)
    es_c2 = contextlib.ExitStack()
    mT = P.ssb(es_c2, [128, 16, 1040], BF16)
    wbs2 = [P.ssb(es_c2, [128, 16, 512], BF16) for _ in range(2)]
    mrow = [P.psb([128, D], BF16) for _ in range(2)]
    tc1 = {"n": 0}

    def transpose_rows(src_rows, npart, dstT, col0, rtag="srcrows"):
        for c4 in range(4):
            half = tc1["n"] % 2
            tc1["n"] += 1
            for j in range(4):
                c = c4 * 4 + j
                P.I("pe", "transpose", out=PTs[half][:, j, :npart], in_=src_rows[:, c * 128:(c + 1) * 128], identity=identb[:npart, :npart],
                    R=[rtag, "identb"], W=[("PT", half)])
            if c4 % 2 == 0:
                P.I("act", "copy", out=dstT[:, c4 * 4:(c4 + 1) * 4, col0:col0 + npart], in_=PTs[half][:, 0:4, :npart], R=[("PT", half)], W=["dstT"])
            else:
                P.I("dve", "tensor_copy", out=dstT[:, c4 * 4:(c4 + 1) * 4, col0:col0 + npart], in_=PTs[half][:, 0:4, :npart], R=[("PT", half)], W=["dstT"])

    for tb in range(9):
        npart = 128 if tb < 8 else 16
        mr = mrow[tb % 2]
        P.D("sp", out=mr[:npart], in_=mgd[tb, 0:npart], W=[("mrow", tb % 2)])
        transpose_rows(mr[:npart, :], npart, mT, tb * 128, rtag=("mrow", tb % 2))
    P.end_phase()

    xres = [P.psb([128, 512], F32) for _ in range(2)]
    ssC = P.psb([128, 9], F32)
    rsC = P.psb([128, 9], F32)
    gBffn = P.psb([128, D], F32)
    xnbC = {"sq": P.psb([128, D], BF16), "b": [P.psb([128, D], BF16) for _ in range(2)]}
    P.D("sp", out=gBffn[:], in_=g_ffn.partition_broadcast(128), W=["gB"])
    wn = {"n": 0}
    xr = 0
    for ct in range(4):
        wb = wbs2[wn["n"] % 2]
        wn["n"] += 1
        P.D("pool", out=wb[:], in_=w_out[:, ct * 512:(ct + 1) * 512].rearrange("(c p) n -> p c n", p=128), W=[("wb", id(wb))])
        for tb in range(9):
            npart = 128 if tb < 8 else 16
            k = pjn["n"] % 2
            pjn["n"] += 1
            pj = PJ[k]
            for dc in range(16):
                P.I("pe", "matmul", pj[:npart, :], lhsT=mT[:, dc, tb * 128:tb * 128 + npart], rhs=wb[:, dc, :],
                                                                                       start=(dc == 0), stop=(dc == 15),
                     R=[("wb", id(wb))], W=[("PJ", k)])
            xrt = xres[xr % 2]
            src = xp[1024 + tb * 128:1024 + (tb + 1) * 128, ct * 512:(ct + 1) * 512] if tb < 8 else xs[:, ct * 512:(ct + 1) * 512]
            P.D("sp", out=xrt[:npart], in_=src, W=[("xres", xr % 2)])
            P.I("dve", "tensor_tensor", out=acc[:npart, tb, ct * 512:(ct + 1) * 512], in0=pj[:npart], in1=xrt[:npart], op=ALU.add,
                 R=[("PJ", k), ("xres", xr % 2)], W=[("acc", tb, ct)])
            xr += 1
    if debug:
        dbg["h"] = do("dbg_h", [128, 9, D])
        P.D("sp", out=dbg["h"][:, 0:8], in_=acc[:, 0:8], R=[("acc", tb, c) for tb in range(8) for c in range(4)])
        P.D("sp", out=dbg["h"][0:16, 8], in_=acc[0:16, 8], R=[("acc", 8, c) for c in range(4)])
    tcount["n"] = 0
    for tb in range(9):
        npart = 128 if tb < 8 else 16
        k = tb
        ss = ssC[:, tb:tb + 1]
        rs = rsC[:, tb:tb + 1]
        sq = xnbC["sq"]
        rt = [("acc", tb, c) for c in range(4)]
        P.I("act", "activation", out=sq[:npart], in_=acc[:npart, tb, :], func=AF.Square, accum_out=ss[:npart], R=rt, W=["sqj", ("ss", k)])
        P.I("act", "activation", out=rs[:npart], in_=ss[:npart], func=AF.Sqrt, scale=1.0 / D, bias=EPS, R=[("ss", k)], W=[("rs", k)])
        P.I("dve", "reciprocal", out=rs[:npart], in_=rs[:npart], R=[("rs", k)], W=[("rs", k)])
        xb = xnbC["b"][k % 2]
        P.I("dve", "scalar_tensor_tensor", out=xb[:npart], in0=acc[:npart, tb, :], scalar=rs[:npart, 0:1], in1=gBffn[:npart], op0=ALU.mult, op1=ALU.mult,
             R=rt + [("rs", k), "gB"], W=["srcrows"])
        transpose_rows(xb[:npart, :], npart, xn2T, tb * 128)
    P.end_phase()
    es_c2.close()
    if stop_after == "C":
        return nc, P, dbg

    es_q = contextlib.ExitStack()
    qTp = P.ssb(es_q, [128, 16, 1040], BF16)
    kTs = P.ssb(es_q, [128, 2, 128], BF16)
    wq = [P.psb([128, 16, 512], BF16) for _ in range(2)]
    skl = P.psb([128, 128], F32)
    for c in range(2):
        P.D("sp", out=skl[:], in_=subk[c], W=["skl"])
        P.I("pe", "transpose", out=SBK[0][:, 0:128], in_=skl[:], identity=identf[:], R=["skl", "identf"], W=[("SBK", 0)])
        P.I("dve", "tensor_copy", out=kTs[:, c, :], in_=SBK[0][:, 0:128], R=[("SBK", 0)], W=["kTs"])
    TT = [(0, 512), (512, 512), (1024, 16)]
    for ct in range(4):
        wb = wq[ct % 2]
        P.D("pool", out=wb[:], in_=w_q[:, ct * 512:(ct + 1) * 512].rearrange("(c p) n -> p c n", p=128), W=[("wb", id(wb))])
        for j in range(4):
            hc = ct * 4 + j
            for (t0, tn_) in TT:
                k = pjn["n"] % 2
                pjn["n"] += 1
                pj = PJ[k]
                for dc in range(16):
                    P.I("pe", "matmul", pj[:, 0:tn_], lhsT=wb[:, dc, j * 128:(j + 1) * 128], rhs=xn2T[:, dc, t0:t0 + tn_],
                                                                                          start=(dc == 0), stop=(dc == 15),
                         R=[("wb", id(wb)), "dstT"], W=[("PJ", k)])
                P.I("act", "copy", out=qTp[:, hc, t0:t0 + tn_], in_=pj[:, 0:tn_], R=[("PJ", k)], W=["qTp"])
    P.end_phase()

    s_sb = P.psb([128, 16, 128], F32)
    t16 = P.psb([128, 2, 16], F32)
    tmp128 = P.psb([128, 128], F32)
    cand = P.psb([128, 256], F32)
    cand2 = P.psb([128, 256], F32)
    tc16 = P.psb([128, 16], F32)
    etmp = P.psb([128, 16], F32)
    thr = P.psb([128, 8], F32)
    c0 = P.psb([128, 8], F32)
    zs = P.psb([128, 8], F32)
    NQ = 16
    RQ = 128 // NQ
    At = [P.psb([128, RQ, 128], F32) for _ in range(2)]
    Et = [P.psb([128, RQ, 128], F32) for _ in range(2)]
    GhB = [P.psb([128, RQ, 128], BF16) for _ in range(3)]
    git = 0
    Gq = [P.psb([128, RQ * 128], BF16) for _ in range(2)]
    gq = 0
    for tb in range(9):
        npart = 128 if tb < 8 else 16
        for hc4 in range(4):
            for j in range(4):
                hc = hc4 * 4 + j
                bank = [SBK[0], SBK[1], OB[0], OB[1]][hc4]
                P.I("pe", "matmul", bank[:npart, j * 128:(j + 1) * 128], lhsT=qTp[:, hc, tb * 128:tb * 128 + npart], rhs=kTs[:, hc % 2, :],
                                                                                         start=True, stop=True,
                     R=["qTp", "kTs"], W=[("SC", hc4)])
            bank = [SBK[0], SBK[1], OB[0], OB[1]][hc4]
            P.I("act", "copy", out=s_sb[:npart, hc4 * 4:(hc4 + 1) * 4, :], in_=bank[:npart, :].rearrange("p (a k) -> p a k", a=4),
                 R=[("SC", hc4)], W=[("s_sb", hc4)])
        for h in range(8):
            srd = [("s_sb", h // 2)]
            for c in range(2):
                sv = s_sb[:npart, 2 * h + c, :]
                P.I("dve", "max", out=t16[:npart, c, 0:8], in_=sv, R=srd, W=["t16"])
                P.I("dve", "match_replace", out=tmp128[:npart], in_to_replace=t16[:npart, c, 0:8], in_values=sv, imm_value=-BIG,
                     R=srd + ["t16"], W=["tmp128"])
                P.I("dve", "max", out=t16[:npart, c, 8:16], in_=tmp128[:npart], R=["tmp128"], W=["t16"])
            P.I("dve", "tensor_tensor", out=cand[:npart].rearrange("p (a b) -> p a b", a=16), in0=t16[:npart, 0, :, None].to_broadcast([npart, 16, 16]),
                                                              in1=t16[:npart, 1, None, :].to_broadcast([npart, 16, 16]), op=ALU.add,
                 R=["t16"], W=["cand"])
            P.I("dve", "max", out=tc16[:npart, 0:8], in_=cand[:npart], R=["cand"], W=["tc16"])
            P.I("dve", "match_replace", out=cand2[:npart], in_to_replace=tc16[:npart, 0:8], in_values=cand[:npart], imm_value=-BIG,
                 R=["cand", "tc16"], W=["cand2"])
            P.I("dve", "max", out=tc16[:npart, 8:16], in_=cand2[:npart], R=["cand2"], W=["tc16"])
            P.I("dve", "tensor_copy", out=thr[:npart, h:h + 1], in_=tc16[:npart, 15:16], R=["tc16"], W=[("thr", h)])
            P.I("dve", "tensor_scalar", out=c0[:npart, h:h + 1], in0=tc16[:npart, 0:1], scalar1=-1.0, scalar2=None, op0=ALU.mult,
                 R=["tc16"], W=[("c0", h)])
            P.I("act", "activation", out=etmp[:npart], in_=tc16[:npart], func=AF.Exp, bias=c0[:npart, h:h + 1], scale=1.0, accum_out=zs[:npart, h:h + 1],
                 R=["tc16", ("c0", h)], W=["etmp", ("zs", h)])
            P.I("act", "activation", out=zs[:npart, h:h + 1], in_=zs[:npart, h:h + 1], func=AF.Ln, R=[("zs", h)], W=[("zs", h)])
            P.I("dve", "tensor_tensor", out=c0[:npart, h:h + 1], in0=c0[:npart, h:h + 1], in1=zs[:npart, h:h + 1], op=ALU.subtract,
                 R=[("c0", h), ("zs", h)], W=[("c0", h)])
        for qi in range(NQ):
            for h in range(8):
                sl = h % 2
                A = At[sl]
                E = Et[sl]
                gsl = git % 3
                git += 1
                G_ = GhB[gsl]
                s1 = s_sb[:npart, 2 * h, qi * RQ:(qi + 1) * RQ]
                s2 = s_sb[:npart, 2 * h + 1, :]
                P.I("pool", "tensor_tensor", out=A[:npart], in0=s1[:, :, None].to_broadcast([npart, RQ, 128]),
                    in1=s2[:, None, :].to_broadcast([npart, RQ, 128]), op=ALU.add, R=[("s_sb", h // 2)], W=[("At", sl)])
                P.I("act", "activation", out=E[:npart], in_=A[:npart], func=AF.Exp, bias=c0[:npart, h:h + 1], scale=1.0,
                    R=[("At", sl), ("c0", h)], W=[("Et", sl)])
                P.I("dve", "scalar_tensor_tensor", out=G_[:npart], in0=A[:npart], scalar=thr[:npart, h:h + 1], in1=E[:npart], op0=ALU.is_ge, op1=ALU.mult,
                    R=[("At", sl), ("Et", sl), ("thr", h)], W=[("GhB", gsl)])
                gflat = G_[:npart].rearrange("p a b -> p (a b)")
                for half in range(2):
                    P.I("pe", "matmul", PJ[half][:npart, :], lhsT=identb[:npart, :npart], rhs=gflat[:, half * 512:(half + 1) * 512], start=(h == 0), stop=(h == 7),
                        R=[("GhB", gsl), "identb"], W=[("PJ", half)])
            gb = Gq[gq % 2]
            for half in range(2):
                P.I("act", "copy", out=gb[:npart, half * 512:(half + 1) * 512], in_=PJ[half][:npart, :], R=[("PJ", half)], W=[("Gq", gq % 2)])
            P.D("sp", out=Gd[tb, 0:npart, qi * RQ * 128:(qi + 1) * RQ * 128], in_=gb[:npart], R=[("Gq", gq % 2)], W=["Gd"])
            gq += 1
    P.end_phase()
    es_q.close()
    if stop_after == "D1":
        return nc, P, dbg

    EG = 256
    NG = NEXP // EG
    NCH = EG // 128
    ub = [P.psb([128, NCH, D], BF16) for _ in range(2)]
    vb = [P.psb([128, NCH, D], BF16) for _ in range(2)]
    uT = [P.psb([128, 16, EG], BF16) for _ in range(2)]
    Gt = [P.psb([128, 9, EG], BF16) for _ in range(2)]
    gt32 = [P.psb([128, EG], F32) for _ in range(2)]
    actb = [P.psb([128, EG], BF16) for _ in range(2)]
    actT = [P.psb([128, NCH, 128], BF16) for _ in range(2)]
    o2b = [SBK[0], SBK[1], OB[0], OB[1]]
    it = 0
    for eg in range(NG):
        sl = eg % 2
        for c in range(NCH):
            P.D("pool", out=ub[sl][:, c, :], in_=pu[eg * EG + c * 128:eg * EG + (c + 1) * 128, :], W=[("ub", sl)], max_dma_last_dim=4096)
        for c in range(NCH):
            P.D("pool", out=vb[sl][:, c, :], in_=pv[eg * EG + c * 128:eg * EG + (c + 1) * 128, :], W=[("vb", sl)], max_dma_last_dim=4096)
        P.D("sp", out=Gt[sl][:, 0:8, :], in_=Gd[0:8, :, eg * EG:(eg + 1) * EG].rearrange("t p e -> p t e"), R=["Gd"], W=[("Gt", sl)])
        P.D("sp", out=Gt[sl][0:16, 8, :], in_=Gd[8, 0:16, eg * EG:(eg + 1) * EG], R=["Gd"], W=[("Gt", sl)])
        for c in range(NCH):
            for d4 in range(4):
                half = (c * 4 + d4) % 2
                for j in range(4):
                    dc = d4 * 4 + j
                    P.I("pe", "transpose", out=PTs[half][:, j, :], in_=ub[sl][:, c, dc * 128:(dc + 1) * 128], identity=identb[:],
                         R=[("ub", sl), "identb"], W=[("PT", half)])
                if d4 % 2 == 0:
                    P.I("act", "copy", out=uT[sl][:, d4 * 4:(d4 + 1) * 4, c * 128:(c + 1) * 128], in_=PTs[half][:, 0:4, :],
                         R=[("PT", half)], W=[("uT", sl)])
                else:
                    P.I("dve", "tensor_copy", out=uT[sl][:, d4 * 4:(d4 + 1) * 4, c * 128:(c + 1) * 128], in_=PTs[half][:, 0:4, :],
                         R=[("PT", half)], W=[("uT", sl)])
        for tb in range(9):
            npart = 128 if tb < 8 else 16
            k = pjn["n"] % 2
            pjn["n"] += 1
            pj = PJ[k]
            s2 = it % 2
            it += 1
            for dc in range(16):
                P.I("pe", "matmul", pj[:npart, 0:EG], lhsT=xn2T[:, dc, tb * 128:tb * 128 + npart], rhs=uT[sl][:, dc, :],
                                                                                       start=(dc == 0), stop=(dc == 15),
                     R=[("uT", sl)], W=[("PJ", k)])
            P.I("act", "activation", out=gt32[s2][:npart], in_=pj[:npart, 0:EG], func=AF.Gelu_apprx_tanh, R=[("PJ", k)], W=[("gt32", s2)])
            P.I("dve", "tensor_tensor", out=actb[s2][:npart], in0=gt32[s2][:npart], in1=Gt[sl][:npart, tb, :], op=ALU.mult,
                 R=[("gt32", s2), ("Gt", sl)], W=[("actb", s2)])
            for c in range(NCH):
                P.I("pe", "transpose", out=PTs[s2][:, c, :npart], in_=actb[s2][:npart, c * 128:(c + 1) * 128], identity=identb[:npart, :npart],
                     R=[("actb", s2), "identb"], W=[("PT", s2)])
            P.I("act", "copy", out=actT[s2][:, :, :npart], in_=PTs[s2][:, 0:NCH, :npart], R=[("PT", s2)], W=[("actT", s2)])
            for dt_ in range(4):
                for c in range(NCH):
                    P.I("pe", "matmul", o2b[dt_][:npart, :], lhsT=actT[s2][:, c, :npart], rhs=vb[sl][:, c, dt_ * 512:(dt_ + 1) * 512],
                                                                                         start=(c == 0), stop=(c == NCH - 1),
                         R=[("actT", s2), ("vb", sl)], W=[("O2", dt_)])
            for dt_ in range(4):
                P.I("dve", "tensor_tensor", out=acc[:npart, tb, dt_ * 512:(dt_ + 1) * 512], in0=o2b[dt_][:npart, :], in1=acc[:npart, tb, dt_ * 512:(dt_ + 1) * 512], op=ALU.add,
                     R=[("O2", dt_)], W=[("O2", dt_), ("accD", tb, dt_)])
    P.end_phase()
    es_e.close()

    gBfin = P.psb([128, D], F32)
    sqE = P.psb([128, D], F32)
    outE = [P.psb([128, D], F32) for _ in range(2)]
    P.D("sp", out=gBfin[:], in_=g_fin.partition_broadcast(128), W=["gBfin"])
    for tb in range(9):
        npart = 128 if tb < 8 else 16
        ss = P.psb([128, 1], F32)
        rs = P.psb([128, 1], F32)
        ot = outE[tb % 2]
        P.I("act", "activation", out=sqE[:npart], in_=acc[:npart, tb, :], func=AF.Square, accum_out=ss[:npart], W=["sqE", ("ssE", tb)])
        P.I("act", "activation", out=rs[:npart], in_=ss[:npart], func=AF.Sqrt, scale=1.0 / D, bias=EPS, R=[("ssE", tb)], W=[("rsE", tb)])
        P.I("dve", "reciprocal", out=rs[:npart], in_=rs[:npart], R=[("rsE", tb)], W=[("rsE", tb)])
        P.I("dve", "scalar_tensor_tensor", out=ot[:npart], in0=acc[:npart, tb, :], scalar=rs[:npart, 0:1], in1=gBfin[:npart], op0=ALU.mult, op1=ALU.mult,
             R=[("rsE", tb), "gBfin"], W=[("outE", tb % 2)])
        dst = yp[tb * 128:(tb + 1) * 128, :] if tb < 8 else ys
        P.D("sp", out=dst, in_=ot[:npart], R=[("outE", tb % 2)])
    P.end_phase()
    es_d.close()
    return nc, P, dbg


def make_in_maps(inputs):
    f = lambda a: np.ascontiguousarray(np.asarray(a, dtype=np.float32))
    xpr = f(inputs["x_prompt"])
    xsm = f(inputs["x_sample"])
    shared = dict(
        w_in=f(inputs["w_in"][0]), w_out=f(inputs["w_out"][0]), w_q=f(inputs["peer_w_q"][0]), subk=f(inputs["peer_sub_keys"][0]),
        pu=f(inputs["peer_u"][0]), pv=f(inputs["peer_v"][0]),
        g_mix=f(inputs["norm_mix_g"][0]).reshape(1, D), g_sgu=f(inputs["sgu_norm_g"][0]).reshape(1, D),
        g_ffn=f(inputs["norm_ffn_g"][0]).reshape(1, D), g_fin=f(inputs["norm_final_g"]).reshape(1, D),
        sgu_w=f(inputs["sgu_w"][0]), sgu_b=f(inputs["sgu_b"][0]),
    )
    c128 = f(inputs["cache_kv_w128"][0]).reshape(32, 128, 2, D)
    c512 = f(inputs["cache_kv_w512"][0]).reshape(32, 512, 2, D)
    c2048 = f(inputs["cache_kv_w2048"][0]).reshape(32, 2048, 2, D)
    maps = []
    for c in range(8):
        b, th = c // 2, c % 2
        xpc = np.zeros((2048, D), np.float32)
        if th == 1:
            xpc[:] = xpr[b]
        else:
            xpc[1024:] = xpr[b, :1024]
        m = dict(shared)
        m.update(const_tables(th))
        m["xp"] = xpc
        m["xs"] = np.ascontiguousarray(xsm[4 * c:4 * c + 4].reshape(16, D))
        m["ck128"] = np.ascontiguousarray(c128[4 * c:4 * c + 4])
        m["ck512"] = np.ascontiguousarray(c512[4 * c:4 * c + 4])
        m["ck2048"] = np.ascontiguousarray(c2048[4 * c:4 * c + 4])
        maps.append(m)
    return maps


def assemble(results):
    y_prompt = np.zeros((4, 2048, D), np.float32)
    y_sample = np.zeros((32, 4, D), np.float32)
    kvp = [np.zeros((1, 4, w, 2, NH, DH), np.float32) for w in (128, 512, 2048)]
    kvs = [np.zeros((1, 32, 4, 2, NH, DH), np.float32) for _ in range(3)]
    sguv = np.zeros((1, 32, 4, D), np.float32)
    for c in range(8):
        r = results[c]
        b, th = c // 2, c % 2
        y_prompt[b, th * 1024:(th + 1) * 1024] = r["yp"]
        y_sample[4 * c:4 * c + 4] = r["ys"].reshape(4, 4, D)
        if th == 1:
            kvp[0][0, b] = r["kvp128"].reshape(128, 2, NH, DH)
            kvp[1][0, b] = r["kvp512"].reshape(512, 2, NH, DH)
        kvp[2][0, b, th * 1024:(th + 1) * 1024] = r["kvp2048"].reshape(1024, 2, NH, DH)
        for g, nm in enumerate(("kvs128", "kvs512", "kvs2048")):
            kvs[g][0, 4 * c:4 * c + 4] = r[nm].reshape(4, 4, 2, NH, DH)
        sguv[0, 4 * c:4 * c + 4] = r["sguv"].reshape(4, 4, D)
    return (y_prompt, y_sample, kvp[0], kvp[1], kvp[2], kvs[0], kvs[1], kvs[2], sguv)


_CACHE = {}


def kernel(**inputs):
    if "nc" not in _CACHE:
        _CACHE["nc"] = build()[0]
    nc = _CACHE["nc"]
    maps = make_in_maps(inputs)
    res = run_bass_kernel_spmd(nc, maps, core_ids=list(range(8)))
    return assemble(res.results)
```

```python
import contextlib
import math
import numpy as np
import concourse.bass as bass
import concourse.mybir as mybir
from concourse.bass_utils import run_bass_kernel_spmd

F32 = mybir.dt.float32
BF16 = mybir.dt.bfloat16
AF = mybir.ActivationFunctionType
ALU = mybir.AluOpType
AX = mybir.AxisListType

ENGS = ("pe", "act", "dve", "pool", "sp")
PSUM_RES = ("PJ", "SBK", "OB", "PT", "SC", "O2", "OBS")
NDSEM = 12

D = 2048
NH = 16
DH = 128
IN_COLS = 26624
SOFF = 2048
TOKS = 2048 + 16
BIG = 1.0e30
SCALE = DH ** -0.5
SQ = math.sqrt(DH)
EPS = 1e-6
DIL = (1, 4, 16)
NEXP = 16384


def alibi():
    e = np.arange(1, 49, dtype=np.float64)
    return np.exp2(-8.0 * e / 48).reshape(3, 16)


SLOPES = alibi()
A_BLOCKS = list(range(17))
B1_BUNDLES = [(hq, g) for hq in range(4) for g in range(3)]
B1_CUT = 99
import os
KNOB = os.environ.get('KNOB', '')
DBG_F32 = False


class Op:
    __slots__ = ("eng", "fn", "waits", "ticket", "needed", "is_dma", "idx")


class Prog:
    def __init__(self, nc):
        self.nc = nc
        self.es = contextlib.ExitStack()
        self.pes = contextlib.ExitStack()
        self.ops = {e: [] for e in ENGS}
        self.res = {}
        self.allops = []
        self.sem = {e: self.es.enter_context(nc.semaphore("s_" + e)) for e in ENGS}
        self.dsem = {}
        self.dcount = {}
        for q in ("sp", "act", "pool"):
            self.dsem[q] = [self.es.enter_context(nc.semaphore("d_%s%d" % (q, i))) for i in range(NDSEM)]
            self.dcount[q] = 0
        self.waited = {e: {} for e in ENGS}
        self.n_sb = 0
        self.cnt = {e: 0 for e in ENGS}
        self.dlast = {q: [None] * NDSEM for q in self.dsem}
        self.emitted = 0
        self.bar_start = 0
        self.ninstr = 0

    def _alloc(self, stack, shape, dt, psum=False):
        self.n_sb += 1
        name = "t%d" % self.n_sb
        if psum:
            return stack.enter_context(self.nc.psum_tensor(name, list(shape), dt))
        return stack.enter_context(self.nc.sbuf_tensor(name, list(shape), dt))

    def sb(self, shape, dt):
        return self._alloc(self.es, shape, dt)

    def psb(self, shape, dt):
        return self._alloc(self.pes, shape, dt)

    def ssb(self, stack, shape, dt):
        return self._alloc(stack, shape, dt)

    def ps(self, shape, dt):
        return self._alloc(self.es, shape, dt, psum=True)

    def _deps(self, reads, writes, eng=None):
        deps = []
        for r in reads:
            st = self.res.get(r)
            if st is not None and st[0] is not None:
                deps.append(st[0])
            if st is not None and isinstance(r, tuple) and r[0] in PSUM_RES:
                deps.extend(x for x in st[1] if x.eng != eng)
        for w in writes:
            st = self.res.get(w)
            if st is not None:
                if st[0] is not None:
                    deps.append(st[0])
                deps.extend(st[1])
        return deps

    def _commit(self, op, reads, writes):
        for r in reads:
            st = self.res.setdefault(r, [None, []])
            st[1].append(op)
        for w in writes:
            self.res[w] = [op, []]

    def op(self, eng, fn, reads=(), writes=()):
        o = Op()
        o.eng = eng
        o.fn = fn
        o.is_dma = False
        o.needed = False
        deps = self._deps(reads, writes, eng)
        o.waits = [d for d in deps if not (eng == "pe" and d.eng == "pe" and not d.is_dma)]
        for d in o.waits:
            d.needed = True
        self._commit(o, reads, writes)
        self.ops[eng].append(o)
        self.allops.append(o)
        return o

    def dma(self, q, fn, reads=(), writes=()):
        o = Op()
        o.eng = q
        o.fn = fn
        o.is_dma = True
        o.needed = True
        o.idx = self.dcount[q]
        self.dcount[q] += 1
        o.waits = list(self._deps(reads, writes))
        for d in o.waits:
            d.needed = True
        self._commit(o, reads, writes)
        self.ops[q].append(o)
        self.allops.append(o)
        return o

    def I(self, eng, meth, *args, R=(), W=(), **kw):
        return self.op(eng, (meth, args, kw), R, W)

    def D(self, q, out, in_, R=(), W=(), **kw):
        return self.dma(q, ("dma_start", (), dict(out=out, in_=in_, **kw)), R, W)

    def barrier(self):
        lastc = {}
        dm = {q: [] for q in self.dsem}
        for o in self.allops[self.bar_start:]:
            if o.fn is None:
                continue
            if o.is_dma:
                dm[o.eng].append(o)
            else:
                lastc[o.eng] = o
        self.bar_start = len(self.allops)
        deps = list(lastc.values())
        for q in dm:
            deps.extend(dm[q][-NDSEM:])
        for e in ENGS:
            o = Op()
            o.eng = e
            o.fn = None
            o.is_dma = False
            o.needed = False
            o.waits = [d for d in deps if not (e == "pe" and d.eng == "pe" and not d.is_dma)]
            for d in o.waits:
                d.needed = True
            self.ops[e].append(o)
            self.allops.append(o)
        self.res = {}

    def end_phase(self):
        self.barrier()
        self.emit()
        self.pes.close()
        self.pes = contextlib.ExitStack()

    def emit(self):
        nc = self.nc
        cnt = self.cnt
        dlast = self.dlast
        new_ops = self.allops[self.emitted:]
        self.emitted = len(self.allops)
        for o in new_ops:
            if o.is_dma:
                q = o.eng
                k = o.idx % NDSEM
                o.ticket = (("d", q, k), 16 * (o.idx // NDSEM + 1))
                prev = dlast[q][k]
                if prev is not None:
                    o.waits.append(prev)
                dlast[q][k] = o
            elif o.needed and o.fn is not None:
                cnt[o.eng] += 1
                o.ticket = (("c", o.eng), cnt[o.eng])
            else:
                o.ticket = None

        def semof(key):
            if key[0] == "c":
                return self.sem[key[1]]
            return self.dsem[key[1]][key[2]]

        def run(ename, eng):
            waited = self.waited[ename]
            ops = self.ops[ename]
            self.ops[ename] = []
            for o in ops:
                need = {}
                for d in o.waits:
                    key, val = d.ticket
                    if waited.get(key, 0) >= val:
                        continue
                    if need.get(key, 0) < val:
                        need[key] = val
                for key, val in need.items():
                    eng.wait_ge(semof(key), val)
                    waited[key] = val
                    self.ninstr += 1
                if o.fn is None:
                    continue
                if callable(o.fn):
                    ins = o.fn(eng)
                else:
                    meth, args, kw = o.fn
                    ins = getattr(eng, meth)(*args, **kw)
                self.ninstr += 1
                if o.ticket is not None:
                    key, val = o.ticket
                    ins.then_inc(semof(key), 16 if o.is_dma else 1)

        with nc.Block() as block:
            @block.tensor
            def _(e):
                run("pe", e)

            @block.scalar
            def _(e):
                run("act", e)

            @block.vector
            def _(e):
                run("dve", e)

            @block.gpsimd
            def _(e):
                run("pool", e)

            @block.sync
            def _(e):
                run("sp", e)


def const_tables(th):
    c = {}
    c["ident"] = np.eye(128, dtype=np.float32)
    j = np.arange(128)[:, None]
    i = np.arange(128)[None, :]
    bprev = np.where(i <= j, -(i - j + 128.0), -BIG)
    bcur = np.where(i >= j, -(i - j + 0.0), -BIG)
    c["btab"] = np.concatenate([bprev, bcur], axis=1).astype(np.float32)
    c["cmask"] = (j <= i).astype(np.float32)
    hbv = 0.0 if th == 1 else -BIG
    hb = np.zeros((128, 4), np.float32)
    hb[:, 0] = hbv
    hb[:64, 1] = hbv
    c["hb"] = hb
    st = np.full((128, 12, 16, 4), -BIG, np.float64)
    row = np.arange(128)
    for h in range(16):
        for q in range(4):
            d = 128 + q - row
            st[:, 0, h, q] = np.where(row >= q, -SLOPES[0, h] * d, -BIG)
            for r in range(4):
                if q == r:
                    st[:, 1 + r, h, q] = -SLOPES[1, h] * 4.0 * (128 - row)
                    st[:, 5 + r, h, q] = -SLOPES[2, h] * 16.0 * (128 - row)
            for kp in range(4):
                if kp <= q:
                    st[kp, 9, h, q] = -SLOPES[0, h] * (q - kp)
                if kp == q:
                    st[kp, 10, h, q] = 0.0
                    st[kp, 11, h, q] = 0.0
    c["stab"] = st.reshape(128, 12, 64).astype(np.float32)
    dm = np.zeros((16, 16, 16), np.float32)
    for p in range(16):
        dm[p, :, p] = 1.0
    c["dmask"] = dm.reshape(16, 256)
    cms = np.zeros((16, 16), np.float32)
    for b in range(4):
        for s in range(4):
            for t in range(4):
                if s <= t:
                    cms[4 * b + s, 4 * b + t] = 1.0
    c["cms"] = cms
    return c


def build(stop_after=None, debug=False):
    nc = bass.Bass("TRN2", target_bir_lowering=False)

    def di(n, s, dt=F32):
        return nc.dram_tensor(n, list(s), dt, kind="ExternalInput").ap()

    def do(n, s, dt=F32):
        return nc.dram_tensor(n, list(s), dt, kind="ExternalOutput").ap()

    def dscr(n, s, dt=F32):
        return nc.dram_tensor(n, list(s), dt, kind="Internal").ap()

    xp = di("xp", [2048, D])
    xs = di("xs", [16, D])
    if stop_after == "A":
        _di = di
        di = lambda n, s, dt=F32: (_di(n, s, dt) if n in ("g_mix", "ident") else None)
        dscr = lambda n, s, dt=F32: None
    w_in = di("w_in", [D, IN_COLS])
    w_out = di("w_out", [D, D])
    w_q = di("w_q", [D, D])
    subk = di("subk", [2, 128, 128])
    pu = di("pu", [NEXP, D])
    pv = di("pv", [NEXP, D])
    g_mix = di("g_mix", [1, D])
    g_sgu = di("g_sgu", [1, D])
    g_ffn = di("g_ffn", [1, D])
    g_fin = di("g_fin", [1, D])
    sgu_w = di("sgu_w", [8, 128, 128])
    sgu_b = di("sgu_b", [8, 128])
    ck = [di("ck128", [4, 128, 2, D]), di("ck512", [4, 512, 2, D]), di("ck2048", [4, 2048, 2, D])]
    ident_d = di("ident", [128, 128])
    btab_d = di("btab", [128, 256])
    cmask_d = di("cmask", [128, 128])
    hb_d = di("hb", [128, 4])
    stab_d = di("stab", [128, 12, 64])
    dmask_d = di("dmask", [16, 256])
    cms_d = di("cms", [16, 16])

    yp = do("yp", [1024, D])
    ys = do("ys", [16, D])
    kvp = [do("kvp128", [128, 2, D]), do("kvp512", [512, 2, D]), do("kvp2048", [1024, 2, D])]
    kvs = [do("kvs128", [16, 2, D]), do("kvs512", [16, 2, D]), do("kvs2048", [16, 2, D])]
    sguv = do("sguv", [16, D])

    OG = dscr("OG", [3, 1024, 16, 129])
    sqd = dscr("sqd", [3, 3, 16, D])
    Gd = dscr("Gd", [9, 128, NEXP], BF16)
    mgd = dscr("mgd", [9, 128, D], BF16)
    dbg = {}

    P = Prog(nc)
    PJ = [P.ps([128, 512], F32) for _ in range(2)]
    SBK = [P.ps([128, 512], F32) for _ in range(2)]
    OB = [P.ps([128, 512], F32) for _ in range(2)]
    PTs = [P.ps([128, 8, 128], BF16) for _ in range(2)]

    identf = P.sb([128, 128], F32)
    identb = P.sb([128, 128], BF16)
    onesb = P.sb([128, 16], BF16)
    zcol = P.sb([128, 1], F32)

    P.D("sp", out=identf[:], in_=ident_d, W=["identf"])
    P.I("dve", "tensor_copy", out=identb[:], in_=identf[:], R=["identf"], W=["identb"])
    P.I("dve", "memset", onesb[:], 1.0, W=["onesb"])
    P.I("dve", "memset", zcol[:], 0.0, W=["zcol"])

    bouts = P.sb([16, D], F32)
    es_ab = contextlib.ExitStack()
    xnT = P.ssb(es_ab, [128, 16, TOKS], BF16)
    wbs = [P.ssb(es_ab, [128, 16, 512], BF16) for _ in range(2)]
    wstate = {"n": 0}
    tcount = {"n": 0}

    def load_w(src, col0, extra=None):
        bufs = wbs + ([extra] if extra is not None else [])
        k = wstate["n"] % len(bufs)
        wstate["n"] += 1
        wb = bufs[k]
        P.D("pool", out=wb[:], in_=src[:, col0:col0 + 512].rearrange("(c p) n -> p c n", p=128),
              W=[("wb", id(wb))])
        return wb

    def rmsnorm_T(xt, npart, gB, xnb, dstT, col0, tagp):
        k = tcount["n"]
        tcount["n"] += 1
        ss = P.psb([128, 1], F32)
        rs = P.psb([128, 1], F32)
        sq = xnb["sq"]
        P.I("act", "activation", out=sq[:npart], in_=xt[:npart], func=AF.Square, accum_out=ss[:npart],
             R=[tagp + "xt"], W=["sqj", ("ss", k)])
        P.I("act", "activation", out=rs[:npart], in_=ss[:npart], func=AF.Sqrt, scale=1.0 / D, bias=EPS,
             R=[("ss", k)], W=[("rs", k)])
        P.I("dve", "reciprocal", out=rs[:npart], in_=rs[:npart], R=[("rs", k)], W=[("rs", k)])
        xb = xnb["b"][k % 2]
        P.I("dve", "scalar_tensor_tensor", out=xb[:npart], in0=xt[:npart], scalar=rs[:npart, 0:1], in1=gB[:npart],
                                                     op0=ALU.mult, op1=ALU.mult,
             R=[tagp + "xt", ("rs", k), "gB"], W=[("xnb", k % 2)])
        for c4 in range(4):
            half = (k * 4 + c4) % 2
            for j in range(4):
                c = c4 * 4 + j
                P.I("pe", "transpose", out=PTs[half][:, j, :npart], in_=xb[:npart, c * 128:(c + 1) * 128],
                                                                       identity=identb[:npart, :npart],
                     R=[("xnb", k % 2), "identb"], W=[("PT", half)])
            eng = "act" if c4 % 2 == 0 else "dve"
            if eng == "act":
                P.I("act", "copy", out=dstT[:, c4 * 4:(c4 + 1) * 4, col0:col0 + npart], in_=PTs[half][:, 0:4, :npart],
                     R=[("PT", half)], W=[("dstT", id(dstT), col0)])
            else:
                P.I("dve", "tensor_copy", out=dstT[:, c4 * 4:(c4 + 1) * 4, col0:col0 + npart], in_=PTs[half][:, 0:4, :npart],
                     R=[("PT", half)], W=[("dstT", id(dstT), col0)])

    xts = [P.psb([128, D], F32) for _ in range(2)]
    xnbA = {"sq": P.psb([128, D], BF16), "b": [P.psb([128, D], BF16) for _ in range(2)]}
    gBmix = P.psb([128, D], F32)
    P.D("sp", out=gBmix[:], in_=g_mix.partition_broadcast(128), W=["gB"])
    for blk in A_BLOCKS:
        xt = xts[blk % 2]
        npart = 128 if blk < 16 else 16
        src = xp[blk * 128:(blk + 1) * 128, :] if blk < 16 else xs
        tag = "A%d" % (blk % 2)
        P.D("sp", out=xt[:npart], in_=src, W=[tag + "xt"])
        rmsnorm_T(xt, npart, gBmix, xnbA, xnT, blk * 128, tag)
    if debug:
        if DBG_F32:
            xnT32 = P.psb([128, 16, 256], F32)
            P.I("dve", "tensor_copy", out=xnT32[:], in_=xnT[:, :, 0:256], R=[("dstT", id(xnT), c * 128) for c in range(17)], W=["xnT32"])
            dbg["xnT"] = do("dbg_xnT32", [128, 16, 256], F32)
            P.D("sp", out=dbg["xnT"], in_=xnT32[:], R=["xnT32"])
        else:
            dbg["xnT"] = do("dbg_xnT", [128, 16, TOKS], BF16)
            P.D("sp", out=dbg["xnT"], in_=xnT[:], R=[("dstT", id(xnT), c * 128) for c in range(17)])
    P.end_phase()
    if stop_after == "A":
        return nc, P, dbg

    wb3 = P.psb([128, 16, 512], BF16)
    qT = P.psb([128, 4, 2048], BF16)
    kT = P.psb([128, 4, 2048], BF16)
    vsb = P.psb([128, 16, 512], BF16)
    tqs = [P.psb([128, 512], BF16) for _ in range(2)]
    st32 = [P.psb([128, 512], F32) for _ in range(3)]
    Sbuf = [P.psb([128, 256], F32) for _ in range(2)]
    Ptb_ = [P.psb([128, 256], BF16) for _ in range(2)]
    ostg = [P.psb([128, 4, 129], F32) for _ in range(2)]
    btab = P.psb([128, 256], F32)
    hb = P.psb([128, 4], F32)
    P.D("sp", out=btab[:], in_=btab_d, W=["btab"])
    P.D("sp", out=hb[:], in_=hb_d, W=["hb"])

    pjn = {"n": 0}

    def proj_block(wb, start, step, npart):
        k = pjn["n"] % 2
        pjn["n"] += 1
        pj = PJ[k]
        for dc in range(16):
            if npart == 128:
                lt = xnT[:, dc, start:start + 128 * step:step]
            else:
                lt = xnT[:, dc, start:start + npart]
            P.I("pe", "matmul", pj[:npart, :], lhsT=lt, rhs=wb[:, dc, :], start=(dc == 0), stop=(dc == 15),
                 R=[("wb", id(wb)), "xnT"], W=[("PJ", k)])
        return pj, k

    KV_BLOCKS = [
        [(128 * lb, 1) for lb in range(7, 16)],
        [(rho + 4 * 128 * sb, 4) for rho in range(4) for sb in range(1, 4)],
        [(r, 16) for r in range(16)],
    ]
    Q_BLOCKS = [
        [(128 * lb, 1) for lb in range(8, 16)],
        [(rho + 4 * 128 * sb, 4) for rho in range(4) for sb in range(2, 4)],
        [(r, 16) for r in range(16)],
    ]

    def kv_out_rows(g, bi):
        if g == 0:
            if bi == 8:
                return slice(0, 128), kvp[0][0:128]
            return None
        if g == 1:
            rho, sbi = bi // 3, bi % 3
            if sbi == 2:
                return slice(0, 128), kvp[1][rho:rho + 509:4]
            return None
        r = bi
        return slice(64, 128), kvp[2][r:r + 1009:16]

    tn = {"n": 0, "s": 0}

    def to_T(pj, pk, dst, col0):
        k = tn["n"] % 2
        tn["n"] += 1
        tq = tqs[k]
        P.I("act", "copy", out=tq[:], in_=pj[:], R=[("PJ", pk)], W=[("tq", k)])
        for j in range(4):
            P.I("pe", "transpose", out=PTs[k][:, j, :], in_=tq[:, j * 128:(j + 1) * 128], identity=identb[:],
                 R=[("tq", k), "identb"], W=[("PT", k)])
        P.I("dve", "tensor_copy", out=dst[:, :, col0:col0 + 128], in_=PTs[k][:, 0:4, :],
             R=[("PT", k)], W=[("T", id(dst))])

    def stage32(pj, pk, npart):
        k = tn["s"] % 3
        tn["s"] += 1
        st = st32[k]
        if True:
            P.I("act", "copy", out=st[:npart], in_=pj[:npart], R=[("PJ", pk)], W=[("st32", k)])
        else:
            P.I("dve", "tensor_copy", out=st[:npart], in_=pj[:npart], R=[("PJ", pk)], W=[("st32", k)])
        return st, k

    def attention(g, hq):
        units = []
        if g == 0:
            for qb in range(8):
                units.append((qb * 128, 128, [(qb, 0, (0 if qb == 0 else 2)), (qb + 1, 128, 2)], OG[0, qb * 128:(qb + 1) * 128]))
        elif g == 1:
            for rho in range(4):
                for sbi in range(2):
                    qi = rho * 2 + sbi
                    r0 = rho + 512 * sbi
                    units.append((qi * 128, 128, [(rho * 3 + sbi, 0, (0 if sbi == 0 else 2)), (rho * 3 + sbi + 1, 128, 2)],
                                  OG[1, r0:r0 + 509:4]))
        else:
            for r in range(16):
                units.append((r * 128 + 64, 64, [(r, 128 + 64, 1)], OG[2, r:r + 1009:16]))
        for ui, (qc0, nq, tiles, orow) in enumerate(units):
            osl = ui % 2
            for h in range(4):
                head = hq * 4 + h
                cgh = float(SLOPES[g, head] * DIL[g] * SQ)
                sl = (ui * 4 + h) % 2
                Sps = SBK[sl]
                nt = len(tiles)
                for j, (kidx, tcol, hbsel) in enumerate(tiles):
                    P.I("pe", "matmul", Sps[:, j * 128:j * 128 + nq], lhsT=kT[:, h, kidx * 128:(kidx + 1) * 128],
                                                                                rhs=qT[:, h, qc0:qc0 + nq], start=True, stop=True,
                         R=[("T", id(kT)), ("T", id(qT))], W=[("SBK", sl)])
                Sb = Sbuf[sl]
                if nt == 2:
                    P.I("dve", "scalar_tensor_tensor", out=Sb[:, 0:256], in0=btab[:, 0:256], scalar=cgh, in1=Sps[:, 0:256],
                                                                                         op0=ALU.mult, op1=ALU.add,
                         R=["btab", ("SBK", sl)], W=[("Sbuf", sl)])
                else:
                    tcol = tiles[0][1]
                    P.I("dve", "scalar_tensor_tensor", out=Sb[:, 0:nq], in0=btab[:, tcol:tcol + nq], scalar=cgh,
                                                                                                    in1=Sps[:, 0:nq], op0=ALU.mult, op1=ALU.add,
                         R=["btab", ("SBK", sl)], W=[("Sbuf", sl)])
                Pt = Ptb_[sl]
                for j, (kidx, tcol, hbsel) in enumerate(tiles):
                    P.I("act", "activation", out=Pt[:, j * 128:j * 128 + nq], in_=Sb[:, j * 128:j * 128 + nq], func=AF.Exp,
                                                                                      bias=hb[:, hbsel:hbsel + 1], scale=SCALE,
                         R=[("Sbuf", sl), "hb"], W=[("Pt", sl, j)])
                for j, (kidx, tcol, hbsel) in enumerate(tiles):
                    P.I("pe", "matmul", OB[osl][:nq, h * 128:(h + 1) * 128], lhsT=Pt[:, j * 128:j * 128 + nq],
                                                                             rhs=vsb[:, kidx, h * 128:(h + 1) * 128], start=(j == 0), stop=(j == nt - 1),
                         R=[("Pt", sl, j), "vsb"], W=[("OB", osl)])
                    P.I("pe", "matmul", PJ[osl][:nq, h:h + 1], lhsT=Pt[:, j * 128:j * 128 + nq],
                                                                   rhs=onesb[:, 0:1], start=(j == 0), stop=(j == nt - 1),
                         R=[("Pt", sl, j), "onesb"], W=[("PJ", osl)])
            og = ostg[osl]
            P.I("act", "copy", out=og[:nq, :, 0:128], in_=OB[osl][:nq, :].rearrange("p (h d) -> p h d", h=4),
                 R=[("OB", osl)], W=[("ostg", osl)])
            P.I("dve", "tensor_copy", out=og[:nq, :, 128], in_=PJ[osl][:nq, 0:4],
                 R=[("PJ", osl)], W=[("ostg", osl)])
            P.D("sp", out=orow[:, hq * 4:(hq + 1) * 4, :], in_=og[:nq],
                  R=[("ostg", osl)], W=["OG"])

    for (hq, g) in B1_BUNDLES:
        for _once in (0,):
            base = 4096 + g * 6144 + hq * 512
            wb = load_w(w_in, base, wb3)
            for bi, (st, sp_) in enumerate(Q_BLOCKS[g]):
                pj, pk = proj_block(wb, st, sp_, 128)
                to_T(pj, pk, qT, bi * 128)
            if B1_CUT < 2:
                continue
            pj, pk = proj_block(wb, SOFF, 1, 16)
            s32, sk = stage32(pj, pk, 16)
            P.D("sp", out=sqd[g, 0, :, hq * 512:(hq + 1) * 512], in_=s32[:16],
                  R=[("st32", sk)], W=["sqd"])
            if B1_CUT < 3:
                continue
            wb = load_w(w_in, base + 2048, wb3)
            for bi, (st, sp_) in enumerate(KV_BLOCKS[g]):
                pj, pk = proj_block(wb, st, sp_, 128)
                kvo = kv_out_rows(g, bi) if 'nokvout' not in KNOB else None
                if kvo is not None:
                    psl, rows = kvo
                    s32, sk = stage32(pj, pk, 128)
                    P.D("sp", out=rows[:, 0, hq * 512:(hq + 1) * 512], in_=s32[psl],
                          R=[("st32", sk)])
                to_T(pj, pk, kT, bi * 128)
            if 'nosample' in KNOB:
                continue
            pj, pk = proj_block(wb, SOFF, 1, 16)
            s32, sk = stage32(pj, pk, 16)
            if 'nosqd' not in KNOB:
                P.D("sp", out=sqd[g, 1, :, hq * 512:(hq + 1) * 512], in_=s32[:16],
                      R=[("st32", sk)], W=["sqd"])
            if 'nokvs' not in KNOB:
                P.D("sp", out=kvs[g][:, 0, hq * 512:(hq + 1) * 512], in_=s32[:16],
                      R=[("st32", sk)])
            if B1_CUT < 4:
                continue
            wb = load_w(w_in, base + 4096, wb3)
            for bi, (st, sp_) in enumerate(KV_BLOCKS[g]):
                pj, pk = proj_block(wb, st, sp_, 128)
                kvo = kv_out_rows(g, bi)
                if kvo is not None:
                    psl, rows = kvo
                    s32, sk = stage32(pj, pk, 128)
                    P.D("sp", out=rows[:, 1, hq * 512:(hq + 1) * 512], in_=s32[psl],
                          R=[("st32", sk)])
                P.I("act", "copy", out=vsb[:, bi, :], in_=pj[:], R=[("PJ", pk)], W=["vsb"])
            pj, pk = proj_block(wb, SOFF, 1, 16)
            s32, sk = stage32(pj, pk, 16)
            P.D("sp", out=sqd[g, 2, :, hq * 512:(hq + 1) * 512], in_=s32[:16],
                  R=[("st32", sk)], W=["sqd"])
            P.D("sp", out=kvs[g][:, 1, hq * 512:(hq + 1) * 512], in_=s32[:16],
                  R=[("st32", sk)])
            if B1_CUT < 5:
                continue
            attention(g, hq)
    if debug and B1_CUT >= 6:
        dbg["OG"] = do("dbg_OG", [3, 1024, 16, 129])
        for g in range(3):
            P.D("sp", out=dbg["OG"][g], in_=OG[g], R=["OG"])
    P.end_phase()
    if stop_after == "B1":
        return nc, P, dbg

    stab = P.psb([128, 12, 64], F32)
    dmask = P.psb([16, 256], F32)
    P.D("sp", out=stab[:], in_=stab_d, W=["stab"])
    P.D("sp", out=dmask[:], in_=dmask_d, W=["dmask"])
    qload = P.psb([16, D], F32)
    qbf = P.psb([16, D], BF16)
    qTs = P.psb([128, 3, 16, 16], BF16)
    kTn = P.psb([128, 3, 16, 16], BF16)
    for g in range(3):
        for kind, dst in ((0, qTs), (1, kTn)):
            P.D("sp", out=qload[:], in_=sqd[g, kind], R=["sqd"], W=["qload"])
            P.I("dve", "tensor_copy", out=qbf[:], in_=qload[:], R=["qload"], W=["qbf"])
            for h4 in range(4):
                for j in range(4):
                    h = h4 * 4 + j
                    P.I("pe", "transpose", out=PTs[0][:, j, 0:16], in_=qbf[0:16, h * 128:(h + 1) * 128], identity=identb[0:16, 0:16],
                         R=["qbf", "identb"], W=[("PT", 0)])
                P.I("act", "copy", out=dst[:, g, h4 * 4:(h4 + 1) * 4, :], in_=PTs[0][:, 0:4, 0:16],
                     R=[("PT", 0)], W=[("qk", id(dst))])
    Oacc = P.psb([16, D], F32)
    Lacc = P.psb([16, 256], F32)
    P.I("dve", "memset", Oacc[:], 0.0, W=["Oacc"])
    P.I("dve", "memset", Lacc[:], 0.0, W=["Lacc"])
    PtS = [P.psb([128, 16, 16], BF16) for _ in range(4)]
    for b in range(4):
        P.I("pool", "memset", PtS[b][:], 0.0, W=[("PtS", b)])
    kvc = [P.psb([128, 2, D], BF16) for _ in range(2)]
    kTc = [P.psb([128, 16, 128], BF16) for _ in range(2)]
    SbS = [P.psb([128, 64], F32) for _ in range(2)]
    tl = 0
    for b in range(4):
        for g in range(3):
            tiles = []
            if g == 0:
                tiles.append((128, ck[0][b], 0))
            elif g == 1:
                v4 = ck[1][b].rearrange("(m r) k c -> r m k c", r=4)
                for r in range(4):
                    tiles.append((128, v4[r], 1 + r))
            else:
                v16 = ck[2][b].rearrange("(m r) k c -> r m k c", r=16)
                for r in range(4):
                    tiles.append((128, v16[r], 5 + r))
            tiles.append((4, None, 9 + g))
            for (nk, src, tbl) in tiles:
                sl = tl % 2
                tl += 1
                kv_ = kvc[sl]
                kt_ = kTc[sl]
                if src is not None:
                    P.D("pool", out=kv_[:], in_=src, W=[("kvc", sl)], max_dma_last_dim=4096)
                else:
                    P.D("pool", out=kv_[0:4, 0, :], in_=sqd[g, 1, 4 * b:4 * b + 4, :], R=["sqd"], W=[("kvc", sl)], max_dma_last_dim=4096)
                    P.D("pool", out=kv_[0:4, 1, :], in_=sqd[g, 2, 4 * b:4 * b + 4, :], R=["sqd"], W=[("kvc", sl)], max_dma_last_dim=4096)
                for h4 in range(4):
                    half = h4 % 2
                    for j in range(4):
                        h = h4 * 4 + j
                        P.I("pe", "transpose", out=PTs[half][:, j, 0:nk], in_=kv_[0:nk, 0, h * 128:(h + 1) * 128],
                                                                                             identity=identb[0:nk, 0:nk],
                             R=[("kvc", sl), "identb"], W=[("PT", half)])
                    P.I("act", "copy", out=kt_[:, h4 * 4:(h4 + 1) * 4, 0:nk], in_=PTs[half][:, 0:4, 0:nk],
                         R=[("PT", half)], W=[("kTc", sl)])
                Sps = PJ[sl]
                for h in range(16):
                    P.I("pe", "matmul", Sps[0:nk, h * 4:(h + 1) * 4], lhsT=kt_[:, h, 0:nk],
                                                                                          rhs=qTs[:, g, h, 4 * b:4 * b + 4], start=True, stop=True,
                         R=[("kTc", sl), ("qk", id(qTs))], W=[("PJ", sl)])
                Sb = SbS[sl]
                P.I("dve", "scalar_tensor_tensor", out=Sb[0:nk, :], in0=Sps[0:nk, 0:64], scalar=SCALE, in1=stab[0:nk, tbl, :],
                                                                                             op0=ALU.mult, op1=ALU.add,
                     R=[("PJ", sl), "stab"], W=[("SbS", sl)])
                pts = PtS[b]
                P.I("act", "activation", out=pts[0:nk, :, 4 * b:4 * b + 4], in_=Sb[0:nk, :].rearrange("p (h q) -> p h q", h=16),
                                                                               func=AF.Exp,
                     R=[("SbS", sl)], W=[("PtS", b)])
                obanks = [OB[0], OB[1], SBK[0], SBK[1]]
                for h in range(16):
                    ob = obanks[h // 4]
                    P.I("pe", "matmul", ob[0:16, (h % 4) * 128:(h % 4 + 1) * 128], lhsT=pts[0:nk, h, :],
                                                                                       rhs=kv_[0:nk, 1, h * 128:(h + 1) * 128], start=True, stop=True,
                         R=[("PtS", b), ("kvc", sl)], W=[("OBS", h // 4)])
                P.I("pe", "matmul", PJ[sl][0:16, 256:512], lhsT=onesb[0:nk, 0:16], rhs=pts[0:nk].rearrange("p h q -> p (h q)"), start=True, stop=True,
                     R=[("PtS", b), "onesb"], W=[("PJ", sl)])
                for k4 in range(4):
                    ob = obanks[k4]
                    P.I("dve", "tensor_tensor", out=Oacc[:, k4 * 512:(k4 + 1) * 512], in0=ob[0:16, :], in1=Oacc[:, k4 * 512:(k4 + 1) * 512], op=ALU.add,
                         R=[("OBS", k4), "Oacc"], W=["Oacc", ("OBS", k4)])
                P.I("dve", "tensor_tensor", out=Lacc[:], in0=PJ[sl][0:16, 256:512], in1=Lacc[:], op=ALU.add, R=[("PJ", sl), "Lacc"], W=["Lacc"])
    ltmp = P.psb([16, 256], F32)
    lsum = P.psb([16, 16], F32)
    P.I("dve", "tensor_tensor", out=ltmp[:], in0=Lacc[:], in1=dmask[:], op=ALU.mult, R=["Lacc", "dmask"], W=["ltmp"])
    P.I("dve", "tensor_reduce", out=lsum[:], in_=ltmp[:].rearrange("p (h q) -> p h q", h=16), axis=AX.X, op=ALU.add, R=["ltmp"], W=["lsum"])
    P.I("dve", "reciprocal", out=lsum[:], in_=lsum[:], R=["lsum"], W=["lsum"])
    P.I("dve", "tensor_tensor", out=bouts[:].rearrange("p (h d) -> p h d", h=16), in0=Oacc[:].rearrange("p (h d) -> p h d", h=16),
                                          in1=lsum[:, :, None].to_broadcast([16, 16, 128]), op=ALU.mult,
         R=["Oacc", "lsum"], W=["bouts"])
    if debug:
        dbg["bouts"] = do("dbg_bouts", [16, D])
        P.D("sp", out=dbg["bouts"], in_=bouts[:], R=["bouts"])
    P.end_phase()
    if stop_after == "B1s":
        return nc, P, dbg

    vm = P.psb([128, 8, D], BF16)
    vms = P.psb([16, D], BF16)
    vsf = P.psb([16, D], F32)
    vsb16 = P.psb([16, D], BF16)
    gBsgu = P.psb([128, D], F32)
    cmask = P.psb([128, 128], F32)
    cms = P.psb([16, 16], F32)
    wst = P.psb([128, 128], F32)
    WcT = P.psb([128, 8, 128], BF16)
    WsT = P.psb([16, 8, 16], BF16)
    Wtmp = P.psb([16, 8, 16], F32)
    bT = P.psb([128, 8], F32)
    bsT = P.psb([16, 8], F32)
    sbl = P.psb([8, 128], F32)
    stats = P.psb([128, 9, 4, 6], F32)
    mv = P.psb([128, 9, 2], F32)
    utmp = [P.psb([128, 512], F32) for _ in range(2)]
    t2048 = P.psb([128, D], F32)
    ogt = [P.psb([128, 3, 4, 129], F32) for _ in range(2)]
    num = P.psb([128, 4, 129], F32)
    rl = P.psb([128, 4], F32)
    bt = P.psb([128, 512], F32)
    P.D("sp", out=gBsgu[:], in_=g_sgu.partition_broadcast(128), W=["gBsgu"])
    P.D("sp", out=cmask[:], in_=cmask_d, W=["cmask"])
    P.D("sp", out=cms[:], in_=cms_d, W=["cms"])
    P.D("sp", out=sbl[:], in_=sgu_b, W=["sbl"])
    for g in range(8):
        P.D("sp", out=wst[:], in_=sgu_w[g], W=["wst"])
        P.I("pe", "transpose", out=SBK[0][:, 0:128], in_=wst[:], identity=identf[:], R=["wst", "identf"], W=[("SBK", 0)])
        P.I("dve", "tensor_tensor", out=WcT[:, g, :], in0=SBK[0][:, 0:128], in1=cmask[:], op=ALU.mult,
             R=[("SBK", 0), "cmask"], W=["WcT"])
    P.I("pe", "transpose", out=SBK[1][:, 0:8], in_=sbl[0:8, :], identity=identf[0:8, 0:8], R=["sbl", "identf"], W=[("SBK", 1)])
    P.I("dve", "tensor_copy", out=bT[:], in_=SBK[1][:, 0:8], R=[("SBK", 1)], W=["bT"])
    P.I("dve", "memset", WsT[:], 0.0, W=["WsT"])
    for b in range(4):
        P.D("sp", out=WsT[4 * b:4 * b + 4, :, 4 * b:4 * b + 4], in_=WcT[0:4, :, 0:4], R=["WcT", "WsT"], W=["WsT"])
        P.D("sp", out=bsT[4 * b:4 * b + 4, :], in_=bT[0:4, :], R=["bT"], W=["bsT"])

    TB = list(range(8)) + ["s"]

    def tok_block(tb):
        if tb == "s":
            return SOFF, 16
        return 1024 + tb * 128, 128

    for ct in range(4):
        wb = load_w(w_in, 2048 + ct * 512)
        for ti, tb in enumerate(TB):
            st, npart = tok_block(tb)
            pj, pk = proj_block(wb, st, 1, npart)
            if tb == "s":
                P.I("act", "activation", out=vsf[:, ct * 512:(ct + 1) * 512], in_=pj[:16], func=AF.Gelu_apprx_tanh,
                     R=[("PJ", pk)], W=[("vsf", ct)])
                P.I("dve", "bn_stats", out=stats[:16, 8, ct, :], in_=vsf[:, ct * 512:(ct + 1) * 512], R=[("vsf", ct)], W=[("stats", 8)])
            else:
                P.I("act", "activation", out=vm[:, tb, ct * 512:(ct + 1) * 512], in_=pj[:], func=AF.Gelu_apprx_tanh,
                     R=[("PJ", pk)], W=[("vm", tb, ct)])
                P.I("dve", "bn_stats", out=stats[:, tb, ct, :], in_=vm[:, tb, ct * 512:(ct + 1) * 512], R=[("vm", tb, ct)], W=[("stats", tb)])
    for ti, tb in enumerate(TB):
        npart = 16 if tb == "s" else 128
        P.I("dve", "bn_aggr", out=mv[:npart, ti, :], in_=stats[:npart, ti].rearrange("p a b -> p (a b)"),
             R=[("stats", ti)], W=[("mv", ti)])
        P.I("act", "activation", out=mv[:npart, ti, 1:2], in_=mv[:npart, ti, 1:2], func=AF.Sqrt, bias=EPS, scale=1.0,
             R=[("mv", ti)], W=[("mv", ti)])
        P.I("dve", "reciprocal", out=mv[:npart, ti, 1:2], in_=mv[:npart, ti, 1:2], R=[("mv", ti)], W=[("mv", ti)])
        src = vsf[:] if tb == "s" else vm[:, tb, :]
        rtag = [("vsf", c) for c in range(4)] if tb == "s" else [("vm", tb, c) for c in range(4)]
        P.I("dve", "scalar_tensor_tensor", out=t2048[:npart], in0=src, scalar=mv[:npart, ti, 0:1], in1=gBsgu[:npart],
                                                                                 op0=ALU.subtract, op1=ALU.mult,
             R=rtag + [("mv", ti), "gBsgu"], W=["t2048"])
        if tb == "s":
            P.I("act", "activation", out=vsf[:], in_=t2048[:16], func=AF.Identity, scale=mv[:16, ti, 1:2], R=["t2048", ("mv", ti)], W=rtag)
            P.I("dve", "tensor_copy", out=vsb16[:], in_=vsf[:], R=rtag, W=["vsb16"])
            P.D("sp", out=sguv, in_=vsf[:], R=rtag)
        else:
            P.I("act", "activation", out=vm[:, tb, :], in_=t2048[:], func=AF.Identity, scale=mv[:, ti, 1:2], R=["t2048", ("mv", ti)], W=rtag)
    for ct in range(4):
        wb = load_w(w_in, ct * 512)
        for ti, tb in enumerate(TB):
            st, npart = tok_block(tb)
            pj, pk = proj_block(wb, st, 1, npart)
            ut = utmp[ti % 2]
            P.I("act", "activation", out=ut[:npart], in_=pj[:npart], func=AF.Gelu_apprx_tanh,
                 R=[("PJ", pk)], W=[("utmp", ti % 2)])
            sl = ti % 2
            for gg in range(2):
                g = ct * 2 + gg
                if tb == "s":
                    P.I("pe", "matmul", SBK[sl][0:16, gg * 256:(gg + 1) * 256], lhsT=WsT[:, g, :], rhs=vsb16[:, g * 256:(g + 1) * 256], start=True, stop=True,
                         R=["WsT", "vsb16"], W=[("SBK", sl)])
                else:
                    P.I("pe", "matmul", SBK[sl][:, gg * 256:(gg + 1) * 256], lhsT=WcT[:, g, :], rhs=vm[:, tb, g * 256:(g + 1) * 256], start=True, stop=True,
                         R=["WcT", ("vm", tb, ct)], W=[("SBK", sl)])
            for gg in range(2):
                g = ct * 2 + gg
                if tb == "s":
                    P.I("dve", "scalar_tensor_tensor", out=vms[:, g * 256:(g + 1) * 256], in0=SBK[sl][0:16, gg * 256:(gg + 1) * 256], scalar=bsT[:, g:g + 1],
                                                                                          in1=ut[:16, gg * 256:(gg + 1) * 256], op0=ALU.add, op1=ALU.mult,
                         R=[("SBK", sl), "bsT", ("utmp", ti % 2)], W=[("vms", ct)])
                else:
                    P.I("dve", "scalar_tensor_tensor", out=vm[:, tb, g * 256:(g + 1) * 256], in0=SBK[sl][:, gg * 256:(gg + 1) * 256], scalar=bT[:, g:g + 1],
                                                                                                 in1=ut[:, gg * 256:(gg + 1) * 256], op0=ALU.add, op1=ALU.mult,
                         R=[("SBK", sl), "bT", ("utmp", ti % 2)], W=[("vm", tb, ct)])
    if debug:
        dbg["aout"] = do("dbg_aout", [128, 8, D], BF16)
        P.D("sp", out=dbg["aout"], in_=vm[:], R=[("vm", tb, c) for tb in range(8) for c in range(4)])
    gbase = 4096 + 3 * 6144
    for ct in range(4):
        wb = load_w(w_in, gbase + ct * 512)
        for ti, tb in enumerate(TB):
            st, npart = tok_block(tb)
            pj, pk = proj_block(wb, st, 1, npart)
            ut = utmp[ti % 2]
            P.I("act", "activation", out=ut[:npart], in_=pj[:npart], func=AF.Sigmoid, R=[("PJ", pk)], W=[("utmp", ti % 2)])
            if tb == "s":
                P.I("dve", "tensor_tensor", out=vms[:, ct * 512:(ct + 1) * 512], in0=ut[:16], in1=vms[:, ct * 512:(ct + 1) * 512], op=ALU.mult,
                     R=[("utmp", ti % 2), ("vms", ct)], W=[("vms", ct)])
            else:
                P.I("dve", "tensor_tensor", out=vm[:, tb, ct * 512:(ct + 1) * 512], in0=ut[:], in1=vm[:, tb, ct * 512:(ct + 1) * 512], op=ALU.mult,
                     R=[("utmp", ti % 2), ("vm", tb, ct)], W=[("vm", tb, ct)])
    for ct in range(4):
        wb = load_w(w_in, gbase + 2048 + ct * 512)
        for ti, tb in enumerate(TB):
            st, npart = tok_block(tb)
            pj, pk = proj_block(wb, st, 1, npart)
            ut = utmp[ti % 2]
            P.I("act", "activation", out=ut[:npart], in_=pj[:npart], func=AF.Sigmoid, R=[("PJ", pk)], W=[("utmp", ti % 2)])
            if tb == "s":
                P.I("dve", "tensor_tensor", out=bt[:16], in0=ut[:16], in1=bouts[:, ct * 512:(ct + 1) * 512], op=ALU.mult,
                     R=[("utmp", ti % 2)], W=["bt"])
                P.I("dve", "tensor_tensor", out=vms[:, ct * 512:(ct + 1) * 512], in0=bt[:16], in1=vms[:, ct * 512:(ct + 1) * 512], op=ALU.add,
                     R=["bt", ("vms", ct)], W=[("vms", ct)])
            else:
                og = ogt[ti % 2]
                P.D("sp", out=og[:], in_=OG[:, tb * 128:(tb + 1) * 128, ct * 4:(ct + 1) * 4, :].rearrange("g p h d -> p g h d"),
                      W=[("ogt", ti % 2)])
                P.I("dve", "tensor_tensor", out=num[:], in0=og[:, 0], in1=og[:, 1], op=ALU.add, R=[("ogt", ti % 2)], W=["num"])
                P.I("dve", "tensor_tensor", out=num[:], in0=num[:], in1=og[:, 2], op=ALU.add, R=[("ogt", ti % 2), "num"], W=["num"])
                P.I("dve", "reciprocal", out=rl[:], in_=num[:, :, 128], R=["num"], W=["rl"])
                P.I("dve", "tensor_tensor", out=bt[:].rearrange("p (h d) -> p h d", h=4), in0=num[:, :, 0:128], in1=rl[:, :, None].to_broadcast([128, 4, 128]), op=ALU.mult,
                     R=["num", "rl"], W=["bt"])
                P.I("dve", "tensor_tensor", out=bt[:], in0=bt[:], in1=ut[:], op=ALU.mult, R=["bt", ("utmp", ti % 2)], W=["bt"])
                P.I("dve", "tensor_tensor", out=vm[:, tb, ct * 512:(ct + 1) * 512], in0=bt[:], in1=vm[:, tb, ct * 512:(ct + 1) * 512], op=ALU.add,
                     R=["bt", ("vm", tb, ct)], W=[("vm", tb, ct)])
    if debug:
        dbg["merged"] = do("dbg_merged", [128, 8, D], BF16)
        P.D("sp", out=dbg["merged"], in_=vm[:], R=[("vm", tb, c) for tb in range(8) for c in range(4)])
        dbg["merged_s"] = do("dbg_merged_s", [16, D], BF16)
        P.D("sp", out=dbg["merged_s"], in_=vms[:], R=[("vms", c) for c in range(4)])
    for tb in range(8):
        P.D("sp", out=mgd[tb], in_=vm[:, tb, :], R=[("vm", tb, c) for c in range(4)], W=["mgd"])
    P.D("sp", out=mgd[8, 0:16], in_=vms[:], R=[("vms", c) for c in range(4)], W=["mgd"])
    P.end_phase()
    es_ab.close()
    if stop_after == "B2":
        return nc, P, dbg

    es_d = contextlib.ExitStack()
    acc = P.ssb(es_d, [128, 9, D], F32)
    es_e = contextlib.ExitStack()
    xn2T = P.ssb(es_e, [128, 16, 1040], BF16)
    es_c2 = contextlib.ExitStack()
    mT = P.ssb(es_c2, [128, 16, 1040], BF16)
    wbs2 = [P.ssb(es_c2, [128, 16, 512], BF16) for _ in range(2)]
    mrow = [P.psb([128, D], BF16) for _ in range(2)]
    tc1 = {"n": 0}

    def transpose_rows(src_rows, npart, dstT, col0, rtag="srcrows"):
        for c4 in range(4):
            half = tc1["n"] % 2
            tc1["n"] += 1
            for j in range(4):
                c = c4 * 4 + j
                P.I("pe", "transpose", out=PTs[half][:, j, :npart], in_=src_rows[:, c * 128:(c + 1) * 128], identity=identb[:npart, :npart],
                    R=[rtag, "identb"], W=[("PT", half)])
            if c4 % 2 == 0:
                P.I("act", "copy", out=dstT[:, c4 * 4:(c4 + 1) * 4, col0:col0 + npart], in_=PTs[half][:, 0:4, :npart], R=[("PT", half)], W=["dstT"])
            else:
                P.I("dve", "tensor_copy", out=dstT[:, c4 * 4:(c4 + 1) * 4, col0:col0 + npart], in_=PTs[half][:, 0:4, :npart], R=[("PT", half)], W=["dstT"])

    for tb in range(9):
        npart = 128 if tb < 8 else 16
        mr = mrow[tb % 2]
        P.D("sp", out=mr[:npart], in_=mgd[tb, 0:npart], W=[("mrow", tb % 2)])
        transpose_rows(mr[:npart, :], npart, mT, tb * 128, rtag=("mrow", tb % 2))
    P.end_phase()

    xres = [P.psb([128, 512], F32) for _ in range(2)]
    ssC = P.psb([128, 9], F32)
    rsC = P.psb([128, 9], F32)
    gBffn = P.psb([128, D], F32)
    xnbC = {"sq": P.psb([128, D], BF16), "b": [P.psb([128, D], BF16) for _ in range(2)]}
    P.D("sp", out=gBffn[:], in_=g_ffn.partition_broadcast(128), W=["gB"])
    wn = {"n": 0}
    xr = 0
    for ct in range(4):
        wb = wbs2[wn["n"] % 2]
        wn["n"] += 1
        P.D("pool", out=wb[:], in_=w_out[:, ct * 512:(ct + 1) * 512].rearrange("(c p) n -> p c n", p=128), W=[("wb", id(wb))])
        for tb in range(9):
            npart = 128 if tb < 8 else 16
            k = pjn["n"] % 2
            pjn["n"] += 1
            pj = PJ[k]
            for dc in range(16):
                P.I("pe", "matmul", pj[:npart, :], lhsT=mT[:, dc, tb * 128:tb * 128 + npart], rhs=wb[:, dc, :],
                                                                                       start=(dc == 0), stop=(dc == 15),
                     R=[("wb", id(wb))], W=[("PJ", k)])
            xrt = xres[xr % 2]
            src = xp[1024 + tb * 128:1024 + (tb + 1) * 128, ct * 512:(ct + 1) * 512] if tb < 8 else xs[:, ct * 512:(ct + 1) * 512]
            P.D("sp", out=xrt[:npart], in_=src, W=[("xres", xr % 2)])
            P.I("dve", "tensor_tensor", out=acc[:npart, tb, ct * 512:(ct + 1) * 512], in0=pj[:npart], in1=xrt[:npart], op=ALU.add,
                 R=[("PJ", k), ("xres", xr % 2)], W=[("acc", tb, ct)])
            xr += 1
    if debug:
        dbg["h"] = do("dbg_h", [128, 9, D])
        P.D("sp", out=dbg["h"][:, 0:8], in_=acc[:, 0:8], R=[("acc", tb, c) for tb in range(8) for c in range(4)])
        P.D("sp", out=dbg["h"][0:16, 8], in_=acc[0:16, 8], R=[("acc", 8, c) for c in range(4)])
    tcount["n"] = 0
    for tb in range(9):
        npart = 128 if tb < 8 else 16
        k = tb
        ss = ssC[:, tb:tb + 1]
        rs = rsC[:, tb:tb + 1]
        sq = xnbC["sq"]
        rt = [("acc", tb, c) for c in range(4)]
        P.I("act", "activation", out=sq[:npart], in_=acc[:npart, tb, :], func=AF.Square, accum_out=ss[:npart], R=rt, W=["sqj", ("ss", k)])
        P.I("act", "activation", out=rs[:npart], in_=ss[:npart], func=AF.Sqrt, scale=1.0 / D, bias=EPS, R=[("ss", k)], W=[("rs", k)])
        P.I("dve", "reciprocal", out=rs[:npart], in_=rs[:npart], R=[("rs", k)], W=[("rs", k)])
        xb = xnbC["b"][k % 2]
        P.I("dve", "scalar_tensor_tensor", out=xb[:npart], in0=acc[:npart, tb, :], scalar=rs[:npart, 0:1], in1=gBffn[:npart], op0=ALU.mult, op1=ALU.mult,
             R=rt + [("rs", k), "gB"], W=["srcrows"])
        transpose_rows(xb[:npart, :], npart, xn2T, tb * 128)
    P.end_phase()
    es_c2.close()
    if stop_after == "C":
        return nc, P, dbg

    es_q = contextlib.ExitStack()
    qTp = P.ssb(es_q, [128, 16, 1040], BF16)
    kTs = P.ssb(es_q, [128, 2, 128], BF16)
    wq = [P.psb([128, 16, 512], BF16) for _ in range(2)]
    skl = P.psb([128, 128], F32)
    for c in range(2):
        P.D("sp", out=skl[:], in_=subk[c], W=["skl"])
        P.I("pe", "transpose", out=SBK[0][:, 0:128], in_=skl[:], identity=identf[:], R=["skl", "identf"], W=[("SBK", 0)])
        P.I("dve", "tensor_copy", out=kTs[:, c, :], in_=SBK[0][:, 0:128], R=[("SBK", 0)], W=["kTs"])
    TT = [(0, 512), (512, 512), (1024, 16)]
    for ct in range(4):
        wb = wq[ct % 2]
        P.D("pool", out=wb[:], in_=w_q[:, ct * 512:(ct + 1) * 512].rearrange("(c p) n -> p c n", p=128), W=[("wb", id(wb))])
        for j in range(4):
            hc = ct * 4 + j
            for (t0, tn_) in TT:
                k = pjn["n"] % 2
                pjn["n"] += 1
                pj = PJ[k]
                for dc in range(16):
                    P.I("pe", "matmul", pj[:, 0:tn_], lhsT=wb[:, dc, j * 128:(j + 1) * 128], rhs=xn2T[:, dc, t0:t0 + tn_],
                                                                                          start=(dc == 0), stop=(dc == 15),
                         R=[("wb", id(wb)), "dstT"], W=[("PJ", k)])
                P.I("act", "copy", out=qTp[:, hc, t0:t0 + tn_], in_=pj[:, 0:tn_], R=[("PJ", k)], W=["qTp"])
    P.end_phase()

    s_sb = P.psb([128, 16, 128], F32)
    t16 = P.psb([128, 2, 16], F32)
    tmp128 = P.psb([128, 128], F32)
    cand = P.psb([128, 256], F32)
    cand2 = P.psb([128, 256], F32)
    tc16 = P.psb([128, 16], F32)
    etmp = P.psb([128, 16], F32)
    thr = P.psb([128, 8], F32)
    c0 = P.psb([128, 8], F32)
    zs = P.psb([128, 8], F32)
    NQ = 16
    RQ = 128 // NQ
    At = [P.psb([128, RQ, 128], F32) for _ in range(3)]
    Et = [P.psb([128, RQ, 128], F32) for _ in range(3)]
    GhB = [P.psb([128, RQ, 128], BF16) for _ in range(3)]
    git = 0
    Gq = [P.psb([128, RQ * 128], BF16) for _ in range(2)]
    gq = 0
    for tb in range(9):
        npart = 128 if tb < 8 else 16
        for hc4 in range(4):
            for j in range(4):
                hc = hc4 * 4 + j
                bank = [SBK[0], SBK[1], OB[0], OB[1]][hc4]
                P.I("pe", "matmul", bank[:npart, j * 128:(j + 1) * 128], lhsT=qTp[:, hc, tb * 128:tb * 128 + npart], rhs=kTs[:, hc % 2, :],
                                                                                         start=True, stop=True,
                     R=["qTp", "kTs"], W=[("SC", hc4)])
            bank = [SBK[0], SBK[1], OB[0], OB[1]][hc4]
            P.I("act", "copy", out=s_sb[:npart, hc4 * 4:(hc4 + 1) * 4, :], in_=bank[:npart, :].rearrange("p (a k) -> p a k", a=4),
                 R=[("SC", hc4)], W=[("s_sb", hc4)])
        for h in range(8):
            srd = [("s_sb", h // 2)]
            for c in range(2):
                sv = s_sb[:npart, 2 * h + c, :]
                P.I("dve", "max", out=t16[:npart, c, 0:8], in_=sv, R=srd, W=["t16"])
                P.I("dve", "match_replace", out=tmp128[:npart], in_to_replace=t16[:npart, c, 0:8], in_values=sv, imm_value=-BIG,
                     R=srd + ["t16"], W=["tmp128"])
                P.I("dve", "max", out=t16[:npart, c, 8:16], in_=tmp128[:npart], R=["tmp128"], W=["t16"])
            P.I("dve", "tensor_tensor", out=cand[:npart].rearrange("p (a b) -> p a b", a=16), in0=t16[:npart, 0, :, None].to_broadcast([npart, 16, 16]),
                                                              in1=t16[:npart, 1, None, :].to_broadcast([npart, 16, 16]), op=ALU.add,
                 R=["t16"], W=["cand"])
            P.I("dve", "max", out=tc16[:npart, 0:8], in_=cand[:npart], R=["cand"], W=["tc16"])
            P.I("dve", "match_replace", out=cand2[:npart], in_to_replace=tc16[:npart, 0:8], in_values=cand[:npart], imm_value=-BIG,
                 R=["cand", "tc16"], W=["cand2"])
            P.I("dve", "max", out=tc16[:npart, 8:16], in_=cand2[:npart], R=["cand2"], W=["tc16"])
            P.I("dve", "tensor_copy", out=thr[:npart, h:h + 1], in_=tc16[:npart, 15:16], R=["tc16"], W=[("thr", h)])
            P.I("dve", "tensor_scalar", out=c0[:npart, h:h + 1], in0=tc16[:npart, 0:1], scalar1=-1.0, scalar2=None, op0=ALU.mult,
                 R=["tc16"], W=[("c0", h)])
            P.I("act", "activation", out=etmp[:npart], in_=tc16[:npart], func=AF.Exp, bias=c0[:npart, h:h + 1], scale=1.0, accum_out=zs[:npart, h:h + 1],
                 R=["tc16", ("c0", h)], W=["etmp", ("zs", h)])
            P.I("act", "activation", out=zs[:npart, h:h + 1], in_=zs[:npart, h:h + 1], func=AF.Ln, R=[("zs", h)], W=[("zs", h)])
            P.I("dve", "tensor_tensor", out=c0[:npart, h:h + 1], in0=c0[:npart, h:h + 1], in1=zs[:npart, h:h + 1], op=ALU.subtract,
                 R=[("c0", h), ("zs", h)], W=[("c0", h)])
        for qi in range(NQ):
            for h in range(8):
                sl = git % 3
                A = At[sl]
                E = Et[sl]
                gsl = git % 3
                git += 1
                G_ = GhB[gsl]
                s1 = s_sb[:npart, 2 * h, qi * RQ:(qi + 1) * RQ]
                s2 = s_sb[:npart, 2 * h + 1, :]
                P.I("pool", "tensor_tensor", out=A[:npart], in0=s1[:, :, None].to_broadcast([npart, RQ, 128]),
                    in1=s2[:, None, :].to_broadcast([npart, RQ, 128]), op=ALU.add, R=[("s_sb", h // 2)], W=[("At", sl)])
                P.I("act", "activation", out=E[:npart], in_=A[:npart], func=AF.Exp, bias=c0[:npart, h:h + 1], scale=1.0,
                    R=[("At", sl), ("c0", h)], W=[("Et", sl)])
                P.I("dve", "scalar_tensor_tensor", out=G_[:npart], in0=A[:npart], scalar=thr[:npart, h:h + 1], in1=E[:npart], op0=ALU.is_ge, op1=ALU.mult,
                    R=[("At", sl), ("Et", sl), ("thr", h)], W=[("GhB", gsl)])
                gflat = G_[:npart].rearrange("p a b -> p (a b)")
                for half in range(2):
                    P.I("pe", "matmul", PJ[half][:npart, :], lhsT=identb[:npart, :npart], rhs=gflat[:, half * 512:(half + 1) * 512], start=(h == 0), stop=(h == 7),
                        R=[("GhB", gsl), "identb"], W=[("PJ", half)])
            gb = Gq[gq % 2]
            for half in range(2):
                P.I("act", "copy", out=gb[:npart, half * 512:(half + 1) * 512], in_=PJ[half][:npart, :], R=[("PJ", half)], W=[("Gq", gq % 2)])
            P.D("sp", out=Gd[tb, 0:npart, qi * RQ * 128:(qi + 1) * RQ * 128], in_=gb[:npart], R=[("Gq", gq % 2)], W=["Gd"])
            gq += 1
    P.end_phase()
    es_q.close()
    if stop_after == "D1":
        return nc, P, dbg

    EG = 256
    NG = NEXP // EG
    NCH = EG // 128
    ub = [P.psb([128, NCH, D], BF16) for _ in range(2)]
    vb = [P.psb([128, NCH, D], BF16) for _ in range(2)]
    uT = [P.psb([128, 16, EG], BF16) for _ in range(2)]
    Gt = [P.psb([128, 9, EG], BF16) for _ in range(2)]
    gt32 = [P.psb([128, EG], F32) for _ in range(2)]
    actb = [P.psb([128, EG], BF16) for _ in range(2)]
    actT = [P.psb([128, NCH, 128], BF16) for _ in range(2)]
    o2b = [SBK[0], SBK[1], OB[0], OB[1]]
    it = 0
    for eg in range(NG):
        sl = eg % 2
        for c in range(NCH):
            P.D("pool", out=ub[sl][:, c, :], in_=pu[eg * EG + c * 128:eg * EG + (c + 1) * 128, :], W=[("ub", sl)], max_dma_last_dim=4096)
        for c in range(NCH):
            P.D("pool", out=vb[sl][:, c, :], in_=pv[eg * EG + c * 128:eg * EG + (c + 1) * 128, :], W=[("vb", sl)], max_dma_last_dim=4096)
        P.D("sp", out=Gt[sl][:, 0:8, :], in_=Gd[0:8, :, eg * EG:(eg + 1) * EG].rearrange("t p e -> p t e"), R=["Gd"], W=[("Gt", sl)])
        P.D("sp", out=Gt[sl][0:16, 8, :], in_=Gd[8, 0:16, eg * EG:(eg + 1) * EG], R=["Gd"], W=[("Gt", sl)])
        for c in range(NCH):
            for d4 in range(4):
                half = (c * 4 + d4) % 2
                for j in range(4):
                    dc = d4 * 4 + j
                    P.I("pe", "transpose", out=PTs[half][:, j, :], in_=ub[sl][:, c, dc * 128:(dc + 1) * 128], identity=identb[:],
                         R=[("ub", sl), "identb"], W=[("PT", half)])
                if d4 % 2 == 0:
                    P.I("act", "copy", out=uT[sl][:, d4 * 4:(d4 + 1) * 4, c * 128:(c + 1) * 128], in_=PTs[half][:, 0:4, :],
                         R=[("PT", half)], W=[("uT", sl)])
                else:
                    P.I("dve", "tensor_copy", out=uT[sl][:, d4 * 4:(d4 + 1) * 4, c * 128:(c + 1) * 128], in_=PTs[half][:, 0:4, :],
                         R=[("PT", half)], W=[("uT", sl)])
        def stage1(tb):
            npart = 128 if tb < 8 else 16
            k = tb % 2
            pj = PJ[k]
            s2 = tb % 2
            for dc in range(16):
                P.I("pe", "matmul", pj[:npart, 0:EG], lhsT=xn2T[:, dc, tb * 128:tb * 128 + npart], rhs=uT[sl][:, dc, :], start=(dc == 0), stop=(dc == 15),
                    R=[("uT", sl)], W=[("PJ", k)])
            P.I("act", "activation", out=gt32[s2][:npart], in_=pj[:npart, 0:EG], func=AF.Gelu_apprx_tanh, R=[("PJ", k)], W=[("gt32", s2)])
            P.I("dve", "tensor_tensor", out=actb[s2][:npart], in0=gt32[s2][:npart], in1=Gt[sl][:npart, tb, :], op=ALU.mult,
                R=[("gt32", s2), ("Gt", sl)], W=[("actb", s2)])

        def stage2(tb):
            npart = 128 if tb < 8 else 16
            s2 = tb % 2
            for c in range(NCH):
                P.I("pe", "transpose", out=PTs[s2][:, c, :npart], in_=actb[s2][:npart, c * 128:(c + 1) * 128], identity=identb[:npart, :npart],
                    R=[("actb", s2), "identb"], W=[("PT", s2)])
            P.I("act", "copy", out=actT[s2][:, :, :npart], in_=PTs[s2][:, 0:NCH, :npart], R=[("PT", s2)], W=[("actT", s2)])
            for dt_ in range(4):
                for c in range(NCH):
                    P.I("pe", "matmul", o2b[dt_][:npart, :], lhsT=actT[s2][:, c, :npart], rhs=vb[sl][:, c, dt_ * 512:(dt_ + 1) * 512], start=(c == 0), stop=(c == NCH - 1),
                        R=[("actT", s2), ("vb", sl)], W=[("O2", dt_)])
            for dt_ in range(4):
                P.I("dve", "tensor_tensor", out=acc[:npart, tb, dt_ * 512:(dt_ + 1) * 512], in0=o2b[dt_][:npart, :], in1=acc[:npart, tb, dt_ * 512:(dt_ + 1) * 512], op=ALU.add,
                    R=[("O2", dt_)], W=[("O2", dt_), ("accD", tb, dt_)])

        stage1(0)
        stage1(1)
        for tb in range(9):
            stage2(tb)
            if tb + 2 < 9:
                stage1(tb + 2)
    P.end_phase()
    es_e.close()

    gBfin = P.psb([128, D], F32)
    sqE = P.psb([128, D], F32)
    outE = [P.psb([128, D], F32) for _ in range(2)]
    P.D("sp", out=gBfin[:], in_=g_fin.partition_broadcast(128), W=["gBfin"])
    for tb in range(9):
        npart = 128 if tb < 8 else 16
        ss = P.psb([128, 1], F32)
        rs = P.psb([128, 1], F32)
        ot = outE[tb % 2]
        P.I("act", "activation", out=sqE[:npart], in_=acc[:npart, tb, :], func=AF.Square, accum_out=ss[:npart], W=["sqE", ("ssE", tb)])
        P.I("act", "activation", out=rs[:npart], in_=ss[:npart], func=AF.Sqrt, scale=1.0 / D, bias=EPS, R=[("ssE", tb)], W=[("rsE", tb)])
        P.I("dve", "reciprocal", out=rs[:npart], in_=rs[:npart], R=[("rsE", tb)], W=[("rsE", tb)])
        P.I("dve", "scalar_tensor_tensor", out=ot[:npart], in0=acc[:npart, tb, :], scalar=rs[:npart, 0:1], in1=gBfin[:npart], op0=ALU.mult, op1=ALU.mult,
             R=[("rsE", tb), "gBfin"], W=[("outE", tb % 2)])
        dst = yp[tb * 128:(tb + 1) * 128, :] if tb < 8 else ys
        P.D("sp", out=dst, in_=ot[:npart], R=[("outE", tb % 2)])
    P.end_phase()
    es_d.close()
    return nc, P, dbg


def make_in_maps(inputs):
    f = lambda a: np.ascontiguousarray(np.asarray(a, dtype=np.float32))
    xpr = f(inputs["x_prompt"])
    xsm = f(inputs["x_sample"])
    shared = dict(
        w_in=f(inputs["w_in"][0]), w_out=f(inputs["w_out"][0]), w_q=f(inputs["peer_w_q"][0]), subk=f(inputs["peer_sub_keys"][0]),
        pu=f(inputs["peer_u"][0]), pv=f(inputs["peer_v"][0]),
        g_mix=f(inputs["norm_mix_g"][0]).reshape(1, D), g_sgu=f(inputs["sgu_norm_g"][0]).reshape(1, D),
        g_ffn=f(inputs["norm_ffn_g"][0]).reshape(1, D), g_fin=f(inputs["norm_final_g"]).reshape(1, D),
        sgu_w=f(inputs["sgu_w"][0]), sgu_b=f(inputs["sgu_b"][0]),
    )
    c128 = f(inputs["cache_kv_w128"][0]).reshape(32, 128, 2, D)
    c512 = f(inputs["cache_kv_w512"][0]).reshape(32, 512, 2, D)
    c2048 = f(inputs["cache_kv_w2048"][0]).reshape(32, 2048, 2, D)
    maps = []
    for c in range(8):
        b, th = c // 2, c % 2
        xpc = np.zeros((2048, D), np.float32)
        if th == 1:
            xpc[:] = xpr[b]
        else:
            xpc[1024:] = xpr[b, :1024]
        m = dict(shared)
        m.update(const_tables(th))
        m["xp"] = xpc
        m["xs"] = np.ascontiguousarray(xsm[4 * c:4 * c + 4].reshape(16, D))
        m["ck128"] = np.ascontiguousarray(c128[4 * c:4 * c + 4])
        m["ck512"] = np.ascontiguousarray(c512[4 * c:4 * c + 4])
        m["ck2048"] = np.ascontiguousarray(c2048[4 * c:4 * c + 4])
        maps.append(m)
    return maps


def assemble(results):
    y_prompt = np.zeros((4, 2048, D), np.float32)
    y_sample = np.zeros((32, 4, D), np.float32)
    kvp = [np.zeros((1, 4, w, 2, NH, DH), np.float32) for w in (128, 512, 2048)]
    kvs = [np.zeros((1, 32, 4, 2, NH, DH), np.float32) for _ in range(3)]
    sguv = np.zeros((1, 32, 4, D), np.float32)
    for c in range(8):
        r = results[c]
        b, th = c // 2, c % 2
        y_prompt[b, th * 1024:(th + 1) * 1024] = r["yp"]
        y_sample[4 * c:4 * c + 4] = r["ys"].reshape(4, 4, D)
        if th == 1:
            kvp[0][0, b] = r["kvp128"].reshape(128, 2, NH, DH)
            kvp[1][0, b] = r["kvp512"].reshape(512, 2, NH, DH)
        kvp[2][0, b, th * 1024:(th + 1) * 1024] = r["kvp2048"].reshape(1024, 2, NH, DH)
        for g, nm in enumerate(("kvs128", "kvs512", "kvs2048")):
            kvs[g][0, 4 * c:4 * c + 4] = r[nm].reshape(4, 4, 2, NH, DH)
        sguv[0, 4 * c:4 * c + 4] = r["sguv"].reshape(4, 4, D)
    return (y_prompt, y_sample, kvp[0], kvp[1], kvp[2], kvs[0], kvs[1], kvs[2], sguv)


_CACHE = {}


def kernel(**inputs):
    if "nc" not in _CACHE:
        _CACHE["nc"] = build()[0]
    nc = _CACHE["nc"]
    maps = make_in_maps(inputs)
    res = run_bass_kernel_spmd(nc, maps, core_ids=list(range(8)))
    return assemble(res.results)
```
